# Optimizing a Trainium2 kernel written in Bass

```python
import math
import jax, jax.numpy as jnp
from jax import lax
import numpy as np

D_MODEL = 2048
BATCH = 1
SEQ = 8192
DEPTH = 4

N_MIXERS = 3
N_MLSTM_LAYERS = len(range(0, DEPTH, N_MIXERS))
N_RWKV_LAYERS = len(range(1, DEPTH, N_MIXERS))
N_DIFF_LAYERS = len(range(2, DEPTH, N_MIXERS))
NORM_EPS = 1e-6
MLSTM_HEADS = 4
MLSTM_DQK = D_MODEL // 2 // MLSTM_HEADS
MLSTM_DV = D_MODEL // MLSTM_HEADS
MLSTM_CHUNK = 128
GATE_SOFTCAP = 15.0
MLSTM_IN = 2 * MLSTM_HEADS * MLSTM_DQK + 2 * D_MODEL + 2 * MLSTM_HEADS
RWKV_HEAD = 64
RWKV_HEADS = D_MODEL // RWKV_HEAD
DECAY_LORA = 96
AAA_LORA = 96
GATE_LORA = 256
RWKV_GN_EPS = 64e-5
DIFF_HEADS = 8
DIFF_DQK = D_MODEL // DIFF_HEADS // 2
DIFF_DV = 2 * DIFF_DQK
DIFF_IN = 4 * DIFF_HEADS * DIFF_DQK + DIFF_HEADS * DIFF_DV
DIFF_SUBLN_EPS = 1e-5
ROPE_DIM = DIFF_DQK // 4
ROPE_THETA = 500000.0
Q_BLOCK = 128
N_EXPERTS = 32
TOP_K = 4
D_EXPERT = 768
SWIGLU_LIMIT = 7.0
SWIGLU_ALPHA = 1.702
MOE_BLOCK = 128

kernel_name = "hybrid_mlstm_rwkv7_diffattn_moe"


def rms_norm(x, gain, eps=NORM_EPS):
    xf = x.astype(jnp.float32)
    y = xf * lax.rsqrt(jnp.mean(xf * xf, axis=-1, keepdims=True) + eps)
    return (y * gain.astype(jnp.float32)).astype(x.dtype)


def modulate(h, shift, scale):
    return h * (1 + scale[:, None, :]) + shift[:, None, :]


def soft_cap(t):
    return GATE_SOFTCAP * jnp.tanh(t / GATE_SOFTCAP)


def mlstm_mixer(h, w_in, b_gates, norm_gain, w_out):
    B, S, D = h.shape
    H, DK, DV, L = MLSTM_HEADS, MLSTM_DQK, MLSTM_DV, MLSTM_CHUNK
    NC = S // L
    f32 = jnp.float32
    qk = H * DK
    proj = h @ w_in
    o_gate = jax.nn.sigmoid(proj[..., 2 * qk + D:2 * qk + 2 * D])
    gates = soft_cap(proj[..., 2 * qk + 2 * D:].astype(f32) + b_gates.astype(f32))

    def chunked(t, d):
        return t.astype(f32).reshape(B, NC, L, H, d).transpose(0, 3, 1, 2, 4)

    q = chunked(proj[..., :qk], DK) * (DK ** -0.5)
    k = chunked(proj[..., qk:2 * qk], DK)
    v = chunked(proj[..., 2 * qk:2 * qk + D], DV)
    log_i = gates[..., :H].reshape(B, NC, L, H).transpose(0, 3, 1, 2)
    log_f = jax.nn.log_sigmoid(gates[..., H:]).reshape(B, NC, L, H).transpose(0, 3, 1, 2)
    g = jnp.cumsum(log_f, axis=-1)
    g_tot = g[..., -1]

    w_state = g_tot[..., None] - g + log_i
    m_loc = jnp.max(w_state, axis=-1)
    e_state = jnp.exp(w_state - m_loc[..., None])
    kv_chunk = jnp.einsum('bhcl,bhcld,bhcle->bhcde', e_state, k, v)
    n_chunk = jnp.einsum('bhcl,bhcld->bhcd', e_state, k)

    def step(carry, inp):
        C, n, m = carry
        kv_c, n_c, gt, ml = inp
        m_new = jnp.maximum(gt + m, ml)
        a = jnp.exp(gt + m - m_new)
        b = jnp.exp(ml - m_new)
        C_new = a[..., None, None] * C + b[..., None, None] * kv_c
        n_new = a[..., None] * n + b[..., None] * n_c
        return (C_new, n_new, m_new), (C, n, m)

    init = (jnp.zeros((B, H, DK, DV), f32), jnp.zeros((B, H, DK), f32), jnp.zeros((B, H), f32))
    xs = (kv_chunk.transpose(2, 0, 1, 3, 4), n_chunk.transpose(2, 0, 1, 3),
          g_tot.transpose(2, 0, 1), m_loc.transpose(2, 0, 1))
    _, (C_prev, n_prev, m_prev) = lax.scan(step, init, xs)
    C_prev = C_prev.transpose(1, 2, 0, 3, 4)
    n_prev = n_prev.transpose(1, 2, 0, 3)
    m_prev = m_prev.transpose(1, 2, 0)

    causal = jnp.tril(jnp.ones((L, L), dtype=bool))
    d_log = jnp.where(causal, g[..., :, None] - g[..., None, :] + log_i[..., None, :], -jnp.inf)
    inter_log = g + m_prev[..., None]
    m_t = jnp.maximum(inter_log, jnp.max(d_log, axis=-1))
    s = jnp.einsum('bhcid,bhcjd->bhcij', q, k) * jnp.exp(d_log - m_t[..., None])
    inter_scale = jnp.exp(inter_log - m_t)
    num = jnp.einsum('bhcij,bhcje->bhcie', s, v) + inter_scale[..., None] * jnp.einsum('bhcid,bhcde->bhcie', q, C_prev)
    den = jnp.sum(s, axis=-1) + inter_scale * jnp.einsum('bhcid,bhcd->bhci', q, n_prev)
    hh = num / jnp.maximum(jnp.abs(den), jnp.exp(-m_t))[..., None]
    hh = hh.transpose(0, 2, 3, 1, 4).reshape(B, S, H, DV)
    hh = rms_norm(hh, norm_gain.reshape(H, DV)).reshape(B, S, D).astype(h.dtype)
    return (o_gate * hh) @ w_out


def rwkv7_mixer(h, mu, w_rkv, w0, w1, w2, a0, a1, a2, g1, g2, k_k, k_a, r_k, ln_gain, ln_bias, w_o):
    B, S, D = h.shape
    H, N = RWKV_HEADS, RWKV_HEAD
    f32 = jnp.float32
    xx = jnp.pad(h, ((0, 0), (1, 0), (0, 0)))[:, :-1] - h
    mixed = h[:, :, None, :] + xx[:, :, None, :] * mu
    rkv = jnp.einsum('bsjd,jde->bsje', mixed[:, :, :3], w_rkv)
    r, k, v = rkv[:, :, 0], rkv[:, :, 1], rkv[:, :, 2]
    xw, xa, xg = mixed[:, :, 3], mixed[:, :, 4], mixed[:, :, 5]
    w_log = -jax.nn.softplus(-(w0 + jnp.tanh(xw @ w1) @ w2)) - 0.5
    decay = jnp.exp(-jnp.exp(w_log.astype(f32)))
    a = jax.nn.sigmoid(a0 + (xa @ a1) @ a2)
    gate = jax.nn.sigmoid(xg @ g1) @ g2
    kk = (k * k_k).reshape(B, S, H, N).astype(f32)
    kk = kk / jnp.maximum(jnp.linalg.norm(kk, axis=-1, keepdims=True), 1e-12)
    k = k * (1 + (a - 1) * k_a)

    def heads(t):
        return t.astype(f32).reshape(B, S, H, N)

    r_h, k_h, v_h, a_h, w_h = heads(r), heads(k), heads(v), heads(a), heads(decay)

    def step(state, inp):
        r_t, w_t, k_t, v_t, kk_t, a_t = inp
        sa = jnp.einsum('bhvk,bhk->bhv', state, -kk_t)
        state = state * w_t[:, :, None, :] + sa[..., None] * (kk_t * a_t)[:, :, None, :] + v_t[..., None] * k_t[:, :, None, :]
        return state, jnp.einsum('bhvk,bhk->bhv', state, r_t)

    xs = tuple(t.transpose(1, 0, 2, 3) for t in (r_h, w_h, k_h, v_h, kk, a_h))
    _, y = lax.scan(step, jnp.zeros((B, H, N, N), f32), xs)
    y = y.transpose(1, 0, 2, 3)
    mean = jnp.mean(y, axis=-1, keepdims=True)
    var = jnp.mean(jnp.square(y - mean), axis=-1, keepdims=True)
    y = ((y - mean) * lax.rsqrt(var + RWKV_GN_EPS)).reshape(B, S, D) * ln_gain.astype(f32) + ln_bias.astype(f32)
    bonus = jnp.sum(r_h * k_h * r_k.astype(f32), axis=-1, keepdims=True) * v_h
    y = (y + bonus.reshape(B, S, D)).astype(h.dtype)
    return (y * gate) @ w_o


def rope_tables(positions):
    inv_freq = ROPE_THETA ** (-jnp.arange(0, ROPE_DIM, 2, dtype=jnp.float32) / ROPE_DIM)
    ang = positions.astype(jnp.float32)[..., None] * inv_freq
    return jnp.cos(ang)[:, :, None, None, :], jnp.sin(ang)[:, :, None, None, :]


def partial_rope(t, cos, sin):
    half = ROPE_DIM // 2
    tf = t.astype(jnp.float32)
    x1, x2 = tf[..., :half], tf[..., half:ROPE_DIM]
    out = jnp.concatenate([x1 * cos - x2 * sin, x2 * cos + x1 * sin, tf[..., ROPE_DIM:]], axis=-1)
    return out.astype(t.dtype)


def diff_attn_mixer(h, cos, sin, w_qkv, lam_params, subln_gain, w_o, layer):
    B, S, D = h.shape
    H, DK, DV = DIFF_HEADS, DIFF_DQK, DIFF_DV
    proj = h @ w_qkv
    q = partial_rope(proj[..., :2 * H * DK].reshape(B, S, H, 2, DK), cos, sin)
    k = partial_rope(proj[..., 2 * H * DK:4 * H * DK].reshape(B, S, H, 2, DK), cos, sin)
    v = proj[..., 4 * H * DK:].reshape(B, S, H, DV)
    lambda_init = 0.8 - 0.6 * math.exp(-0.3 * layer)
    lp = lam_params.astype(jnp.float32)
    lam = jnp.exp(jnp.sum(lp[0] * lp[1])) - jnp.exp(jnp.sum(lp[2] * lp[3])) + lambda_init
    qh = q.transpose(0, 2, 3, 1, 4)
    kh = k.transpose(0, 2, 3, 1, 4)
    vh = v.transpose(0, 2, 1, 3)
    key_idx = jnp.arange(S)
    scale = DK ** -0.5

    def block(i):
        start = i * Q_BLOCK
        qb = lax.dynamic_slice_in_dim(qh, start, Q_BLOCK, axis=3)
        s = jnp.einsum('bhcqd,bhckd->bhcqk', qb, kh).astype(jnp.float32) * scale
        mask = key_idx[None, :] <= (start + jnp.arange(Q_BLOCK))[:, None]
        p = jax.nn.softmax(jnp.where(mask, s, -jnp.inf), axis=-1)
        attn = p[:, :, 0] - lam * p[:, :, 1]
        return jnp.einsum('bhqk,bhkd->bhqd', attn.astype(vh.dtype), vh)

    out = lax.map(block, jnp.arange(S // Q_BLOCK))
    out = out.transpose(1, 0, 3, 2, 4).reshape(B, S, H, DV)
    out = rms_norm(out, subln_gain, DIFF_SUBLN_EPS) * (1 - lambda_init)
    return out.reshape(B, S, D) @ w_o


def moe_ffn(h, router_w, router_b, w_gu, b_gu, w_down, b_down):
    B, S, D = h.shape
    T = B * S
    E, K, BLK = N_EXPERTS, TOP_K, MOE_BLOCK
    xt = h.reshape(T, D)
    logits = (xt @ router_w + router_b).astype(jnp.float32)
    top_val, top_idx = lax.top_k(logits, K)
    gate_w = jax.nn.softmax(top_val, axis=-1)
    flat_e = top_idx.reshape(-1)
    order = jnp.argsort(flat_e)
    sorted_e = flat_e[order]
    tok = (order // K).astype(jnp.int32)
    counts = jnp.bincount(flat_e, length=E)
    group_start = jnp.cumsum(counts) - counts
    padded = (counts + BLK - 1) // BLK * BLK
    padded_end = jnp.cumsum(padded)
    padded_start = padded_end - padded
    dest = padded_start[sorted_e] + jnp.arange(T * K) - group_start[sorted_e]
    n_rows = T * K + E * BLK
    n_blk = n_rows // BLK
    row_tok = jnp.full((n_rows,), T, jnp.int32).at[dest].set(tok)
    x_pad = jnp.concatenate([xt, jnp.zeros((1, D), xt.dtype)], axis=0)
    xb = x_pad[row_tok].reshape(n_blk, BLK, D)
    blk_expert = jnp.minimum(jnp.searchsorted(padded_end, jnp.arange(n_blk) * BLK, side='right'), E - 1)

    def expert_block(args):
        xblk, e = args
        gu = xblk @ w_gu[e] + b_gu[e]
        g_lin = jnp.minimum(gu[:, 0::2], SWIGLU_LIMIT)
        up = jnp.clip(gu[:, 1::2], -SWIGLU_LIMIT, SWIGLU_LIMIT)
        glu = g_lin * jax.nn.sigmoid(g_lin * SWIGLU_ALPHA)
        return ((up + 1) * glu) @ w_down[e] + b_down[e]

    yb = lax.map(expert_block, (xb, blk_expert)).reshape(n_rows, D)
    w_sorted = gate_w.reshape(-1)[order].astype(yb.dtype)
    y = jax.ops.segment_sum(yb[dest] * w_sorted[:, None], tok, num_segments=T)
    return y.reshape(B, S, D)


def setup_inputs(seed: int = 0) -> dict:
    key = jax.random.key(seed)
    ks = list(jax.random.split(key, 48))
    f32 = jnp.float32
    D = D_MODEL

    def nrm(shape, scale):
        return jax.random.normal(ks.pop(), shape, f32) * scale

    A, Bn, Cn = N_MLSTM_LAYERS, N_RWKV_LAYERS, N_DIFF_LAYERS
    inp = {}
    inp['x'] = nrm((BATCH, SEQ, D), 1.0)
    inp['c'] = nrm((BATCH, D), 1.0)
    offset = jax.random.randint(ks.pop(), (BATCH, 1), 0, 4096, jnp.int32)
    inp['positions'] = (offset + jnp.arange(SEQ, dtype=jnp.int32)[None, :]).astype(jnp.int32)
    inp['norm_gain'] = 1.0 + nrm((DEPTH, 2, D), 0.02)
    inp['ada_w'] = nrm((DEPTH, D, 6 * D), 0.5 * D ** -0.5)
    inp['ada_b'] = nrm((DEPTH, 6 * D), 0.02)
    inp['mlstm_w_in'] = nrm((A, D, MLSTM_IN), D ** -0.5)
    f_bias = jnp.linspace(3.0, 6.0, MLSTM_HEADS, dtype=f32)[None, :]
    inp['mlstm_b_gates'] = jnp.concatenate([nrm((A, MLSTM_HEADS), 0.1), f_bias + nrm((A, MLSTM_HEADS), 0.1)], axis=-1)
    inp['mlstm_norm_gain'] = 1.0 + nrm((A, D), 0.02)
    inp['mlstm_w_out'] = nrm((A, D, D), D ** -0.5)
    inp['rwkv_mu'] = jax.random.uniform(ks.pop(), (Bn, 6, D), f32)
    inp['rwkv_w_rkv'] = nrm((Bn, 3, D, D), D ** -0.5)
    inp['rwkv_w0'] = jnp.linspace(-6.5, -1.5, D, dtype=f32)[None, :] + nrm((Bn, D), 0.1)
    inp['rwkv_w1'] = nrm((Bn, D, DECAY_LORA), D ** -0.5)
    inp['rwkv_w2'] = nrm((Bn, DECAY_LORA, D), 0.1 * DECAY_LORA ** -0.5)
    inp['rwkv_a0'] = nrm((Bn, D), 0.1)
    inp['rwkv_a1'] = nrm((Bn, D, AAA_LORA), D ** -0.5)
    inp['rwkv_a2'] = nrm((Bn, AAA_LORA, D), 0.1 * AAA_LORA ** -0.5)
    inp['rwkv_g1'] = nrm((Bn, D, GATE_LORA), D ** -0.5)
    inp['rwkv_g2'] = nrm((Bn, GATE_LORA, D), GATE_LORA ** -0.5)
    inp['rwkv_k_k'] = 0.85 + nrm((Bn, D), 0.02)
    inp['rwkv_k_a'] = 1.0 + nrm((Bn, D), 0.02)
    inp['rwkv_r_k'] = nrm((Bn, RWKV_HEADS, RWKV_HEAD), 0.1)
    inp['rwkv_ln_gain'] = 1.0 + nrm((Bn, D), 0.02)
    inp['rwkv_ln_bias'] = nrm((Bn, D), 0.02)
    inp['rwkv_w_o'] = nrm((Bn, D, D), D ** -0.5)
    inp['diff_w_qkv'] = nrm((Cn, D, DIFF_IN), D ** -0.5)
    inp['diff_lambda'] = nrm((Cn, 4, DIFF_DQK), 0.1)
    inp['diff_subln_gain'] = 1.0 + nrm((Cn, DIFF_DV), 0.02)
    inp['diff_w_o'] = nrm((Cn, D, D), D ** -0.5)
    inp['moe_router_w'] = nrm((DEPTH, D, N_EXPERTS), D ** -0.5)
    inp['moe_router_b'] = nrm((DEPTH, N_EXPERTS), 0.01)
    inp['moe_w_gate_up'] = nrm((DEPTH, N_EXPERTS, D, 2 * D_EXPERT), D ** -0.5)
    inp['moe_b_gate_up'] = nrm((DEPTH, N_EXPERTS, 2 * D_EXPERT), 0.02)
    inp['moe_w_down'] = nrm((DEPTH, N_EXPERTS, D_EXPERT, D), D_EXPERT ** -0.5)
    inp['moe_b_down'] = nrm((DEPTH, N_EXPERTS, D), 0.02)
    inp['final_gain'] = 1.0 + nrm((D,), 0.02)
    return inp


def reference(x, c, positions, norm_gain, ada_w, ada_b, mlstm_w_in, mlstm_b_gates, mlstm_norm_gain, mlstm_w_out,
              rwkv_mu, rwkv_w_rkv, rwkv_w0, rwkv_w1, rwkv_w2, rwkv_a0, rwkv_a1, rwkv_a2, rwkv_g1, rwkv_g2,
              rwkv_k_k, rwkv_k_a, rwkv_r_k, rwkv_ln_gain, rwkv_ln_bias, rwkv_w_o,
              diff_w_qkv, diff_lambda, diff_subln_gain, diff_w_o,
              moe_router_w, moe_router_b, moe_w_gate_up, moe_b_gate_up, moe_w_down, moe_b_down, final_gain):
    cos, sin = rope_tables(positions)
    mod = jnp.einsum('bd,lde->lbe', jax.nn.silu(c), ada_w) + ada_b[:, None, :]
    seen = [0, 0, 0]
    for layer in range(DEPTH):
        kind = layer % N_MIXERS
        j = seen[kind]
        seen[kind] += 1
        sh1, sc1, g1, sh2, sc2, g2 = jnp.split(mod[layer], 6, axis=-1)
        hm = modulate(rms_norm(x, norm_gain[layer, 0]), sh1, sc1)
        if kind == 0:
            y = mlstm_mixer(hm, mlstm_w_in[j], mlstm_b_gates[j], mlstm_norm_gain[j], mlstm_w_out[j])
        elif kind == 1:
            y = rwkv7_mixer(hm, rwkv_mu[j], rwkv_w_rkv[j], rwkv_w0[j], rwkv_w1[j], rwkv_w2[j], rwkv_a0[j],
                            rwkv_a1[j], rwkv_a2[j], rwkv_g1[j], rwkv_g2[j], rwkv_k_k[j], rwkv_k_a[j],
                            rwkv_r_k[j], rwkv_ln_gain[j], rwkv_ln_bias[j], rwkv_w_o[j])
        else:
            y = diff_attn_mixer(hm, cos, sin, diff_w_qkv[j], diff_lambda[j], diff_subln_gain[j], diff_w_o[j], layer)
        x = x + g1[:, None, :] * y
        hf = modulate(rms_norm(x, norm_gain[layer, 1]), sh2, sc2)
        x = x + g2[:, None, :] * moe_ffn(hf, moe_router_w[layer], moe_router_b[layer], moe_w_gate_up[layer],
                                         moe_b_gate_up[layer], moe_w_down[layer], moe_b_down[layer])
    return rms_norm(x, final_gain)
```

```python
import numpy as np
import concourse.bass as bass
import concourse.mybir as mybir
from concourse.bass_utils import run_bass_kernel_spmd

F32 = mybir.dt.float32
BF16 = mybir.dt.bfloat16
I32 = mybir.dt.int32
ALU = mybir.AluOpType
AF = mybir.ActivationFunctionType
AX = mybir.AxisListType

COMPUTE = ("pe", "act", "dve", "pool")
QUEUES = ("sp", "act", "pool")
NDMASEM = 12


class Tok:
    __slots__ = ("name", "last_w", "readers", "h")

    def __init__(self, name, h=None):
        self.name = name
        self.last_w = None
        self.readers = []
        self.h = h

    def __getitem__(self, idx):
        return self.h[idx]


class Ins:
    __slots__ = ("eng", "fn", "deps", "pos", "is_dma", "observed", "semval", "dsem", "dval", "prev_same_sem")

    def __init__(self, eng, fn, is_dma):
        self.eng = eng
        self.fn = fn
        self.deps = []
        self.is_dma = is_dma
        self.observed = False
        self.semval = None
        self.dsem = None
        self.dval = None
        self.prev_same_sem = None


class Prog:
    def __init__(self, same_engine_sync=True):
        self.nc = bass.Bass("TRN2", target_bir_lowering=False)
        self.ins = []
        self.streams = {e: [] for e in ("pe", "act", "dve", "pool", "sp")}
        self.same_engine_sync = same_engine_sync
        self.dma_rr = {q: 0 for q in QUEUES}
        self.dma_last = {}
        self._n = 0

    def dram_in(self, name, shape, dtype=F32):
        return self.nc.dram_tensor(name, list(shape), dtype, kind="ExternalInput")

    def dram_out(self, name, shape, dtype=F32):
        return self.nc.dram_tensor(name, list(shape), dtype, kind="ExternalOutput")

    def dram(self, name, shape, dtype=F32):
        return self.nc.dram_tensor(name, list(shape), dtype, kind="Internal")

    def sb(self, name, shape, dtype=F32):
        self._n += 1
        h = self.nc.alloc_sbuf_tensor(f"{name}_{self._n}", list(shape), dtype)
        return Tok(name, h)

    def ps(self, name, shape, dtype=F32):
        self._n += 1
        h = self.nc.alloc_psum_tensor(f"{name}_{self._n}", list(shape), dtype)
        return Tok(name, h)

    def tok(self, name, h=None):
        return Tok(name, h)

    def _record(self, eng, fn, reads, writes, is_dma):
        ins = Ins(eng, fn, is_dma)
        deps = []
        for t in reads:
            if t.last_w is not None:
                deps.append(t.last_w)
        for t in writes:
            if t.last_w is not None:
                deps.append(t.last_w)
            deps.extend(t.readers)
        seen = set()
        for d in deps:
            if id(d) not in seen and d is not ins:
                seen.add(id(d))
                ins.deps.append(d)
        for t in reads:
            t.readers.append(ins)
        for t in writes:
            t.last_w = ins
            t.readers = []
        ins.pos = len(self.streams[eng])
        self.streams[eng].append(ins)
        self.ins.append(ins)
        return ins

    def op(self, eng, fn, reads=(), writes=()):
        return self._record(eng, fn, reads, writes, False)

    def I(self, eng, name, kw, reads=(), writes=()):
        def fn(e, name=name, kw=kw):
            return getattr(e, name)(**kw)
        return self._record(eng, fn, reads, writes, False)

    def dma(self, q, out, in_, reads=(), writes=(), **kw):
        def fn(e, out=out, in_=in_, kw=kw):
            return e.dma_start(out=out, in_=in_, **kw)
        ins = self._record(q, fn, reads, writes, True)
        slot = self.dma_rr[q]
        self.dma_rr[q] = (slot + 1) % NDMASEM
        key = (q, slot)
        ins.dsem = key
        prev = self.dma_last.get(key)
        ins.prev_same_sem = prev
        ins.dval = (prev.dval if prev is not None else 0) + 16
        self.dma_last[key] = ins
        return ins

    def build(self):
        nc = self.nc
        seen_pos = {e: {f: -1 for f in self.streams} for e in self.streams}
        seen_dma = {e: set() for e in self.streams}
        waits = {}
        for ins in self.ins:
            w = []
            E = ins.eng
            for d in ins.deps:
                if d.is_dma:
                    if id(d) in seen_dma[E]:
                        continue
                    seen_dma[E].add(id(d))
                    w.append(d)
                else:
                    F = d.eng
                    if F == E:
                        if not self.same_engine_sync or E == "pe":
                            continue
                    if seen_pos[E][F] >= d.pos:
                        continue
                    seen_pos[E][F] = d.pos
                    d.observed = True
                    w.append(d)
            if ins.is_dma and ins.prev_same_sem is not None:
                p = ins.prev_same_sem
                if id(p) not in seen_dma[E]:
                    seen_dma[E].add(id(p))
                    w.append(p)
            waits[id(ins)] = w
        cnt = {}
        for e, st in self.streams.items():
            c = 0
            for ins in st:
                if not ins.is_dma and ins.observed:
                    c += 1
                    ins.semval = c
            cnt[e] = c
        self.sem_counts = cnt
        esem = {e: nc.alloc_semaphore(f"s_{e}") for e in COMPUTE}
        dsem = {}
        for q in QUEUES:
            for s in range(NDMASEM):
                if (q, s) in self.dma_last:
                    dsem[(q, s)] = nc.alloc_semaphore(f"d_{q}_{s}")
        streams = self.streams

        def emit(e, eng):
            for ins in streams[e]:
                for d in waits[id(ins)]:
                    if d.is_dma:
                        eng.wait_ge(dsem[d.dsem], d.dval)
                    else:
                        eng.wait_ge(esem[d.eng], d.semval)
                r = ins.fn(eng)
                if ins.is_dma:
                    r.then_inc(dsem[ins.dsem], 16)
                elif ins.observed:
                    r.then_inc(esem[e], 1)
            for (q, s), last in self.dma_last.items():
                if q == e:
                    eng.wait_ge(dsem[(q, s)], last.dval)

        with nc.Block() as block:
            @block.sync
            def _(eng):
                emit("sp", eng)

            @block.scalar
            def _(eng):
                emit("act", eng)

            @block.vector
            def _(eng):
                emit("dve", eng)

            @block.gpsimd
            def _(eng):
                emit("pool", eng)

            @block.tensor
            def _(eng):
                emit("pe", eng)
        return nc


TOK = 1024
HALVES = [(0, 512), (512, 1024)]
NCORES = 8


class Ring:
    def __init__(self, toks):
        self.t = toks
        self.i = 0

    def next(self):
        t = self.t[self.i % len(self.t)]
        self.i += 1
        return t


class Ctx:
    pass


def setup_common(P, npsum=8):
    C = Ctx()
    C.ps = Ring([P.ps(f"ps{i}", [128, 512]) for i in range(npsum)])
    C.ones_bf = P.sb("ones_bf", [128, 128], BF16)
    P.I("dve", "memset", dict(ap=C.ones_bf[:, :], constant=1.0), writes=[C.ones_bf])
    C.ones_f = P.sb("ones_f", [128, 128], F32)
    P.I("dve", "memset", dict(ap=C.ones_f[:, :], constant=1.0), writes=[C.ones_f])
    return C


def load_x(P, x_dram, name="xT_sb"):
    h = P.nc.alloc_sbuf_tensor(name, [128, 16, TOK], F32)
    toks = [P.tok(f"{name}{k}") for k in range(16)]
    for k in range(16):
        P.dma("sp", h[:, k, :], x_dram[k * 128:(k + 1) * 128, :], writes=[toks[k]])
    return h, toks


def make_AB(P, vecs, ig, isc, ish):
    A = P.sb("Amod", [128, 16], F32)
    P.I("dve", "tensor_scalar", dict(out=A[:, :], in0=vecs[:, isc, :], scalar1=1.0, scalar2=None, op0=ALU.add), reads=[vecs], writes=[A])
    P.I("dve", "tensor_tensor", dict(out=A[:, :], in0=A[:, :], in1=vecs[:, ig, :], op=ALU.mult), reads=[A, vecs], writes=[A])
    return A


def rstd_from_ps(P, ps, w, inv_n, eps, dst):
    P.I("dve", "tensor_scalar", dict(out=dst[:, :w], in0=ps[:, :w], scalar1=inv_n, scalar2=eps, op0=ALU.mult, op1=ALU.add), reads=[ps], writes=[dst])
    P.I("act", "activation", dict(out=dst[:, :w], in_=dst[:, :w], func=AF.Sqrt), reads=[dst], writes=[dst])
    P.I("dve", "reciprocal", dict(out=dst[:, :w], in_=dst[:, :w]), reads=[dst], writes=[dst])


def normmod(P, C, xh, xtoks, A, vecs, ish, hbf, hbf_toks, S, half_hook=None, cols=None):
    for (c0, c1) in (cols or HALVES):
        w = c1 - c0
        for kc in range(16):
            P.I("act", "activation", dict(out=S.sq[:, kc, :w], in_=xh[:, kc, c0:c1], func=AF.Square), reads=[xtoks[kc]], writes=[S.sq])
        ps = C.ps.next()
        for kc in range(16):
            P.I("pe", "matmul", dict(out=ps[:, :w], lhsT=C.ones_bf[:, :], rhs=S.sq[:, kc, :w], start=(kc == 0), stop=(kc == 15)), reads=[C.ones_bf, S.sq], writes=[ps])
        rstd_from_ps(P, ps, w, 1.0 / 2048, 1e-6, S.rstd)
        for kc in range(16):
            if S.tmp32 is not None:
                dst = S.tmp32[:, kc, :w]
                dt = [S.tmp32_toks[kc]]
            else:
                dst = S.tmpk[kc % 2][:, :w]
                dt = [S.tmpk[kc % 2]]
            P.I("dve", "tensor_tensor", dict(out=dst, in0=xh[:, kc, c0:c1], in1=S.rstd[:, :w], op=ALU.mult), reads=[xtoks[kc], S.rstd], writes=dt)
            if S.tmp32 is not None:
                P.I("dve", "tensor_scalar", dict(out=dst, in0=dst, scalar1=A[:, kc:kc + 1], scalar2=vecs[:, ish, kc:kc + 1], op0=ALU.mult, op1=ALU.add), reads=dt + [A, vecs], writes=dt)
                P.I("act", "copy", dict(out=hbf[:, kc, c0:c1], in_=dst), reads=dt, writes=[hbf_toks[kc]])
            else:
                P.I("dve", "tensor_scalar", dict(out=hbf[:, kc, c0:c1], in0=dst, scalar1=A[:, kc:kc + 1], scalar2=vecs[:, ish, kc:kc + 1], op0=ALU.mult, op1=ALU.add), reads=dt + [A, vecs], writes=[hbf_toks[kc]])
        if half_hook is not None:
            half_hook(c0, c1)


def norm_scratch(P, want32, sw=512):
    S = Ctx()
    S.sq = P.sb("nm_sq", [128, 16, sw], BF16)
    S.rstd = P.sb("nm_rstd", [128, sw], F32)
    if want32:
        S.tmp32 = P.nc.alloc_sbuf_tensor("nm_tmp32", [128, 16, sw], F32)
        S.tmp32_toks = [P.tok(f"tmp32_{k}") for k in range(16)]
    else:
        S.tmp32 = None
        S.tmpk = [P.sb(f"nm_tmpk{i}", [128, 512], F32) for i in range(2)]
    return S


def linear_fm(P, C, W, K, N, src, evac, wring, halves=HALVES, n0=0):
    KC = (K + 127) // 128
    kp = min(K, 128)
    for nb in range(0, N, 512):
        nbw = min(512, N - nb)
        wt = wring.next()
        if K % 128 == 0:
            P.dma("pool", wt[:, :KC, :nbw], W[:, nb:nb + nbw].rearrange("(k p) n -> p k n", p=128), writes=[wt])
        else:
            P.dma("pool", wt[:kp, 0, :nbw], W[:, nb:nb + nbw], writes=[wt])
        for j in range(0, nbw, 128):
            m = min(128, nbw - j)
            for (c0, c1) in halves:
                ps = C.ps.next()
                for kc in range(KC):
                    sap, stoks = src(kc, c0, c1)
                    P.I("pe", "matmul", dict(out=ps[:m, :c1 - c0], lhsT=wt[:kp, kc, j:j + m], rhs=sap, start=(kc == 0), stop=(kc == KC - 1)),
                         reads=[wt] + stoks, writes=[ps])
                evac(n0 + (nb + j) // 128, m, c0, c1, ps)


def build_mod():
    P = Prog()
    c = P.dram_in("c", [1, 2048])
    w = P.dram_in("ada_w", [4, 2048, 1536])
    b = P.dram_in("ada_b", [4, 1536])
    o = P.dram_out("mod", [4, 1536])
    c_sb = P.sb("c_sb", [128, 16])
    sc = P.sb("sc", [128, 16])
    P.dma("sp", c_sb[:, :], c[0, :].rearrange("(k p) -> p k", p=128), writes=[c_sb], allow_slow_non_contiguous=True)
    P.I("act", "activation", dict(out=sc[:, :], in_=c_sb[:, :], func=AF.Silu), reads=[c_sb], writes=[sc])
    bsb = P.sb("bsb", [1, 4 * 1536])
    P.dma("sp", bsb[:, :], b.ap().rearrange("l n -> (l n)")[None, :], writes=[bsb])
    res = P.sb("res", [1, 4 * 1536])
    wt = [P.sb(f"wt{i}", [128, 4, 1536]) for i in range(3)]
    pss = [P.ps(f"ps{i}", [1, 512]) for i in range(6)]
    n = 0
    for l in range(4):
        for kg in range(4):
            t = wt[n % 3]
            n += 1
            P.dma("sp", t[:, :, :], w[l, kg * 512:(kg + 1) * 512, :].rearrange("(k p) n -> p k n", p=128), writes=[t])
            for nb in range(3):
                ps = pss[(l % 2) * 3 + nb]
                for k in range(4):
                    kc = kg * 4 + k
                    P.I("pe", "matmul", dict(out=ps[:, :], lhsT=sc[:, kc:kc + 1], rhs=t[:, k, nb * 512:(nb + 1) * 512], start=(kc == 0), stop=(kc == 15)),
                         reads=[sc, t], writes=[ps])
        for nb in range(3):
            ps = pss[(l % 2) * 3 + nb]
            off = l * 1536 + nb * 512
            P.I("dve", "tensor_tensor", dict(out=res[:, off:off + 512], in0=ps[:, :], in1=bsb[:, off:off + 512], op=ALU.add), reads=[ps, bsb], writes=[res])
    P.dma("sp", o.ap().rearrange("l n -> (l n)")[None, :], res[:, :], reads=[res])
    return P.build()


def moe_inputs(P, L):
    D = Ctx()
    D.rw = P.dram_in("moe_rw", [2048, 32])
    D.rb = P.dram_in("moe_rb", [1, 32])
    D.wgu = P.dram_in("moe_wgu", [32, 2048, 1536])
    D.bgu = P.dram_in("moe_bgu", [128, 32 * 12])
    D.wd = P.dram_in("moe_wd", [32, 768, 2048])
    D.bd = P.dram_in("moe_bd", [32, 2048])
    D.ident = P.dram_in("ident", [128, 128])
    return D


SL128 = [(i * 128, (i + 1) * 128) for i in range(8)]


def moe_tail(P, C, D, xh, xtoks, vecs, iv, wring, hbf, hbt, final_gain_idx=None, out_dram=None):
    A2 = make_AB(P, vecs, iv["gain2"], iv["sc2"], iv["sh2"])
    S = norm_scratch(P, True, 128)
    ident = P.sb("ident", [128, 128], F32)
    P.dma("sp", ident[:, :], D.ident[:, :], writes=[ident])
    rw = P.sb("rw", [128, 16, 32], F32)
    P.dma("sp", rw[:, :, :], D.rw.ap().rearrange("(k p) e -> p k e", p=128), writes=[rw], allow_slow_non_contiguous=True)
    rb = P.sb("rb", [128, 32], F32)
    P.dma("sp", rb[:, :], D.rb.ap().partition_broadcast(128), writes=[rb], allow_slow_non_contiguous=True)
    bgu = P.sb("bgu", [128, 32 * 12], F32)
    P.dma("sp", bgu[:, :], D.bgu[:, :], writes=[bgu])
    bd = P.sb("bd", [32, 2048], F32)
    P.dma("sp", bd[:, :], D.bd[:, :], writes=[bd])
    gT = P.sb("gT", [32, TOK], F32)
    sm = [P.sb(f"rt_sm{i}", [128, 64], F32) for i in range(6)]

    def router(c0, c1):
        for tt in range(c0 // 128, c1 // 128):
            o = tt * 128 - c0
            ps = C.ps.next()
            for kc in range(16):
                P.I("pe", "matmul", dict(out=ps[:, :32], lhsT=S.tmp32[:, kc, o:o + 128], rhs=rw[:, kc, :], start=(kc == 0), stop=(kc == 15)),
                     reads=[S.tmp32_toks[kc], rw], writes=[ps])
            lg, mx, ex, mk, sm_, gt = sm
            P.I("dve", "tensor_tensor", dict(out=lg[:, :32], in0=ps[:, :32], in1=rb[:, :], op=ALU.add), reads=[ps, rb], writes=[lg])
            P.I("dve", "max", dict(out=mx[:, :8], in_=lg[:, :32]), reads=[lg], writes=[mx])
            P.I("dve", "tensor_scalar", dict(out=mk[:, :32], in0=lg[:, :32], scalar1=mx[:, 3:4], scalar2=None, op0=ALU.is_ge), reads=[lg, mx], writes=[mk])
            P.I("dve", "tensor_scalar", dict(out=mx[:, 8:9], in0=mx[:, 0:1], scalar1=-1.0, scalar2=None, op0=ALU.mult), reads=[mx], writes=[mx])
            P.I("act", "activation", dict(out=ex[:, :32], in_=lg[:, :32], func=AF.Exp, bias=mx[:, 8:9], scale=1.0), reads=[lg, mx], writes=[ex])
            P.I("dve", "tensor_tensor", dict(out=ex[:, :32], in0=ex[:, :32], in1=mk[:, :32], op=ALU.mult), reads=[ex, mk], writes=[ex])
            P.I("dve", "reduce_sum", dict(out=sm_[:, 0:1], in_=ex[:, :32], axis=AX.X), reads=[ex], writes=[sm_])
            P.I("dve", "reciprocal", dict(out=sm_[:, 1:2], in_=sm_[:, 0:1]), reads=[sm_], writes=[sm_])
            P.I("dve", "tensor_scalar", dict(out=gt[:, :32], in0=ex[:, :32], scalar1=sm_[:, 1:2], scalar2=None, op0=ALU.mult), reads=[ex, sm_], writes=[gt])
            ps2 = C.ps.next()
            P.I("pe", "matmul", dict(out=ps2[:32, :128], lhsT=gt[:, :32], rhs=ident[:, :], start=True, stop=True), reads=[gt, ident], writes=[ps2])
            P.I("act", "copy", dict(out=gT[:, tt * 128:(tt + 1) * 128], in_=ps2[:32, :128]), reads=[ps2], writes=[gT])

    normmod(P, C, xh, xtoks, A2, vecs, iv["sh2"], hbf, hbt, S, half_hook=router, cols=SL128)

    Ge = [P.sb(f"Ge{i}", [128, TOK], F32) for i in range(1)]
    hT = [P.nc.alloc_sbuf_tensor(f"hT{i}", [128, 6, TOK], BF16) for i in range(1)]
    hTt = [[P.tok(f"hT{i}_{f}") for f in range(6)] for i in range(1)]
    sg = Ring([P.sb(f"sw_g{i}", [128, 512], F32) for i in range(1)])
    ss = Ring([P.sb(f"sw_s{i}", [128, 512], F32) for i in range(1)])
    su = Ring([P.sb(f"sw_u{i}", [128, 512], F32) for i in range(1)])
    g2i = iv["g2"]

    def src_h(kc, c0, c1):
        return hbf[:, kc, c0:c1], [hbt[kc]]

    for ex in range(32):
        G = Ge[0]
        hTe = hT[0]
        hTet = hTt[0]
        for (c0, c1) in HALVES:
            ps = C.ps.next()
            P.I("pe", "matmul", dict(out=ps[:, :], lhsT=ident[:32, ex:ex + 1].to_broadcast([32, 128]), rhs=gT[:, c0:c1], start=True, stop=True), reads=[ident, gT], writes=[ps])
            P.I("act", "copy", dict(out=G[:, c0:c1], in_=ps[:, :]), reads=[ps], writes=[G])
        for blk in range(3):
            wt = wring.next()
            P.dma("pool", wt[:, :, :], D.wgu[ex, :, blk * 512:(blk + 1) * 512].rearrange("(k p) n -> p k n", p=128), writes=[wt])
            for c2 in range(2):
                fc = blk * 2 + c2
                for (c0, c1) in HALVES:
                    psg = C.ps.next()
                    psu = C.ps.next()
                    for kc in range(16):
                        P.I("pe", "matmul", dict(out=psg[:, :], lhsT=wt[:, kc, c2 * 256:c2 * 256 + 256:2], rhs=hbf[:, kc, c0:c1], start=(kc == 0), stop=(kc == 15)),
                             reads=[wt, hbt[kc]], writes=[psg])
                    for kc in range(16):
                        P.I("pe", "matmul", dict(out=psu[:, :], lhsT=wt[:, kc, c2 * 256 + 1:c2 * 256 + 256:2], rhs=hbf[:, kc, c0:c1], start=(kc == 0), stop=(kc == 15)),
                             reads=[wt, hbt[kc]], writes=[psu])
                    bi = (ex * 6 + fc) * 2
                    tg, ts, tu = sg.next(), ss.next(), su.next()
                    P.I("dve", "tensor_scalar", dict(out=tg[:, :], in0=psg[:, :], scalar1=bgu[:, bi:bi + 1], scalar2=7.0, op0=ALU.add, op1=ALU.min), reads=[psg, bgu], writes=[tg])
                    P.I("act", "activation", dict(out=ts[:, :], in_=tg[:, :], func=AF.Sigmoid, scale=1.702), reads=[tg], writes=[ts])
                    P.I("dve", "tensor_scalar", dict(out=tu[:, :], in0=psu[:, :], scalar1=bgu[:, bi + 1:bi + 2], scalar2=7.0, op0=ALU.add, op1=ALU.min), reads=[psu, bgu], writes=[tu])
                    P.I("pool", "tensor_scalar", dict(out=tu[:, :], in0=tu[:, :], scalar1=-7.0, scalar2=1.0, op0=ALU.max, op1=ALU.add), reads=[tu], writes=[tu])
                    P.I("pool", "tensor_tensor", dict(out=tg[:, :], in0=tg[:, :], in1=ts[:, :], op=ALU.mult), reads=[tg, ts], writes=[tg])
                    P.I("dve", "tensor_tensor", dict(out=tu[:, :], in0=tu[:, :], in1=tg[:, :], op=ALU.mult), reads=[tg, tu], writes=[tu])
                    P.I("pool", "tensor_tensor", dict(out=hTe[:, fc, c0:c1], in0=tu[:, :], in1=G[:, c0:c1], op=ALU.mult), reads=[tu, G], writes=[hTet[fc]])
        for blk in range(4):
            wt = wring.next()
            P.dma("pool", wt[:, :6, :], D.wd[ex, :, blk * 512:(blk + 1) * 512].rearrange("(k p) n -> p k n", p=128), writes=[wt])
            for j in range(4):
                n = blk * 4 + j
                for (c0, c1) in HALVES:
                    ps = C.ps.next()
                    for f in range(6):
                        P.I("pe", "matmul", dict(out=ps[:, :], lhsT=wt[:, f, j * 128:(j + 1) * 128], rhs=hTe[:, f, c0:c1], start=(f == 0), stop=(f == 5)),
                             reads=[wt, hTet[f]], writes=[ps])
                    P.I("dve", "scalar_tensor_tensor", dict(out=xh[:, n, c0:c1], in0=ps[:, :], scalar=vecs[:, g2i, n:n + 1], in1=xh[:, n, c0:c1], op0=ALU.mult, op1=ALU.add),
                         reads=[ps, vecs, xtoks[n]], writes=[xtoks[n]])
    for n in range(16):
        for (c0, c1) in HALVES:
            ps = C.ps.next()
            P.I("pe", "matmul", dict(out=ps[:, :], lhsT=bd[:, n * 128:(n + 1) * 128], rhs=gT[:, c0:c1], start=True, stop=True), reads=[bd, gT], writes=[ps])
            P.I("dve", "scalar_tensor_tensor", dict(out=xh[:, n, c0:c1], in0=ps[:, :], scalar=vecs[:, g2i, n:n + 1], in1=xh[:, n, c0:c1], op0=ALU.mult, op1=ALU.add),
                 reads=[ps, vecs, xtoks[n]], writes=[xtoks[n]])
    if final_gain_idx is not None:
        for (c0, c1) in SL128:
            w = c1 - c0
            for kc in range(16):
                P.I("act", "activation", dict(out=S.sq[:, kc, :w], in_=xh[:, kc, c0:c1], func=AF.Square), reads=[xtoks[kc]], writes=[S.sq])
            ps = C.ps.next()
            for kc in range(16):
                P.I("pe", "matmul", dict(out=ps[:, :w], lhsT=C.ones_bf[:, :], rhs=S.sq[:, kc, :w], start=(kc == 0), stop=(kc == 15)), reads=[C.ones_bf, S.sq], writes=[ps])
            rstd_from_ps(P, ps, w, 1.0 / 2048, 1e-6, S.rstd)
            for kc in range(16):
                P.I("dve", "scalar_tensor_tensor", dict(out=xh[:, kc, c0:c1], in0=xh[:, kc, c0:c1], scalar=vecs[:, final_gain_idx, kc:kc + 1], in1=S.rstd[:, :w], op0=ALU.mult, op1=ALU.mult),
                     reads=[xtoks[kc], vecs, S.rstd], writes=[xtoks[kc]])
    for kc in range(16):
        P.dma("sp", out_dram[kc * 128:(kc + 1) * 128, :], xh[:, kc, :], reads=[xtoks[kc]])


def build_t1m():
    P = Prog()
    C = setup_common(P)
    x = P.dram_in("xT", [2048, TOK])
    vd = P.dram_in("vecs", [128, 3, 16])
    w = P.dram_in("w_in", [2048, 6152])
    bgd = P.dram_in("b_gates", [1, 8])
    trid = P.dram_in("triu", [128, 128])
    qkv = P.dram_out("qkvT", [4096, TOK])
    og = P.dram_out("ogT", [2048, TOK])
    ab = P.dram_out("ab", [TOK, 12])
    xh, xt = load_x(P, x)
    vecs = P.sb("vecs", [128, 3, 16])
    P.dma("sp", vecs[:, :, :], vd[:, :, :], writes=[vecs])
    A = make_AB(P, vecs, 0, 1, 2)
    S = norm_scratch(P, False)
    hbf = P.nc.alloc_sbuf_tensor("hbf", [128, 16, TOK], BF16)
    hbt = [P.tok(f"hbf{k}") for k in range(16)]
    normmod(P, C, xh, xt, A, vecs, 2, hbf, hbt, S)
    wring = Ring([P.sb(f"wt{i}", [128, 16, 512], BF16) for i in range(2)])
    stg = Ring([P.sb(f"stg{i}", [128, 512], F32) for i in range(4)])

    def src(kc, c0, c1):
        return hbf[:, kc, c0:c1], [hbt[kc]]

    def evac(n, m, c0, c1, ps):
        t = stg.next()
        if n < 8:
            P.I("act", "mul", dict(out=t[:, :], in_=ps[:, :], mul=1.0 / 16.0), reads=[ps], writes=[t])
        elif n < 32:
            P.I("dve", "tensor_copy", dict(out=t[:, :], in_=ps[:, :]), reads=[ps], writes=[t])
        else:
            P.I("act", "activation", dict(out=t[:, :], in_=ps[:, :], func=AF.Sigmoid), reads=[ps], writes=[t])
        if n < 32:
            P.dma("sp", qkv[n * 128:(n + 1) * 128, c0:c1], t[:, :], reads=[t])
        else:
            P.dma("sp", og[(n - 32) * 128:(n - 31) * 128, c0:c1], t[:, :], reads=[t])

    linear_fm(P, C, w[:, 0:6144], 2048, 6144, src, evac, wring)
    wg = P.sb("wg", [128, 16, 8], BF16)
    P.dma("pool", wg[:, :, :], w[:, 6144:6152].rearrange("(k p) n -> p k n", p=128), writes=[wg], allow_slow_non_contiguous=True)
    bg = P.sb("bg", [128, 8], F32)
    P.dma("sp", bg[:, :], bgd.ap().partition_broadcast(128), writes=[bg], allow_slow_non_contiguous=True)
    tri = P.sb("tri", [128, 128], F32)
    P.dma("sp", tri[:, :], trid[:, :], writes=[tri])
    for tt in range(8):
        ps = C.ps.next()
        for kc in range(16):
            P.I("pe", "matmul", dict(out=ps[:, :8], lhsT=hbf[:, kc, tt * 128:(tt + 1) * 128], rhs=wg[:, kc, :], start=(kc == 0), stop=(kc == 15)), reads=[hbt[kc], wg], writes=[ps])
        g = P.sb(f"gt{tt}", [128, 40], F32)
        P.I("dve", "tensor_tensor", dict(out=g[:, 0:8], in0=ps[:, :8], in1=bg[:, :], op=ALU.add), reads=[ps, bg], writes=[g])
        P.I("act", "activation", dict(out=g[:, 0:8], in_=g[:, 0:8], func=AF.Tanh, scale=1.0 / 15.0), reads=[g], writes=[g])
        P.I("act", "activation", dict(out=g[:, 8:12], in_=g[:, 4:8], func=AF.Exp, scale=-15.0), reads=[g], writes=[g])
        P.I("dve", "tensor_scalar", dict(out=g[:, 8:12], in0=g[:, 8:12], scalar1=1.0, scalar2=None, op0=ALU.add), reads=[g], writes=[g])
        P.I("act", "activation", dict(out=g[:, 8:12], in_=g[:, 8:12], func=AF.Ln), reads=[g], writes=[g])
        ps2 = C.ps.next()
        P.I("pe", "matmul", dict(out=ps2[:, 0:4], lhsT=tri[:, :], rhs=g[:, 8:12], start=True, stop=True), reads=[tri, g], writes=[ps2])
        ps3 = C.ps.next()
        P.I("pe", "matmul", dict(out=ps3[:, 0:4], lhsT=C.ones_f[:, :], rhs=g[:, 8:12], start=True, stop=True), reads=[C.ones_f, g], writes=[ps3])
        P.I("act", "activation", dict(out=g[:, 16:20], in_=ps2[:, 0:4], func=AF.Exp, scale=-1.0), reads=[ps2], writes=[g])
        P.I("dve", "scalar_tensor_tensor", dict(out=g[:, 12:16], in0=g[:, 0:4], scalar=15.0, in1=ps2[:, 0:4], op0=ALU.mult, op1=ALU.add), reads=[ps2, g], writes=[g])
        P.I("act", "activation", dict(out=g[:, 20:24], in_=g[:, 12:16], func=AF.Exp), reads=[g], writes=[g])
        P.I("act", "activation", dict(out=g[:, 24:28], in_=ps3[:, 0:4], func=AF.Exp, scale=-1.0), reads=[ps3], writes=[g])
        P.dma("sp", ab[tt * 128:(tt + 1) * 128, :], g[:, 16:28], reads=[g])
    return P.build()


def build_mixm():
    P = Prog()
    C = setup_common(P)
    S_ = 8192
    qd = P.dram_in("qT", [256, S_])
    kd = P.dram_in("kT", [256, S_])
    ktd = P.dram_in("ktok", [S_, 256])
    vtd = P.dram_in("vtok", [S_, 256])
    abd = P.dram_in("abg", [128, 192])
    trid = P.dram_in("triu", [128, 128])
    out = P.dram_out("hh", [S_, 256])
    NP = 8
    pw = S_ // NP
    qr = [P.sb(f"qb{i}", [128, 2, pw], F32) for i in range(2)]
    kr = [P.sb(f"kb{i}", [128, 2, pw], F32) for i in range(2)]
    ktr = [P.sb(f"ktb{i}", [128, 8, 256], F32) for i in range(2)]
    vr = [P.sb(f"vb{i}", [128, 8, 256], F32) for i in range(2)]
    abg = P.sb("abg", [128, 192], F32)
    P.dma("sp", abg[:, :], abd[:, :], writes=[abg])
    tri = P.sb("tri", [128, 128], F32)
    P.dma("sp", tri[:, :], trid[:, :], writes=[tri])
    C32 = P.sb("C32", [128, 2, 256], F32)
    n32 = P.sb("n32", [128, 2], F32)
    for t_ in (C32, n32):
        P.I("dve", "memset", dict(ap=t_[:], constant=0.0), writes=[t_])
    onec = P.sb("onec", [128, 1], F32)
    P.I("dve", "memset", dict(ap=onec[:, :], constant=1.0), writes=[onec])
    STr = Ring([P.sb(f"ST{i}", [128, 128], F32) for i in range(2)])
    kpr = Ring([P.sb(f"kp{i}", [128, 256], F32) for i in range(2)])
    smr = Ring([P.sb(f"sm{i}", [128, 4], F32) for i in range(2)])
    hor = Ring([P.sb(f"ho{i}", [128, 256], F32) for i in range(2)])
    tmpc = P.sb("tmpc", [128, 2, 256], F32)
    for n in range(64):
        pi = n // 8
        ci = n % 8
        qb, kb, ktb, vb = qr[pi % 2], kr[pi % 2], ktr[pi % 2], vr[pi % 2]
        if ci == 0:
            P.dma("sp", qb[:, :, :], qd[:, pi * pw:(pi + 1) * pw].rearrange("(k p) t -> p k t", p=128), writes=[qb])
            P.dma("sp", kb[:, :, :], kd[:, pi * pw:(pi + 1) * pw].rearrange("(k p) t -> p k t", p=128), writes=[kb])
            P.dma("sp", ktb[:, :, :], ktd[pi * pw:(pi + 1) * pw, :].rearrange("(n j) d -> j n d", j=128), writes=[ktb])
            P.dma("sp", vb[:, :, :], vtd[pi * pw:(pi + 1) * pw, :].rearrange("(n j) d -> j n d", j=128), writes=[vb])
        a_ap = abg[:, n * 3 + 0:n * 3 + 1]
        b_ap = abg[:, n * 3 + 1:n * 3 + 2]
        G_ap = abg[:, n * 3 + 2:n * 3 + 3]
        sl = slice(ci * 128, (ci + 1) * 128)
        osl = slice(n * 128, (n + 1) * 128)
        ps = C.ps.next()
        for kc in range(2):
            P.I("pe", "matmul", dict(out=ps[:, :128], lhsT=kb[:, kc, sl], rhs=qb[:, kc, sl], start=(kc == 0), stop=(kc == 1)), reads=[kb, qb], writes=[ps])
        ST = STr.next()
        P.I("dve", "scalar_tensor_tensor", dict(out=ST[:, :], in0=ps[:, :128], scalar=b_ap, in1=tri[:, :], op0=ALU.mult, op1=ALU.mult), reads=[ps, abg, tri], writes=[ST])
        pn = C.ps.next()
        P.I("pe", "matmul", dict(out=pn[:, :256], lhsT=ST[:, :], rhs=vb[:, ci, :], start=True, stop=False), reads=[ST, vb], writes=[pn])
        for kc in range(2):
            P.I("pe", "matmul", dict(out=pn[:, :256], lhsT=qb[:, kc, sl], rhs=C32[:, kc, :], start=False, stop=(kc == 1)), reads=[qb, C32], writes=[pn])
        pd = C.ps.next()
        P.I("pe", "matmul", dict(out=pd[:, :1], lhsT=ST[:, :], rhs=onec[:, :], start=True, stop=False), reads=[ST, onec], writes=[pd])
        for kc in range(2):
            P.I("pe", "matmul", dict(out=pd[:, :1], lhsT=qb[:, kc, sl], rhs=n32[:, kc:kc + 1], start=False, stop=(kc == 1)), reads=[qb, n32], writes=[pd])
        sm = smr.next()
        P.I("act", "activation", dict(out=sm[:, 3:4], in_=pd[:, :1], func=AF.Abs, scale=a_ap), reads=[pd, abg], writes=[sm])
        P.I("dve", "tensor_scalar", dict(out=sm[:, 0:1], in0=sm[:, 3:4], scalar1=1.0, scalar2=None, op0=ALU.max), reads=[sm], writes=[sm])
        P.I("dve", "reciprocal", dict(out=sm[:, 1:2], in_=sm[:, 0:1]), reads=[sm], writes=[sm])
        P.I("dve", "tensor_tensor", dict(out=sm[:, 2:3], in0=sm[:, 1:2], in1=a_ap, op=ALU.mult), reads=[sm, abg], writes=[sm])
        ho = hor.next()
        P.I("act", "mul", dict(out=ho[:, :], in_=pn[:, :256], mul=sm[:, 2:3]), reads=[pn, sm], writes=[ho])
        P.dma("sp", out[osl, :], ho[:, :], reads=[ho])
        kp = kpr.next()
        P.I("dve", "tensor_scalar", dict(out=kp[:, :], in0=ktb[:, ci, :], scalar1=b_ap, scalar2=None, op0=ALU.mult), reads=[ktb, abg], writes=[kp])
        for kc in range(2):
            pc = C.ps.next()
            P.I("pe", "matmul", dict(out=pc[:, :256], lhsT=kp[:, kc * 128:(kc + 1) * 128], rhs=vb[:, ci, :], start=True, stop=True), reads=[kp, vb], writes=[pc])
            P.I("pe", "matmul", dict(out=pc[:, 256:257], lhsT=kp[:, kc * 128:(kc + 1) * 128], rhs=onec[:, :], start=True, stop=True), reads=[kp, onec], writes=[pc])
            P.I("dve", "tensor_tensor", dict(out=tmpc[:, kc, :], in0=C32[:, kc, :], in1=pc[:, :256], op=ALU.add), reads=[pc, C32], writes=[tmpc])
            P.I("dve", "tensor_scalar", dict(out=C32[:, kc, :], in0=tmpc[:, kc, :], scalar1=G_ap, scalar2=None, op0=ALU.mult), reads=[tmpc, abg], writes=[C32])
            P.I("dve", "tensor_tensor", dict(out=n32[:, kc:kc + 1], in0=n32[:, kc:kc + 1], in1=pc[:, 256:257], op=ALU.add), reads=[pc, n32], writes=[n32])
            P.I("dve", "tensor_scalar", dict(out=n32[:, kc:kc + 1], in0=n32[:, kc:kc + 1], scalar1=G_ap, scalar2=None, op0=ALU.mult), reads=[n32, abg], writes=[n32])
    return P.build()


def t2_common_inputs(P, nv):
    x = P.dram_in("xT", [2048, TOK])
    vd = P.dram_in("vecs", [128, nv, 16])
    out = P.dram_out("xout", [2048, TOK])
    xh, xt = load_x(P, x)
    vecs = P.sb("vecs", [128, nv, 16])
    P.dma("sp", vecs[:, :, :], vd[:, :, :], writes=[vecs])
    return xh, xt, vecs, out


def outproj_residual(P, C, wo, zbf, zt, xh, xt, vecs, ig1, wring):
    def src(kc, c0, c1):
        return zbf[:, kc, c0:c1], [zt[kc]]

    def evac(n, m, c0, c1, ps):
        P.I("dve", "scalar_tensor_tensor", dict(out=xh[:, n, c0:c1], in0=ps[:, :], scalar=vecs[:, ig1, n:n + 1], in1=xh[:, n, c0:c1], op0=ALU.mult, op1=ALU.add),
             reads=[ps, vecs, xt[n]], writes=[xt[n]])

    linear_fm(P, C, wo[:, :], 2048, 2048, src, evac, wring)


def build_t2m(final):
    P = Prog()
    C = setup_common(P)
    xh, xt, vecs, out = t2_common_inputs(P, 7)
    hhd = P.dram_in("hhT", [2048, TOK])
    ogd = P.dram_in("ogT", [2048, TOK])
    wo = P.dram_in("w_out", [2048, 2048])
    D = moe_inputs(P, 0)
    wring = Ring([P.sb(f"wt{i}", [128, 16, 512], BF16) for i in range(2)])
    zbf = P.nc.alloc_sbuf_tensor("zbf", [128, 16, TOK], BF16)
    zt = [P.tok(f"z{k}") for k in range(16)]
    hr = Ring([P.sb(f"hh{i}", [128, TOK], F32) for i in range(2)])
    orr = Ring([P.sb(f"og{i}", [128, TOK], F32) for i in range(2)])
    rstd = P.sb("rstd_h", [128, TOK], F32)
    for h in range(4):
        pss = [C.ps.next(), C.ps.next()]
        for k4 in range(4):
            kc = h * 4 + k4
            t = hr.next()
            P.dma("sp", t[:, :], hhd[kc * 128:(kc + 1) * 128, :], writes=[t])
            P.I("act", "activation", dict(out=zbf[:, kc, :], in_=t[:, :], func=AF.Square), reads=[t], writes=[zt[kc]])
            for hi, (c0, c1) in enumerate(HALVES):
                P.I("pe", "matmul", dict(out=pss[hi][:, :], lhsT=C.ones_bf[:, :], rhs=zbf[:, kc, c0:c1], start=(k4 == 0), stop=(k4 == 3)), reads=[C.ones_bf, zt[kc]], writes=[pss[hi]])
        for hi, (c0, c1) in enumerate(HALVES):
            ps = pss[hi]
            P.I("dve", "tensor_scalar", dict(out=rstd[:, c0:c1], in0=ps[:, :], scalar1=1.0 / 512, scalar2=1e-6, op0=ALU.mult, op1=ALU.add), reads=[ps], writes=[rstd])
        P.I("act", "activation", dict(out=rstd[:, :], in_=rstd[:, :], func=AF.Sqrt), reads=[rstd], writes=[rstd])
        P.I("dve", "reciprocal", dict(out=rstd[:, :], in_=rstd[:, :]), reads=[rstd], writes=[rstd])
        for k4 in range(4):
            kc = h * 4 + k4
            t = hr.next()
            o = orr.next()
            P.dma("sp", t[:, :], hhd[kc * 128:(kc + 1) * 128, :], writes=[t])
            P.dma("sp", o[:, :], ogd[kc * 128:(kc + 1) * 128, :], writes=[o])
            P.I("dve", "scalar_tensor_tensor", dict(out=t[:, :], in0=t[:, :], scalar=vecs[:, 0, kc:kc + 1], in1=rstd[:, :], op0=ALU.mult, op1=ALU.mult), reads=[t, vecs, rstd], writes=[t])
            P.I("pool", "tensor_tensor", dict(out=zbf[:, kc, :], in0=t[:, :], in1=o[:, :], op=ALU.mult), reads=[t, o], writes=[zt[kc]])
    outproj_residual(P, C, wo, zbf, zt, xh, xt, vecs, 1, wring)
    iv = dict(gain2=2, sc2=3, sh2=4, g2=5)
    moe_tail(P, C, D, xh, xt, vecs, iv, wring, zbf, zt, final_gain_idx=(6 if final else None), out_dram=out)
    return P.build()


_PROGS = {}
DEBUG = {}


def _prog(name, builder, *a):
    key = (name,) + a
    if key not in _PROGS:
        _PROGS[key] = builder(*a)
    return _PROGS[key]


def _run(nc, in_maps):
    res = run_bass_kernel_spmd(nc, in_maps, core_ids=list(range(NCORES)))
    return res.results


def _c(a):
    return np.ascontiguousarray(a, dtype=np.float32)


def vlay(vs):
    return _c(np.stack([np.asarray(v, np.float32).reshape(16, 128).T for v in vs], axis=1))


def _consts():
    return dict(triu=_c(np.triu(np.ones((128, 128), np.float32))), ident=_c(np.eye(128, dtype=np.float32)))


def run_mod(inp):
    nc = _prog("mod", build_mod)
    w = inp["ada_w"]
    b = inp["ada_b"]
    maps = [{"c": _c(inp["c"]), "ada_w": _c(w[:, :, i * 1536:(i + 1) * 1536]), "ada_b": _c(b[:, i * 1536:(i + 1) * 1536])} for i in range(NCORES)]
    r = _run(nc, maps)
    mod = np.concatenate([x["mod"] for x in r], axis=1)
    return mod.reshape(4, 6, 2048)


def moe_maps(inp, layer):
    bgu = inp["moe_b_gate_up"][layer]
    bgu_l = _c(bgu.reshape(32, 6, 128, 2).transpose(2, 0, 1, 3).reshape(128, 32 * 12))
    return dict(moe_rw=_c(inp["moe_router_w"][layer]), moe_rb=_c(inp["moe_router_b"][layer][None, :]), moe_wgu=_c(inp["moe_w_gate_up"][layer]),
                moe_bgu=bgu_l, moe_wd=_c(inp["moe_w_down"][layer]), moe_bd=_c(inp["moe_b_down"][layer]), ident=_consts()["ident"])


def layer_mlstm(inp, xs, mod, layer, j, final):
    sh1, sc1, g1, sh2, sc2, g2 = [mod[layer, i] for i in range(6)]
    cst = _consts()
    nc1 = _prog("t1m", build_t1m)
    v1 = vlay([inp["norm_gain"][layer, 0], sc1, sh1])
    w_in = _c(inp["mlstm_w_in"][j])
    bg = _c(inp["mlstm_b_gates"][j][None, :])
    r1 = _run(nc1, [dict(xT=xs[c], vecs=v1, w_in=w_in, b_gates=bg, triu=cst["triu"]) for c in range(NCORES)])
    qkvT = np.concatenate([r["qkvT"] for r in r1], axis=1)
    ab = np.concatenate([r["ab"] for r in r1], axis=0)
    if "on" in DEBUG:
        DEBUG["qkvT"] = qkvT; DEBUG["ab"] = ab; DEBUG["ogT"] = np.concatenate([r["ogT"] for r in r1], axis=1)
    maps = []
    for c in range(NCORES):
        h, half = c // 2, c % 2
        vrows = qkvT[2048 + h * 512 + half * 256: 2048 + h * 512 + half * 256 + 256]
        abh = np.stack([ab[:, h], ab[:, 4 + h], ab[:, 8 + h]], axis=1)
        abg = _c(abh.reshape(64, 128, 3).transpose(1, 0, 2).reshape(128, 192))
        maps.append(dict(qT=_c(qkvT[h * 256:(h + 1) * 256]), kT=_c(qkvT[1024 + h * 256:1024 + (h + 1) * 256]),
                         ktok=_c(qkvT[1024 + h * 256:1024 + (h + 1) * 256].T), vtok=_c(vrows.T), abg=abg, triu=cst["triu"]))
    r2 = _run(_prog("mixm", build_mixm), maps)
    hhT = np.concatenate([r["hh"].T for r in r2], axis=0)
    if "on" in DEBUG:
        DEBUG["hhT"] = hhT
        if "stop_after_mix" in DEBUG:
            return None
    v2 = vlay([inp["mlstm_norm_gain"][j], g1, inp["norm_gain"][layer, 1], sc2, sh2, g2, inp["final_gain"]])
    mm = moe_maps(inp, layer)
    wo = _c(inp["mlstm_w_out"][j])
    maps = [dict(xT=xs[c], vecs=v2, hhT=_c(hhT[:, c * TOK:(c + 1) * TOK]), ogT=r1[c]["ogT"], w_out=wo, **mm) for c in range(NCORES)]
    r3 = _run(_prog("t2m", build_t2m, final), maps)
    return [r["xout"] for r in r3]


import math
LAM_INIT2 = 0.8 - 0.6 * math.exp(-0.3 * 2)


def build_t1d():
    P = Prog()
    C = setup_common(P)
    x = P.dram_in("xT", [2048, TOK])
    vd = P.dram_in("vecs", [128, 3, 16])
    w = P.dram_in("w_qkv", [2048, 6144])
    posd = P.dram_in("pos", [1, TOK], I32)
    invfd = P.dram_in("invf", [128, 1])
    rotd = P.dram_in("rotT", [128, 128])
    qk = P.dram_out("qkT", [4096, TOK])
    vo = P.dram_out("vT", [2048, TOK])
    xh, xt = load_x(P, x)
    vecs = P.sb("vecs", [128, 3, 16])
    P.dma("sp", vecs[:, :, :], vd[:, :, :], writes=[vecs])
    A = make_AB(P, vecs, 0, 1, 2)
    S = norm_scratch(P, False)
    hbf = P.nc.alloc_sbuf_tensor("hbf", [128, 16, TOK], BF16)
    hbt = [P.tok(f"hbf{k}") for k in range(16)]
    normmod(P, C, xh, xt, A, vecs, 2, hbf, hbt, S)
    posi = P.sb("posi", [128, TOK], I32)
    P.dma("sp", posi[:, :], posd.ap().partition_broadcast(128), writes=[posi], allow_slow_non_contiguous=True)
    invf = P.sb("invf", [128, 1], F32)
    P.dma("sp", invf[:, :], invfd[:, :], writes=[invf])
    rot = P.sb("rot", [128, 128], F32)
    P.dma("sp", rot[:, :], rotd[:, :], writes=[rot])
    ang = P.sb("ang", [128, TOK], F32)
    cos = P.sb("cos", [128, TOK], F32)
    sin = P.sb("sin", [128, TOK], F32)
    P.I("dve", "tensor_copy", dict(out=ang[:, :], in_=posi[:, :]), reads=[posi], writes=[ang])
    P.I("dve", "tensor_scalar", dict(out=ang[:, :], in0=ang[:, :], scalar1=invf[:, 0:1], scalar2=None, op0=ALU.mult), reads=[ang, invf], writes=[ang])
    pit = P.sb("pit", [128, 1], F32)
    P.I("dve", "memset", dict(ap=pit[:, :], constant=math.pi), writes=[pit])
    ki = P.sb("ki", [128, TOK], I32)
    kf = P.sb("kf", [128, TOK], F32)
    for dst, off in ((sin, 0.0), (cos, 0.25)):
        P.I("dve", "tensor_scalar", dict(out=dst[:, :], in0=ang[:, :], scalar1=off, scalar2=None, op0=ALU.add), reads=[ang], writes=[dst])
        P.I("dve", "tensor_copy", dict(out=ki[:, :], in_=dst[:, :]), reads=[dst], writes=[ki])
        P.I("dve", "tensor_copy", dict(out=kf[:, :], in_=ki[:, :]), reads=[ki], writes=[kf])
        P.I("dve", "tensor_tensor", dict(out=dst[:, :], in0=dst[:, :], in1=kf[:, :], op=ALU.subtract), reads=[dst, kf], writes=[dst])
        P.I("dve", "tensor_scalar", dict(out=kf[:, :], in0=dst[:, :], scalar1=0.0, scalar2=None, op0=ALU.is_lt), reads=[dst], writes=[kf])
        P.I("dve", "tensor_tensor", dict(out=dst[:, :], in0=dst[:, :], in1=kf[:, :], op=ALU.add), reads=[dst, kf], writes=[dst])
        P.I("act", "activation", dict(out=dst[:, :], in_=dst[:, :], func=AF.Sin, bias=pit[:, 0:1], scale=-2.0 * math.pi), reads=[dst, pit], writes=[dst])
    wring = Ring([P.sb(f"wt{i}", [128, 16, 512], BF16) for i in range(2)])
    stg = Ring([P.sb(f"stg{i}", [128, 512], F32) for i in range(4)])
    xbr = Ring([P.sb(f"xb{i}", [128, 512], F32) for i in range(2)])
    t2r = Ring([P.sb(f"t2{i}", [128, 512], F32) for i in range(2)])

    def src(kc, c0, c1):
        return hbf[:, kc, c0:c1], [hbt[kc]]

    def evac(n, m, c0, c1, ps):
        t = stg.next()
        if n < 32:
            xb = xbr.next()
            if n < 16:
                P.I("act", "mul", dict(out=xb[:, :], in_=ps[:, :], mul=128.0 ** -0.5), reads=[ps], writes=[xb])
            else:
                P.I("act", "copy", dict(out=xb[:, :], in_=ps[:, :]), reads=[ps], writes=[xb])
            ps2 = C.ps.next()
            P.I("pe", "matmul", dict(out=ps2[:, :], lhsT=rot[:, :], rhs=xb[:, :], start=True, stop=True), reads=[rot, xb], writes=[ps2])
            t2 = t2r.next()
            P.I("dve", "tensor_tensor", dict(out=t2[:, :], in0=ps2[:, :], in1=sin[:, c0:c1], op=ALU.mult), reads=[ps2, sin], writes=[t2])
            P.I("dve", "tensor_tensor", dict(out=t[:, :], in0=xb[:, :], in1=cos[:, c0:c1], op=ALU.mult), reads=[xb, cos], writes=[t])
            P.I("dve", "tensor_tensor", dict(out=t[:, :], in0=t[:, :], in1=t2[:, :], op=ALU.add), reads=[t, t2], writes=[t])
            P.dma("sp", qk[n * 128:(n + 1) * 128, c0:c1], t[:, :], reads=[t])
        else:
            P.I("act", "copy", dict(out=t[:, :], in_=ps[:, :]), reads=[ps], writes=[t])
            P.dma("sp", vo[(n - 32) * 128:(n - 31) * 128, c0:c1], t[:, :], reads=[t])

    linear_fm(P, C, w[:, :], 2048, 6144, src, evac, wring)
    return P.build()


def build_mixd():
    P = Prog()
    C = setup_common(P, npsum=4)
    S_ = 8192
    qd = P.dram_in("qT", [256, S_])
    kd = P.dram_in("kT", [256, S_])
    vtd = P.dram_in("vtok", [S_, 256])
    lamd = P.dram_in("lam", [1, 512])
    sgd = P.dram_in("sgain", [1, 256])
    idd = P.dram_in("ident", [128, 128])
    mkd = P.dram_in("maskb", [128, 128])
    out = P.dram_out("attn", [S_, 256])
    scale = 128.0 ** -0.5
    qb = P.nc.alloc_sbuf_tensor("qb", [128, 2, S_], BF16)
    kb = P.nc.alloc_sbuf_tensor("kb", [128, 2, S_], BF16)
    vb = P.nc.alloc_sbuf_tensor("vb", [128, 64, 256], BF16)
    NP = 8
    pw = S_ // NP
    qt = [P.tok(f"q{i}") for i in range(NP)]
    kt = [P.tok(f"k{i}") for i in range(NP)]
    vt = [P.tok(f"v{i}") for i in range(NP)]
    for i in range(NP):
        P.dma("pool", qb[:, :, i * pw:(i + 1) * pw], qd[:, i * pw:(i + 1) * pw].rearrange("(k p) t -> p k t", p=128), writes=[qt[i]])
        P.dma("pool", kb[:, :, i * pw:(i + 1) * pw], kd[:, i * pw:(i + 1) * pw].rearrange("(k p) t -> p k t", p=128), writes=[kt[i]])
        P.dma("pool", vb[:, i * 8:(i + 1) * 8, :], vtd[i * pw:(i + 1) * pw, :].rearrange("(n j) d -> j n d", j=128), writes=[vt[i]])
    identb = P.sb("identb", [128, 128], BF16)
    P.dma("pool", identb[:, :], idd[:, :], writes=[identb])
    maskb = P.sb("maskb", [128, 128], F32)
    P.dma("sp", maskb[:, :], mkd[:, :], writes=[maskb])
    sg = P.sb("sg", [128, 256], F32)
    P.dma("sp", sg[:, :], sgd.ap().partition_broadcast(128), writes=[sg], allow_slow_non_contiguous=True)
    P.I("dve", "tensor_scalar", dict(out=sg[:, :], in0=sg[:, :], scalar1=1.0 - LAM_INIT2, scalar2=None, op0=ALU.mult), reads=[sg], writes=[sg])
    lp = P.sb("lp", [1, 4, 128], F32)
    P.dma("sp", lp[:, :, :], lamd.ap().rearrange("o (a b) -> o a b", a=4), writes=[lp])
    pr = P.sb("pr", [1, 2, 128], F32)
    P.I("dve", "tensor_tensor", dict(out=pr[:, :, :], in0=lp[:, 0::2, :], in1=lp[:, 1::2, :], op=ALU.mult), reads=[lp], writes=[pr])
    l2 = P.sb("l2", [1, 4], F32)
    P.I("dve", "reduce_sum", dict(out=l2[:, 0:2], in_=pr[:, :, :], axis=AX.X), reads=[pr], writes=[l2])
    P.I("act", "activation", dict(out=l2[:, 0:2], in_=l2[:, 0:2], func=AF.Exp), reads=[l2], writes=[l2])
    P.I("dve", "tensor_tensor", dict(out=l2[:, 2:3], in0=l2[:, 0:1], in1=l2[:, 1:2], op=ALU.subtract), reads=[l2], writes=[l2])
    P.I("dve", "tensor_scalar", dict(out=l2[:, 2:3], in0=l2[:, 2:3], scalar1=-1.0, scalar2=-LAM_INIT2, op0=ALU.mult, op1=ALU.add), reads=[l2], writes=[l2])
    psl = C.ps.next()
    P.I("pe", "matmul", dict(out=psl[:, 0:1], lhsT=C.ones_f[0:1, :], rhs=l2[0:1, 2:3], start=True, stop=True), reads=[C.ones_f, l2], writes=[psl])
    nlam = P.sb("nlam", [128, 1], F32)
    P.I("act", "copy", dict(out=nlam[:, :], in_=psl[:, 0:1]), reads=[psl], writes=[nlam])
    Sc = P.sb("Sc", [128, S_], F32)
    Pb = P.sb("Pb", [128, S_], BF16)
    pTr = Ring([P.ps(f"pT{i}", [128, 512], BF16) for i in range(2)])
    PTr = Ring([P.sb(f"PTs{i}", [128, 512], BF16) for i in range(3)])
    po = [P.ps(f"po{i}", [128, 512], F32) for i in range(2)]
    st = P.sb("st", [128, 16], F32)
    o1 = P.sb("o1", [128, 256], F32)
    o2 = P.sb("o2", [128, 256], F32)
    junk = P.sb("junk", [128, 256], F32)
    hor = Ring([P.sb(f"ho{i}", [128, 256], F32) for i in range(2)])
    ev = 0
    for qi in range(64):
        nk = (qi + 1) * 128
        qsl = slice(qi * 128, (qi + 1) * 128)
        qp = qi // 8
        for comp in range(2):
            for ck in range(0, nk, 512):
                w_ = min(512, nk - ck)
                ps = C.ps.next()
                rds = [qt[qp]] + [kt[i] for i in range(ck // pw, (ck + w_ - 1) // pw + 1)]
                P.I("pe", "matmul", dict(out=ps[:, :w_], lhsT=qb[:, comp, qsl], rhs=kb[:, comp, ck:ck + w_], start=True, stop=True), reads=rds, writes=[ps])
                last = (ck + w_ == nk)
                wf = w_ - 128 if last else w_
                if wf > 0:
                    eng = "act" if ev % 2 == 0 else "dve"
                    ev += 1
                    if eng == "act":
                        P.I("act", "copy", dict(out=Sc[:, ck:ck + wf], in_=ps[:, :wf]), reads=[ps], writes=[Sc])
                    else:
                        P.I("dve", "tensor_copy", dict(out=Sc[:, ck:ck + wf], in_=ps[:, :wf]), reads=[ps], writes=[Sc])
                if last:
                    P.I("dve", "tensor_tensor", dict(out=Sc[:, nk - 128:nk], in0=ps[:, w_ - 128:w_], in1=maskb[:, :], op=ALU.add), reads=[ps, maskb], writes=[Sc])
            P.I("dve", "reduce_max", dict(out=st[:, 0:1], in_=Sc[:, :nk], axis=AX.X), reads=[Sc], writes=[st])
            P.I("dve", "tensor_scalar", dict(out=st[:, 1:2], in0=st[:, 0:1], scalar1=-1.0, scalar2=None, op0=ALU.mult), reads=[st], writes=[st])
            P.I("act", "activation", dict(out=Pb[:, :nk], in_=Sc[:, :nk], func=AF.Exp, bias=st[:, 1:2], scale=1.0, accum_out=st[:, 2 + comp:3 + comp]), reads=[Sc, st], writes=[Pb, st])
            acc = po[comp]
            for g0 in range(0, qi + 1, 4):
                gn = min(4, qi + 1 - g0)
                pT = pTr.next()
                for b_ in range(gn):
                    kb_ = g0 + b_
                    P.I("pe", "transpose", dict(out=pT[:, b_ * 128:(b_ + 1) * 128], in_=Pb[:, kb_ * 128:(kb_ + 1) * 128], identity=identb[:, :]), reads=[Pb, identb], writes=[pT])
                PT = PTr.next()
                eng = "act" if ev % 2 == 0 else "dve"
                ev += 1
                if eng == "act":
                    P.I("act", "copy", dict(out=PT[:, :gn * 128], in_=pT[:, :gn * 128]), reads=[pT], writes=[PT])
                else:
                    P.I("dve", "tensor_copy", dict(out=PT[:, :gn * 128], in_=pT[:, :gn * 128]), reads=[pT], writes=[PT])
                for b_ in range(gn):
                    kb_ = g0 + b_
                    P.I("pe", "matmul", dict(out=acc[:, :256], lhsT=PT[:, b_ * 128:(b_ + 1) * 128], rhs=vb[:, kb_, :], start=(kb_ == 0), stop=(kb_ == qi)), reads=[PT, vt[kb_ // 8]], writes=[acc])
        P.I("dve", "reciprocal", dict(out=st[:, 4:6], in_=st[:, 2:4]), reads=[st], writes=[st])
        P.I("dve", "tensor_tensor", dict(out=st[:, 6:7], in0=st[:, 5:6], in1=nlam[:, :], op=ALU.mult), reads=[st, nlam], writes=[st])
        P.I("act", "mul", dict(out=o1[:, :], in_=po[0][:, :256], mul=st[:, 4:5]), reads=[po[0], st], writes=[o1])
        P.I("dve", "scalar_tensor_tensor", dict(out=o2[:, :], in0=po[1][:, :256], scalar=st[:, 6:7], in1=o1[:, :], op0=ALU.mult, op1=ALU.add), reads=[po[1], st, o1], writes=[o2])
        P.I("act", "activation", dict(out=junk[:, :], in_=o2[:, :], func=AF.Square, accum_out=st[:, 8:9]), reads=[o2], writes=[junk, st])
        P.I("dve", "tensor_scalar", dict(out=st[:, 9:10], in0=st[:, 8:9], scalar1=1.0 / 256, scalar2=1e-5, op0=ALU.mult, op1=ALU.add), reads=[st], writes=[st])
        P.I("act", "activation", dict(out=st[:, 9:10], in_=st[:, 9:10], func=AF.Sqrt), reads=[st], writes=[st])
        P.I("dve", "reciprocal", dict(out=st[:, 10:11], in_=st[:, 9:10]), reads=[st], writes=[st])
        ho = hor.next()
        P.I("dve", "scalar_tensor_tensor", dict(out=ho[:, :], in0=o2[:, :], scalar=st[:, 10:11], in1=sg[:, :], op0=ALU.mult, op1=ALU.mult), reads=[o2, st, sg], writes=[ho])
        P.dma("sp", out[qsl, :], ho[:, :], reads=[ho])
    return P.build()


def build_t2_simple(final, wname):
    P = Prog()
    C = setup_common(P)
    xh, xt, vecs, out = t2_common_inputs(P, 6)
    zd = P.dram_in("zT", [2048, TOK])
    wo = P.dram_in(wname, [2048, 2048])
    D = moe_inputs(P, 0)
    wring = Ring([P.sb(f"wt{i}", [128, 16, 512], BF16) for i in range(2)])
    zbf = P.nc.alloc_sbuf_tensor("zbf", [128, 16, TOK], BF16)
    zt = [P.tok(f"z{k}") for k in range(16)]
    for k in range(16):
        P.dma("pool", zbf[:, k, :], zd[k * 128:(k + 1) * 128, :], writes=[zt[k]])
    outproj_residual(P, C, wo, zbf, zt, xh, xt, vecs, 0, wring)
    iv = dict(gain2=1, sc2=2, sh2=3, g2=4)
    moe_tail(P, C, D, xh, xt, vecs, iv, wring, zbf, zt, final_gain_idx=(5 if final else None), out_dram=out)
    return P.build()


def layer_diff(inp, xs, mod, layer, j, final):
    sh1, sc1, g1, sh2, sc2, g2 = [mod[layer, i] for i in range(6)]
    v1 = vlay([inp["norm_gain"][layer, 0], sc1, sh1])
    pos = np.asarray(inp["positions"], np.int32)
    invf = np.zeros((128, 1), np.float32)
    fr = (500000.0 ** (-np.arange(0, 32, 2, dtype=np.float32) / 32)).astype(np.float32)
    invf[:16, 0] = fr / np.float32(2 * np.pi)
    invf[16:32, 0] = fr / np.float32(2 * np.pi)
    rotT = np.zeros((128, 128), np.float32)
    for m in range(16):
        rotT[m + 16, m] = -1.0
        rotT[m, m + 16] = 1.0
    w = _c(inp["diff_w_qkv"][j])
    r1 = _run(_prog("t1d", build_t1d), [dict(xT=xs[c], vecs=v1, w_qkv=w, pos=np.ascontiguousarray(pos[:, c * TOK:(c + 1) * TOK]), invf=invf, rotT=rotT) for c in range(NCORES)])
    qkT = np.concatenate([r["qkT"] for r in r1], axis=1)
    vT = np.concatenate([r["vT"] for r in r1], axis=1)
    if "on" in DEBUG:
        DEBUG["qkT"] = qkT; DEBUG["vT"] = vT
    maskb = _c(np.where(np.tril(np.ones((128, 128))) > 0, 0.0, -30000.0))
    cst = _consts()
    lam = _c(inp["diff_lambda"][j].reshape(1, 512))
    sg = _c(inp["diff_subln_gain"][j][None, :])
    maps = [dict(qT=_c(qkT[h * 256:(h + 1) * 256]), kT=_c(qkT[2048 + h * 256:2048 + (h + 1) * 256]), vtok=_c(vT[h * 256:(h + 1) * 256].T),
                 lam=lam, sgain=sg, ident=cst["ident"], maskb=maskb) for h in range(NCORES)]
    r2 = _run(_prog("mixd", build_mixd), maps)
    zT = np.concatenate([r["attn"].T for r in r2], axis=0)
    if "on" in DEBUG:
        DEBUG["zT"] = zT
        if "stop_after_mix" in DEBUG:
            return None
    v2 = vlay([g1, inp["norm_gain"][layer, 1], sc2, sh2, g2, inp["final_gain"]])
    mm = moe_maps(inp, layer)
    wo = _c(inp["diff_w_o"][j])
    maps = [dict(xT=xs[c], vecs=v2, zT=_c(zT[:, c * TOK:(c + 1) * TOK]), w_o=wo, **mm) for c in range(NCORES)]
    r3 = _run(_prog("t2s", build_t2_simple, final, "w_o"), maps)
    return [r["xout"] for r in r3]


NEG_E05 = -math.exp(-0.5)


def build_t1r():
    P = Prog()
    C = setup_common(P)
    x = P.dram_in("xT", [2048, TOK])
    xpd = P.dram_in("xprev", [128, 16])
    nfd = P.dram_in("notfirst", [128, 1])
    vd = P.dram_in("vecs", [128, 13, 16])
    wrkv = P.dram_in("w_rkv", [3, 2048, 2048])
    w1 = P.dram_in("w1", [2048, 96]); w2 = P.dram_in("w2", [96, 2048])
    a1 = P.dram_in("a1", [2048, 96]); a2 = P.dram_in("a2", [96, 2048])
    g1 = P.dram_in("g1", [2048, 256]); g2 = P.dram_in("g2", [256, 2048])
    blkd = P.dram_in("blk64", [128, 128])
    outs = {nm: P.dram_out(nm, [2048, TOK]) for nm in ("r", "k2", "v", "lw", "kk", "kka", "gate")}
    a_scr = P.dram("a_scr", [2048, TOK])
    xh, xt = load_x(P, x)
    vecs = P.sb("vecs", [128, 13, 16])
    P.dma("sp", vecs[:, :, :], vd[:, :, :], writes=[vecs])
    A = make_AB(P, vecs, 0, 1, 2)
    blk = P.sb("blk", [128, 128], BF16)
    P.dma("pool", blk[:, :], blkd[:, :], writes=[blk])
    xp = P.sb("xp", [128, 16], F32)
    P.dma("sp", xp[:, :], xpd[:, :], writes=[xp])
    nf = P.sb("nf", [128, 1], F32)
    P.dma("sp", nf[:, :], nfd[:, :], writes=[nf])
    xq = P.sb("xq", [128, 16], F32)
    P.I("dve", "tensor_tensor", dict(out=xq[:, :], in0=xp[:, :], in1=xp[:, :], op=ALU.mult), reads=[xp], writes=[xq])
    xs_ = P.sb("xs_", [128, 2], F32)
    P.I("dve", "reduce_sum", dict(out=xs_[:, 0:1], in_=xq[:, :], axis=AX.X), reads=[xq], writes=[xs_])
    psx = C.ps.next()
    P.I("pe", "matmul", dict(out=psx[:, 0:1], lhsT=C.ones_f[:, :], rhs=xs_[:, 0:1], start=True, stop=True), reads=[C.ones_f, xs_], writes=[psx])
    P.I("dve", "tensor_scalar", dict(out=xs_[:, 1:2], in0=psx[:, 0:1], scalar1=1.0 / 2048, scalar2=1e-6, op0=ALU.mult, op1=ALU.add), reads=[psx], writes=[xs_])
    P.I("act", "activation", dict(out=xs_[:, 1:2], in_=xs_[:, 1:2], func=AF.Sqrt), reads=[xs_], writes=[xs_])
    P.I("dve", "reciprocal", dict(out=xs_[:, 1:2], in_=xs_[:, 1:2]), reads=[xs_], writes=[xs_])
    hp = P.sb("hp", [128, 16], F32)
    P.I("dve", "tensor_scalar", dict(out=hp[:, :], in0=xp[:, :], scalar1=xs_[:, 1:2], scalar2=None, op0=ALU.mult), reads=[xp, xs_], writes=[hp])
    P.I("dve", "tensor_tensor", dict(out=hp[:, :], in0=hp[:, :], in1=A[:, :], op=ALU.mult), reads=[hp, A], writes=[hp])
    P.I("dve", "tensor_tensor", dict(out=hp[:, :], in0=hp[:, :], in1=vecs[:, 2, :], op=ALU.add), reads=[hp, vecs], writes=[hp])
    P.I("dve", "tensor_scalar", dict(out=hp[:, :], in0=hp[:, :], scalar1=nf[:, 0:1], scalar2=None, op0=ALU.mult), reads=[hp, nf], writes=[hp])
    sq = P.sb("nm_sq", [128, 16, 512], BF16)
    rstd = P.sb("nm_rstd", [128, 512], F32)
    for (c0, c1) in HALVES:
        w_ = c1 - c0
        for kc in range(16):
            P.I("act", "activation", dict(out=sq[:, kc, :w_], in_=xh[:, kc, c0:c1], func=AF.Square), reads=[xt[kc]], writes=[sq])
        ps = C.ps.next()
        for kc in range(16):
            P.I("pe", "matmul", dict(out=ps[:, :w_], lhsT=C.ones_bf[:, :], rhs=sq[:, kc, :w_], start=(kc == 0), stop=(kc == 15)), reads=[C.ones_bf, sq], writes=[ps])
        rstd_from_ps(P, ps, w_, 1.0 / 2048, 1e-6, rstd)
        for kc in range(16):
            P.I("dve", "tensor_tensor", dict(out=xh[:, kc, c0:c1], in0=xh[:, kc, c0:c1], in1=rstd[:, :w_], op=ALU.mult), reads=[xt[kc], rstd], writes=[xt[kc]])
            P.I("dve", "tensor_scalar", dict(out=xh[:, kc, c0:c1], in0=xh[:, kc, c0:c1], scalar1=A[:, kc:kc + 1], scalar2=vecs[:, 2, kc:kc + 1], op0=ALU.mult, op1=ALU.add), reads=[xt[kc], A, vecs], writes=[xt[kc]])
    xx = P.nc.alloc_sbuf_tensor("xx", [128, 16, TOK], BF16)
    xxt = [P.tok(f"xx{k}") for k in range(16)]
    for kc in range(16):
        P.I("pool", "tensor_tensor", dict(out=xx[:, kc, 1:TOK], in0=xh[:, kc, 0:TOK - 1], in1=xh[:, kc, 1:TOK], op=ALU.subtract), reads=[xt[kc]], writes=[xxt[kc]])
        P.I("dve", "tensor_tensor", dict(out=xx[:, kc, 0:1], in0=hp[:, kc:kc + 1], in1=xh[:, kc, 0:1], op=ALU.subtract), reads=[xt[kc], hp], writes=[xxt[kc]])
    mx = P.nc.alloc_sbuf_tensor("mixed", [128, 16, TOK], BF16)
    mxt = [P.tok(f"mx{k}") for k in range(16)]
    wring = Ring([P.sb(f"wt{i}", [128, 16, 512], BF16) for i in range(2)])
    stg = Ring([P.sb(f"stg{i}", [128, 512], F32) for i in range(4)])
    lora = P.nc.alloc_sbuf_tensor("lora", [128, 2, TOK], BF16)
    lot = [P.tok("lo0"), P.tok("lo1")]

    def mix(j):
        for kc in range(16):
            eng = "dve" if kc % 2 == 0 else "pool"
            P.I("dve", "scalar_tensor_tensor", dict(out=mx[:, kc, :], in0=xx[:, kc, :], scalar=vecs[:, 3 + j, kc:kc + 1], in1=xh[:, kc, :], op0=ALU.mult, op1=ALU.add), reads=[xxt[kc], vecs, xt[kc]], writes=[mxt[kc]])

    def src_m(kc, c0, c1):
        return mx[:, kc, c0:c1], [mxt[kc]]

    def lora_src(kp):
        def f(kc, c0, c1):
            return lora[:kp, kc, c0:c1], [lot[kc]]
        return f

    def ev_lora(func):
        def f(n, m, c0, c1, ps):
            if func is None:
                P.I("act", "copy", dict(out=lora[:m, n, c0:c1], in_=ps[:m, :]), reads=[ps], writes=[lot[n]])
            else:
                P.I("act", "activation", dict(out=lora[:m, n, c0:c1], in_=ps[:m, :], func=func), reads=[ps], writes=[lot[n]])
        return f

    def ev_out(dst, post):
        def f(n, m, c0, c1, ps):
            t = stg.next()
            post(n, ps, t)
            P.dma("sp", dst[n * 128:(n + 1) * 128, c0:c1], t[:, :], reads=[t])
        return f

    mix(4)
    linear_fm(P, C, a1[:, :], 2048, 96, src_m, ev_lora(None), wring)
    linear_fm(P, C, a2[:, :], 96, 2048, lora_src(96), ev_out(a_scr, lambda n, ps, t: P.I("act", "activation", dict(out=t[:, :], in_=ps[:, :], func=AF.Sigmoid, bias=vecs[:, 10, n:n + 1], scale=1.0), reads=[ps, vecs], writes=[t])), wring)
    mix(3)
    linear_fm(P, C, w1[:, :], 2048, 96, src_m, ev_lora(AF.Tanh), wring)

    def post_w(n, ps, t):
        P.I("act", "activation", dict(out=t[:, :], in_=ps[:, :], func=AF.Sigmoid, bias=vecs[:, 9, n:n + 1], scale=1.0), reads=[ps, vecs], writes=[t])
        P.I("dve", "tensor_scalar", dict(out=t[:, :], in0=t[:, :], scalar1=NEG_E05, scalar2=None, op0=ALU.mult), reads=[t], writes=[t])
    linear_fm(P, C, w2[:, :], 96, 2048, lora_src(96), ev_out(outs["lw"], post_w), wring)
    mix(5)
    linear_fm(P, C, g1[:, :], 2048, 256, src_m, ev_lora(AF.Sigmoid), wring)
    linear_fm(P, C, g2[:, :], 256, 2048, lora_src(128), ev_out(outs["gate"], lambda n, ps, t: P.I("act", "copy", dict(out=t[:, :], in_=ps[:, :]), reads=[ps], writes=[t])), wring)
    mix(2)
    linear_fm(P, C, wrkv[2], 2048, 2048, src_m, ev_out(outs["v"], lambda n, ps, t: P.I("act", "copy", dict(out=t[:, :], in_=ps[:, :]), reads=[ps], writes=[t])), wring)
    mix(0)
    linear_fm(P, C, wrkv[0], 2048, 2048, src_m, ev_out(outs["r"], lambda n, ps, t: P.I("act", "copy", dict(out=t[:, :], in_=ps[:, :]), reads=[ps], writes=[t])), wring)
    mix(1)
    at = Ring([P.sb(f"at{i}", [128, 512], F32) for i in range(2)])
    kq = Ring([P.sb(f"kq{i}", [128, 512], BF16) for i in range(2)])
    kt_ = Ring([P.sb(f"ktmp{i}", [128, 512], F32) for i in range(2)])

    def ev_k(n, m, c0, c1, ps):
        a_t = at.next()
        P.dma("sp", a_t[:, :], a_scr[n * 128:(n + 1) * 128, c0:c1], writes=[a_t])
        kp = kt_.next()
        P.I("dve", "tensor_scalar", dict(out=kp[:, :], in0=ps[:, :], scalar1=vecs[:, 11, n:n + 1], scalar2=None, op0=ALU.mult), reads=[ps, vecs], writes=[kp])
        q_ = kq.next()
        P.I("act", "activation", dict(out=q_[:, :], in_=kp[:, :], func=AF.Square), reads=[kp], writes=[q_])
        ps2 = C.ps.next()
        P.I("pe", "matmul", dict(out=ps2[:, :], lhsT=blk[:, :], rhs=q_[:, :], start=True, stop=True), reads=[blk, q_], writes=[ps2])
        rn = stg.next()
        P.I("dve", "tensor_scalar", dict(out=rn[:, :], in0=ps2[:, :], scalar1=1e-24, scalar2=None, op0=ALU.add), reads=[ps2], writes=[rn])
        P.I("act", "activation", dict(out=rn[:, :], in_=rn[:, :], func=AF.Sqrt), reads=[rn], writes=[rn])
        P.I("dve", "reciprocal", dict(out=rn[:, :], in_=rn[:, :]), reads=[rn], writes=[rn])
        tkk = stg.next()
        P.I("dve", "tensor_tensor", dict(out=tkk[:, :], in0=kp[:, :], in1=rn[:, :], op=ALU.mult), reads=[kp, rn], writes=[tkk])
        P.dma("sp", outs["kk"][n * 128:(n + 1) * 128, c0:c1], tkk[:, :], reads=[tkk])
        tka = stg.next()
        P.I("pool", "tensor_tensor", dict(out=tka[:, :], in0=tkk[:, :], in1=a_t[:, :], op=ALU.mult), reads=[tkk, a_t], writes=[tka])
        P.dma("sp", outs["kka"][n * 128:(n + 1) * 128, c0:c1], tka[:, :], reads=[tka])
        P.I("dve", "tensor_scalar", dict(out=a_t[:, :], in0=a_t[:, :], scalar1=-1.0, scalar2=vecs[:, 12, n:n + 1], op0=ALU.add, op1=ALU.mult), reads=[a_t, vecs], writes=[a_t])
        tk2 = stg.next()
        P.I("dve", "scalar_tensor_tensor", dict(out=tk2[:, :], in0=a_t[:, :], scalar=1.0, in1=ps[:, :], op0=ALU.add, op1=ALU.mult), reads=[a_t, ps], writes=[tk2])
        P.dma("sp", outs["k2"][n * 128:(n + 1) * 128, c0:c1], tk2[:, :], reads=[tk2])

    linear_fm(P, C, wrkv[1], 2048, 2048, src_m, ev_k, wring)
    return P.build()


def build_mixr(nchunks=128, seg=False):
    P = Prog()
    C = setup_common(P)
    L = 64
    NB = nchunks if seg else DEBUG.get("mixr_nb", 4)
    S_ = nchunks * L if seg else 8192
    BW = NB * L
    fmn = ("rF", "k2F", "kkF", "kkaF", "lwF")
    tmn = ("vT", "k2T", "kkaT", "lwT")
    fmd = {n: P.dram_in(n, [64, 4, S_]) for n in fmn}
    tmd = {n: P.dram_in(n, [S_, 256]) for n in tmn}
    triud = P.dram_in("triu64", [64, 64])
    sud = P.dram_in("su4", [64, 4, 64])
    sld = P.dram_in("sl4", [64, 4, 64])
    tr4d = P.dram_in("triu4", [64, 4, 64])
    out = P.dram_out("y", [S_, 256])
    triu = P.sb("triu", [64, 64], F32); P.dma("sp", triu[:, :], triud[:, :], writes=[triu])
    su4 = P.sb("su4", [64, 4, 64], F32); P.dma("sp", su4[:, :, :], sud[:, :, :], writes=[su4])
    sl4 = P.sb("sl4", [64, 4, 64], F32); P.dma("sp", sl4[:, :, :], sld[:, :, :], writes=[sl4])
    tr4 = P.sb("tr4", [64, 4, 64], F32); P.dma("sp", tr4[:, :, :], tr4d[:, :, :], writes=[tr4])
    nbuf = 1 if seg else 2
    fmb = {n: [P.sb(f"{n}{i}", [64, 4, BW], F32) for i in range(nbuf)] for n in fmn}
    tmb = {n: [P.sb(f"{n}{i}", [64, NB, 256], F32) for i in range(nbuf)] for n in tmn}
    M0 = P.sb("M0", [64, 4, 64], F32)
    if seg:
        m_in = P.dram_in("m_in", [64, 4, 64])
        m_out = P.dram_out("m_out", [64, 4, 64])
        P.dma("sp", M0[:, :, :], m_in[:, :, :], writes=[M0])
    else:
        P.I("dve", "memset", dict(ap=M0[:, :, :], constant=0.0), writes=[M0])

    def t3(name, n=2):
        return Ring([P.sb(f"{name}{i}", [64, 4, 64], F32) for i in range(n)])
    eGr, eGnr, eGxr, eGtr = t3("eG"), t3("eGn"), t3("eGx"), t3("eGt")
    rTr, aTr, bTr, kTr, btr, ktr = t3("rT"), t3("aT"), t3("bT"), t3("kT"), t3("b_t"), t3("k_t")
    Aakr, Arbr, Arkr = t3("Aak"), t3("Arb"), t3("Ark")
    Nr = [t3(f"N{i}") for i in range(6)]
    Ar = [t3(f"A{i}") for i in range(5)]
    Zr = t3("Z", 3)
    Yr = t3("Y")
    tM = P.sb("tM", [64, 4, 64], F32)

    def v3(ps):
        return ps[:64, :256].rearrange("p (h t) -> p h t", h=4)

    engs = ["dve", "dve"]
    ei = 0
    for c in range(nchunks):
        bi = c // NB
        ci = c % NB
        bsel = (bi % 2) if (DEBUG.get("mixr_db", 1) and not seg) else 0
        if ci == 0:
            for n in fmn:
                sb_ = 0 if DEBUG.get("mixr_src0") else bi
                P.dma(DEBUG.get("mixr_q", "pool"), fmb[n][bsel][:, :, :], fmd[n][:, :, sb_ * BW:(sb_ + 1) * BW], writes=[fmb[n][bsel]])
            for n in tmn:
                P.dma(DEBUG.get("mixr_q", "pool"), tmb[n][bsel][:, :, :], tmd[n][sb_ * BW:(sb_ + 1) * BW, :].rearrange("(n t) d -> t n d", t=L), writes=[tmb[n][bsel]])
        F = {n: fmb[n][bsel] for n in fmn}
        T = {n: tmb[n][bsel] for n in tmn}
        cs = slice(ci * L, (ci + 1) * L)
        psG = C.ps.next()
        for hd in range(4):
            P.I("pe", "matmul", dict(out=psG[:64, hd * 64:(hd + 1) * 64], lhsT=T["lwT"][:, ci, hd * 64:(hd + 1) * 64], rhs=triu[:, :], start=True, stop=True), reads=[T["lwT"], triu], writes=[psG])
        psGt = C.ps.next()
        P.I("pe", "matmul", dict(out=psGt[:64, :256], lhsT=triu[:, :], rhs=T["lwT"][:, ci, :], start=True, stop=True), reads=[T["lwT"], triu], writes=[psGt])
        eG, eGn, eGx, eGt = eGr.next(), eGnr.next(), eGxr.next(), eGtr.next()
        P.I("act", "activation", dict(out=eG[:, :, :], in_=v3(psG), func=AF.Exp), reads=[psG], writes=[eG])
        P.I("act", "activation", dict(out=eGn[:, :, :], in_=v3(psG), func=AF.Exp, scale=-1.0), reads=[psG], writes=[eGn])
        P.I("dve", "tensor_tensor", dict(out=eGx[:, :, :], in0=v3(psG), in1=F["lwF"][:, :, cs], op=ALU.subtract), reads=[psG, F["lwF"]], writes=[eGx])
        P.I("act", "activation", dict(out=eGx[:, :, :], in_=eGx[:, :, :], func=AF.Exp), reads=[eGx], writes=[eGx])
        P.I("act", "activation", dict(out=eGt[:, :, :], in_=v3(psGt), func=AF.Exp, scale=-1.0), reads=[psGt], writes=[eGt])
        rT, aT, bT, kT, b_t, k_t = rTr.next(), aTr.next(), bTr.next(), kTr.next(), btr.next(), ktr.next()
        P.I("dve", "tensor_tensor", dict(out=rT[:, :, :], in0=F["rF"][:, :, cs], in1=eG[:, :, :], op=ALU.mult), reads=[F["rF"], eG], writes=[rT])
        P.I("dve", "scalar_tensor_tensor", dict(out=aT[:, :, :], in0=F["kkF"][:, :, cs], scalar=-1.0, in1=eGx[:, :, :], op0=ALU.mult, op1=ALU.mult), reads=[F["kkF"], eGx], writes=[aT])
        P.I("dve", "tensor_tensor", dict(out=bT[:, :, :], in0=F["kkaF"][:, :, cs], in1=eGn[:, :, :], op=ALU.mult), reads=[F["kkaF"], eGn], writes=[bT])
        P.I("dve", "tensor_tensor", dict(out=kT[:, :, :], in0=F["k2F"][:, :, cs], in1=eGn[:, :, :], op=ALU.mult), reads=[F["k2F"], eGn], writes=[kT])
        P.I("dve", "tensor_tensor", dict(out=b_t[:, :, :], in0=T["kkaT"][:, ci, :].rearrange("p (h k) -> p h k", h=4), in1=eGt[:, :, :], op=ALU.mult), reads=[T["kkaT"], eGt], writes=[b_t])
        P.I("dve", "tensor_tensor", dict(out=k_t[:, :, :], in0=T["k2T"][:, ci, :].rearrange("p (h k) -> p h k", h=4), in1=eGt[:, :, :], op=ALU.mult), reads=[T["k2T"], eGt], writes=[k_t])

        def mm4(lhs, rhs, dst, mask):
            ps = C.ps.next()
            for hd in range(4):
                P.I("pe", "matmul", dict(out=ps[:64, hd * 64:(hd + 1) * 64], lhsT=lhs[:, hd, :], rhs=rhs[:, hd, :], start=True, stop=True), reads=[lhs, rhs], writes=[ps])
            nonlocal ei
            eng = engs[ei % 2]
            ei += 1
            if mask is not None:
                if eng == "pool":
                    eng = "dve"
                P.I(eng, "tensor_tensor", dict(out=dst[:, :, :], in0=v3(ps), in1=mask[:, :, :], op=ALU.mult), reads=[ps, mask], writes=[dst])
            else:
                if ei % 2 == 0:
                    P.I("act", "copy", dict(out=dst[:, :, :], in_=v3(ps)), reads=[ps], writes=[dst])
                else:
                    P.I("dve", "tensor_copy", dict(out=dst[:, :, :], in_=v3(ps)), reads=[ps], writes=[dst])

        N = [r_.next() for r_ in Nr]
        A = [r_.next() for r_ in Ar]
        Aak, Arb, Ark = Aakr.next(), Arbr.next(), Arkr.next()
        mm4(bT, aT, N[0], su4)
        mm4(aT, bT, A[0], sl4)
        mm4(kT, aT, Aak, su4)
        mm4(bT, rT, Arb, tr4)
        mm4(kT, rT, Ark, tr4)
        for i in range(5):
            if i < 4:
                mm4(N[i], A[i], A[i + 1], None)
            mm4(A[i], N[i], N[i + 1], None)
        vT = T["vT"]
        psX = C.ps.next()
        for hd in range(4):
            hs = slice(hd * 64, (hd + 1) * 64)
            P.I("pe", "matmul", dict(out=psX[:64, hs], lhsT=aT[:, hd, :], rhs=M0[:, hd, :], start=True, stop=False), reads=[aT, M0], writes=[psX])
            P.I("pe", "matmul", dict(out=psX[:64, hs], lhsT=Aak[:, hd, :], rhs=vT[:, ci, hs], start=False, stop=True), reads=[Aak, vT], writes=[psX])
        Z = Zr.next()
        P.I("act", "copy", dict(out=Z[:, :, :], in_=v3(psX)), reads=[psX], writes=[Z])
        for i in range(6):
            psZ = C.ps.next()
            for hd in range(4):
                P.I("pe", "matmul", dict(out=psZ[:64, hd * 64:(hd + 1) * 64], lhsT=N[i][:, hd, :], rhs=Z[:, hd, :], start=True, stop=True), reads=[N[i], Z], writes=[psZ])
            Z2 = Zr.next()
            P.I("dve", "tensor_tensor", dict(out=Z2[:, :, :], in0=v3(psZ), in1=Z[:, :, :], op=ALU.add), reads=[psZ, Z], writes=[Z2])
            Z = Z2
        U = Z
        psY = C.ps.next()
        for hd in range(4):
            hs = slice(hd * 64, (hd + 1) * 64)
            P.I("pe", "matmul", dict(out=psY[:64, hs], lhsT=rT[:, hd, :], rhs=M0[:, hd, :], start=True, stop=False), reads=[rT, M0], writes=[psY])
            P.I("pe", "matmul", dict(out=psY[:64, hs], lhsT=Arb[:, hd, :], rhs=U[:, hd, :], start=False, stop=False), reads=[Arb, U], writes=[psY])
            P.I("pe", "matmul", dict(out=psY[:64, hs], lhsT=Ark[:, hd, :], rhs=vT[:, ci, hs], start=False, stop=True), reads=[Ark, vT], writes=[psY])
        Y = Yr.next()
        P.I("act", "copy", dict(out=Y[:, :, :], in_=v3(psY)), reads=[psY], writes=[Y])
        P.dma("sp", out[c * L:(c + 1) * L, :].rearrange("t (h v) -> t h v", h=4), Y[:, :, :], reads=[Y])
        psM = C.ps.next()
        for hd in range(4):
            hs = slice(hd * 64, (hd + 1) * 64)
            P.I("pe", "matmul", dict(out=psM[:64, hs], lhsT=b_t[:, hd, :], rhs=U[:, hd, :], start=True, stop=False), reads=[b_t, U], writes=[psM])
            P.I("pe", "matmul", dict(out=psM[:64, hs], lhsT=k_t[:, hd, :], rhs=vT[:, ci, hs], start=False, stop=True), reads=[k_t, vT], writes=[psM])
        P.I("dve", "tensor_tensor", dict(out=tM[:, :, :], in0=v3(psM), in1=M0[:, :, :], op=ALU.add), reads=[psM, M0], writes=[tM])
        for hd in range(4):
            P.I("dve", "tensor_scalar", dict(out=M0[:, hd, :], in0=tM[:, hd, :], scalar1=eG[:, hd, 63:64], scalar2=None, op0=ALU.mult), reads=[tM, eG], writes=[M0])
    if seg:
        P.dma("sp", m_out[:, :, :], M0[:, :, :], reads=[M0])
    return P.build()


def build_t2r(final):
    P = Prog()
    C = setup_common(P)
    xh, xt, vecs, out = t2_common_inputs(P, 9)
    ind = {n: P.dram_in(n, [2048, TOK]) for n in ("yT", "r", "k2", "v", "gate")}
    wo = P.dram_in("w_o", [2048, 2048])
    blkd = P.dram_in("blk64", [128, 128])
    D = moe_inputs(P, 0)
    wring = Ring([P.sb(f"wt{i}", [128, 16, 512], BF16) for i in range(2)])
    zbf = P.nc.alloc_sbuf_tensor("zbf", [128, 16, TOK], BF16)
    zt = [P.tok(f"z{k}") for k in range(16)]
    blk = P.sb("blk", [128, 128], F32)
    P.dma("sp", blk[:, :], blkd[:, :], writes=[blk])
    rings = {n: Ring([P.sb(f"{n}{i}", [128, 512], F32) for i in range(1)]) for n in ("yT", "r", "k2", "v", "gate")}
    dr = Ring([P.sb(f"d{i}", [128, 512], F32) for i in range(1)])
    d2r = Ring([P.sb(f"d2{i}", [128, 512], F32) for i in range(1)])
    for kc in range(16):
        for (c0, c1) in HALVES:
            t = {}
            for n in ("yT", "r", "k2", "v", "gate"):
                t[n] = rings[n].next()
                P.dma("sp", t[n][:, :], ind[n][kc * 128:(kc + 1) * 128, c0:c1], writes=[t[n]])
            y = t["yT"]
            psm = C.ps.next()
            P.I("pe", "matmul", dict(out=psm[:, :], lhsT=blk[:, :], rhs=y[:, :], start=True, stop=True), reads=[blk, y], writes=[psm])
            d = dr.next()
            P.I("dve", "scalar_tensor_tensor", dict(out=d[:, :], in0=psm[:, :], scalar=-1.0 / 64, in1=y[:, :], op0=ALU.mult, op1=ALU.add), reads=[psm, y], writes=[d])
            d2 = d2r.next()
            P.I("act", "activation", dict(out=d2[:, :], in_=d[:, :], func=AF.Square), reads=[d], writes=[d2])
            psv = C.ps.next()
            P.I("pe", "matmul", dict(out=psv[:, :], lhsT=blk[:, :], rhs=d2[:, :], start=True, stop=True), reads=[blk, d2], writes=[psv])
            P.I("dve", "tensor_scalar", dict(out=d2[:, :], in0=psv[:, :], scalar1=1.0 / 64, scalar2=64e-5, op0=ALU.mult, op1=ALU.add), reads=[psv], writes=[d2])
            P.I("act", "activation", dict(out=d2[:, :], in_=d2[:, :], func=AF.Sqrt), reads=[d2], writes=[d2])
            P.I("dve", "reciprocal", dict(out=d2[:, :], in_=d2[:, :]), reads=[d2], writes=[d2])
            P.I("pool", "tensor_tensor", dict(out=d[:, :], in0=d[:, :], in1=d2[:, :], op=ALU.mult), reads=[d, d2], writes=[d])
            P.I("dve", "tensor_scalar", dict(out=d[:, :], in0=d[:, :], scalar1=vecs[:, 0, kc:kc + 1], scalar2=vecs[:, 1, kc:kc + 1], op0=ALU.mult, op1=ALU.add), reads=[d, vecs], writes=[d])
            rk = t["r"]
            P.I("pool", "tensor_tensor", dict(out=rk[:, :], in0=rk[:, :], in1=t["k2"][:, :], op=ALU.mult), reads=[rk, t["k2"]], writes=[rk])
            P.I("dve", "tensor_scalar", dict(out=rk[:, :], in0=rk[:, :], scalar1=vecs[:, 2, kc:kc + 1], scalar2=None, op0=ALU.mult), reads=[rk, vecs], writes=[rk])
            psb = C.ps.next()
            P.I("pe", "matmul", dict(out=psb[:, :], lhsT=blk[:, :], rhs=rk[:, :], start=True, stop=True), reads=[blk, rk], writes=[psb])
            P.I("dve", "tensor_tensor", dict(out=t["v"][:, :], in0=psb[:, :], in1=t["v"][:, :], op=ALU.mult), reads=[psb, t["v"]], writes=[t["v"]])
            P.I("pool", "tensor_tensor", dict(out=d[:, :], in0=d[:, :], in1=t["v"][:, :], op=ALU.add), reads=[d, t["v"]], writes=[d])
            P.I("dve", "tensor_tensor", dict(out=zbf[:, kc, c0:c1], in0=d[:, :], in1=t["gate"][:, :], op=ALU.mult), reads=[d, t["gate"]], writes=[zt[kc]])
    outproj_residual(P, C, wo, zbf, zt, xh, xt, vecs, 3, wring)
    iv = dict(gain2=4, sc2=5, sh2=6, g2=7)
    moe_tail(P, C, D, xh, xt, vecs, iv, wring, zbf, zt, final_gain_idx=(8 if final else None), out_dram=out)
    return P.build()


def layer_rwkv(inp, xs, mod, layer, j, final):
    sh1, sc1, g1, sh2, sc2, g2 = [mod[layer, i] for i in range(6)]
    mu = inp["rwkv_mu"][j]
    v1 = vlay([inp["norm_gain"][layer, 0], sc1, sh1] + [mu[i] for i in range(6)] + [inp["rwkv_w0"][j], inp["rwkv_a0"][j], inp["rwkv_k_k"][j], inp["rwkv_k_a"][j]])
    blk = _c(np.kron(np.eye(2), np.ones((64, 64))))
    wts = dict(w_rkv=_c(inp["rwkv_w_rkv"][j]), w1=_c(inp["rwkv_w1"][j]), w2=_c(inp["rwkv_w2"][j]), a1=_c(inp["rwkv_a1"][j]), a2=_c(inp["rwkv_a2"][j]),
               g1=_c(inp["rwkv_g1"][j]), g2=_c(inp["rwkv_g2"][j]), blk64=blk)
    maps = []
    for c in range(NCORES):
        if c == 0:
            xp = np.zeros((128, 16), np.float32); nf = np.zeros((128, 1), np.float32)
        else:
            xp = _c(xs[c - 1][:, TOK - 1].reshape(16, 128).T); nf = np.ones((128, 1), np.float32)
        maps.append(dict(xT=xs[c], xprev=xp, notfirst=nf, vecs=v1, **wts))
    r1 = _run(_prog("t1r", build_t1r), maps)
    full = {n: np.concatenate([r[n] for r in r1], axis=1) for n in ("r", "k2", "v", "lw", "kk", "kka")}
    if "on" in DEBUG:
        DEBUG["t1r"] = full; DEBUG["gate"] = np.concatenate([r["gate"] for r in r1], axis=1)
    triu64 = _c(np.triu(np.ones((64, 64))))
    su = np.triu(np.ones((64, 64)), 1)
    cst = dict(triu64=triu64, su4=_c(np.repeat(su[:, None, :], 4, axis=1)), sl4=_c(np.repeat(su.T[:, None, :], 4, axis=1)), triu4=_c(np.repeat(triu64[:, None, :], 4, axis=1)))
    NSEG = 8
    SEGT = 8192 // NSEG
    states = [np.zeros((64, 4, 64), np.float32) for _ in range(NCORES)]
    ysegs = [[] for _ in range(NCORES)]
    ncm = _prog("mixr", build_mixr, SEGT // 64, True)
    for sg_ in range(NSEG):
        ts_ = slice(sg_ * SEGT, (sg_ + 1) * SEGT)
        maps = []
        for c in range(NCORES):
            rows = slice(c * 256, (c + 1) * 256)
            m = dict(cst)
            for fn, src in (("rF", "r"), ("k2F", "k2"), ("kkF", "kk"), ("kkaF", "kka"), ("lwF", "lw")):
                m[fn] = _c(full[src][rows, ts_].reshape(4, 64, SEGT).transpose(1, 0, 2))
            for tn, src in (("vT", "v"), ("k2T", "k2"), ("kkaT", "kka"), ("lwT", "lw")):
                m[tn] = _c(full[src][rows, ts_].T)
            m["m_in"] = states[c]
            maps.append(m)
        rs = _run(ncm, maps)
        for c in range(NCORES):
            states[c] = _c(rs[c]["m_out"])
            ysegs[c].append(rs[c]["y"])
    r2 = [dict(y=np.concatenate(ysegs[c], axis=0)) for c in range(NCORES)]
    yT = np.concatenate([r["y"].T for r in r2], axis=0)
    if "on" in DEBUG:
        DEBUG["yT"] = yT
        if "stop_after_mix" in DEBUG:
            return None
    v2 = vlay([inp["rwkv_ln_gain"][j], inp["rwkv_ln_bias"][j], inp["rwkv_r_k"][j].reshape(-1), g1, inp["norm_gain"][layer, 1], sc2, sh2, g2, inp["final_gain"]])
    mm = moe_maps(inp, layer)
    wo = _c(inp["rwkv_w_o"][j])
    maps = [dict(xT=xs[c], vecs=v2, yT=_c(yT[:, c * TOK:(c + 1) * TOK]), r=r1[c]["r"], k2=r1[c]["k2"], v=r1[c]["v"], gate=r1[c]["gate"], w_o=wo, blk64=blk, **mm) for c in range(NCORES)]
    r3 = _run(_prog("t2r", build_t2r, final), maps)
    return [r["xout"] for r in r3]


def kernel(**inputs):
    inp = {k: np.asarray(v) for k, v in inputs.items()}
    mod = run_mod(inp)
    x = inp["x"][0]
    xs = [_c(x[c * TOK:(c + 1) * TOK].T) for c in range(NCORES)]
    seen = [0, 0, 0]
    for layer in range(4):
        kind = layer % 3
        j = seen[kind]
        seen[kind] += 1
        final = (layer == 3)
        if kind == 0:
            xs = layer_mlstm(inp, xs, mod, layer, j, final)
        elif kind == 1:
            xs = layer_rwkv(inp, xs, mod, layer, j, final)
        else:
            xs = layer_diff(inp, xs, mod, layer, j, final)
    out = np.concatenate([a.T for a in xs], axis=0)[None]
    return np.ascontiguousarray(out, dtype=np.float32)
```

```python
import numpy as np
import concourse.bass as bass
import concourse.mybir as mybir
from concourse.bass_utils import run_bass_kernel_spmd

F32 = mybir.dt.float32
BF16 = mybir.dt.bfloat16
I32 = mybir.dt.int32
ALU = mybir.AluOpType
AF = mybir.ActivationFunctionType
AX = mybir.AxisListType

COMPUTE = ("pe", "act", "dve", "pool")
QUEUES = ("sp", "act", "pool")
NDMASEM = 12


class Tok:
    __slots__ = ("name", "last_w", "readers", "h")

    def __init__(self, name, h=None):
        self.name = name
        self.last_w = None
        self.readers = []
        self.h = h

    def __getitem__(self, idx):
        return self.h[idx]


class Ins:
    __slots__ = ("eng", "fn", "deps", "pos", "is_dma", "observed", "semval", "dsem", "dval", "prev_same_sem")

    def __init__(self, eng, fn, is_dma):
        self.eng = eng
        self.fn = fn
        self.deps = []
        self.is_dma = is_dma
        self.observed = False
        self.semval = None
        self.dsem = None
        self.dval = None
        self.prev_same_sem = None


class Prog:
    def __init__(self, same_engine_sync=True):
        self.nc = bass.Bass("TRN2", target_bir_lowering=False)
        self.ins = []
        self.streams = {e: [] for e in ("pe", "act", "dve", "pool", "sp")}
        self.same_engine_sync = same_engine_sync
        self.dma_rr = {q: 0 for q in QUEUES}
        self.dma_last = {}
        self._n = 0

    def dram_in(self, name, shape, dtype=F32):
        return self.nc.dram_tensor(name, list(shape), dtype, kind="ExternalInput")

    def dram_out(self, name, shape, dtype=F32):
        return self.nc.dram_tensor(name, list(shape), dtype, kind="ExternalOutput")

    def dram(self, name, shape, dtype=F32):
        return self.nc.dram_tensor(name, list(shape), dtype, kind="Internal")

    def sb(self, name, shape, dtype=F32):
        self._n += 1
        h = self.nc.alloc_sbuf_tensor(f"{name}_{self._n}", list(shape), dtype)
        return Tok(name, h)

    def ps(self, name, shape, dtype=F32):
        self._n += 1
        h = self.nc.alloc_psum_tensor(f"{name}_{self._n}", list(shape), dtype)
        return Tok(name, h)

    def tok(self, name, h=None):
        return Tok(name, h)

    def _record(self, eng, fn, reads, writes, is_dma):
        ins = Ins(eng, fn, is_dma)
        deps = []
        for t in reads:
            if t.last_w is not None:
                deps.append(t.last_w)
        for t in writes:
            if t.last_w is not None:
                deps.append(t.last_w)
            deps.extend(t.readers)
        seen = set()
        for d in deps:
            if id(d) not in seen and d is not ins:
                seen.add(id(d))
                ins.deps.append(d)
        for t in reads:
            t.readers.append(ins)
        for t in writes:
            t.last_w = ins
            t.readers = []
        ins.pos = len(self.streams[eng])
        self.streams[eng].append(ins)
        self.ins.append(ins)
        return ins

    def op(self, eng, fn, reads=(), writes=()):
        return self._record(eng, fn, reads, writes, False)

    def I(self, eng, name, kw, reads=(), writes=()):
        def fn(e, name=name, kw=kw):
            return getattr(e, name)(**kw)
        return self._record(eng, fn, reads, writes, False)

    def dma(self, q, out, in_, reads=(), writes=(), **kw):
        def fn(e, out=out, in_=in_, kw=kw):
            return e.dma_start(out=out, in_=in_, **kw)
        ins = self._record(q, fn, reads, writes, True)
        slot = self.dma_rr[q]
        self.dma_rr[q] = (slot + 1) % NDMASEM
        key = (q, slot)
        ins.dsem = key
        prev = self.dma_last.get(key)
        ins.prev_same_sem = prev
        ins.dval = (prev.dval if prev is not None else 0) + 16
        self.dma_last[key] = ins
        return ins

    def build(self):
        nc = self.nc
        seen_pos = {e: {f: -1 for f in self.streams} for e in self.streams}
        seen_dma = {e: set() for e in self.streams}
        waits = {}
        for ins in self.ins:
            w = []
            E = ins.eng
            for d in ins.deps:
                if d.is_dma:
                    if id(d) in seen_dma[E]:
                        continue
                    seen_dma[E].add(id(d))
                    w.append(d)
                else:
                    F = d.eng
                    if F == E:
                        if not self.same_engine_sync or E == "pe":
                            continue
                    if seen_pos[E][F] >= d.pos:
                        continue
                    seen_pos[E][F] = d.pos
                    d.observed = True
                    w.append(d)
            if ins.is_dma and ins.prev_same_sem is not None:
                p = ins.prev_same_sem
                if id(p) not in seen_dma[E]:
                    seen_dma[E].add(id(p))
                    w.append(p)
            waits[id(ins)] = w
        cnt = {}
        for e, st in self.streams.items():
            c = 0
            for ins in st:
                if not ins.is_dma and ins.observed:
                    c += 1
                    ins.semval = c
            cnt[e] = c
        self.sem_counts = cnt
        esem = {e: nc.alloc_semaphore(f"s_{e}") for e in COMPUTE}
        dsem = {}
        for q in QUEUES:
            for s in range(NDMASEM):
                if (q, s) in self.dma_last:
                    dsem[(q, s)] = nc.alloc_semaphore(f"d_{q}_{s}")
        streams = self.streams

        def emit(e, eng):
            for ins in streams[e]:
                for d in waits[id(ins)]:
                    if d.is_dma:
                        eng.wait_ge(dsem[d.dsem], d.dval)
                    else:
                        eng.wait_ge(esem[d.eng], d.semval)
                r = ins.fn(eng)
                if ins.is_dma:
                    r.then_inc(dsem[ins.dsem], 16)
                elif ins.observed:
                    r.then_inc(esem[e], 1)
            for (q, s), last in self.dma_last.items():
                if q == e:
                    eng.wait_ge(dsem[(q, s)], last.dval)

        with nc.Block() as block:
            @block.sync
            def _(eng):
                emit("sp", eng)

            @block.scalar
            def _(eng):
                emit("act", eng)

            @block.vector
            def _(eng):
                emit("dve", eng)

            @block.gpsimd
            def _(eng):
                emit("pool", eng)

            @block.tensor
            def _(eng):
                emit("pe", eng)
        return nc


TOK = 1024
HALVES = [(0, 512), (512, 1024)]
NCORES = 8


class Ring:
    def __init__(self, toks):
        self.t = toks
        self.i = 0

    def next(self):
        t = self.t[self.i % len(self.t)]
        self.i += 1
        return t


class Ctx:
    pass


def setup_common(P, npsum=8):
    C = Ctx()
    C.ps = Ring([P.ps(f"ps{i}", [128, 512]) for i in range(npsum)])
    C.ones_bf = P.sb("ones_bf", [128, 128], BF16)
    P.I("dve", "memset", dict(ap=C.ones_bf[:, :], constant=1.0), writes=[C.ones_bf])
    C.ones_f = P.sb("ones_f", [128, 128], F32)
    P.I("dve", "memset", dict(ap=C.ones_f[:, :], constant=1.0), writes=[C.ones_f])
    return C


def load_x(P, x_dram, name="xT_sb"):
    h = P.nc.alloc_sbuf_tensor(name, [128, 16, TOK], F32)
    toks = [P.tok(f"{name}{k}") for k in range(16)]
    for k in range(16):
        P.dma("sp", h[:, k, :], x_dram[k * 128:(k + 1) * 128, :], writes=[toks[k]])
    return h, toks


def make_AB(P, vecs, ig, isc, ish):
    A = P.sb("Amod", [128, 16], F32)
    P.I("dve", "tensor_scalar", dict(out=A[:, :], in0=vecs[:, isc, :], scalar1=1.0, scalar2=None, op0=ALU.add), reads=[vecs], writes=[A])
    P.I("dve", "tensor_tensor", dict(out=A[:, :], in0=A[:, :], in1=vecs[:, ig, :], op=ALU.mult), reads=[A, vecs], writes=[A])
    return A


def rstd_from_ps(P, ps, w, inv_n, eps, dst):
    P.I("dve", "tensor_scalar", dict(out=dst[:, :w], in0=ps[:, :w], scalar1=inv_n, scalar2=eps, op0=ALU.mult, op1=ALU.add), reads=[ps], writes=[dst])
    P.I("act", "activation", dict(out=dst[:, :w], in_=dst[:, :w], func=AF.Sqrt), reads=[dst], writes=[dst])
    P.I("dve", "reciprocal", dict(out=dst[:, :w], in_=dst[:, :w]), reads=[dst], writes=[dst])


def normmod(P, C, xh, xtoks, A, vecs, ish, hbf, hbf_toks, S, half_hook=None, cols=None):
    for (c0, c1) in (cols or HALVES):
        w = c1 - c0
        for kc in range(16):
            P.I("act", "activation", dict(out=S.sq[:, kc, :w], in_=xh[:, kc, c0:c1], func=AF.Square), reads=[xtoks[kc]], writes=[S.sq])
        ps = C.ps.next()
        for kc in range(16):
            P.I("pe", "matmul", dict(out=ps[:, :w], lhsT=C.ones_bf[:, :], rhs=S.sq[:, kc, :w], start=(kc == 0), stop=(kc == 15)), reads=[C.ones_bf, S.sq], writes=[ps])
        rstd_from_ps(P, ps, w, 1.0 / 2048, 1e-6, S.rstd)
        for kc in range(16):
            if S.tmp32 is not None:
                dst = S.tmp32[:, kc, :w]
                dt = [S.tmp32_toks[kc]]
            else:
                dst = S.tmpk[kc % 2][:, :w]
                dt = [S.tmpk[kc % 2]]
            P.I("dve", "tensor_tensor", dict(out=dst, in0=xh[:, kc, c0:c1], in1=S.rstd[:, :w], op=ALU.mult), reads=[xtoks[kc], S.rstd], writes=dt)
            if S.tmp32 is not None:
                P.I("dve", "tensor_scalar", dict(out=dst, in0=dst, scalar1=A[:, kc:kc + 1], scalar2=vecs[:, ish, kc:kc + 1], op0=ALU.mult, op1=ALU.add), reads=dt + [A, vecs], writes=dt)
                P.I("act", "copy", dict(out=hbf[:, kc, c0:c1], in_=dst), reads=dt, writes=[hbf_toks[kc]])
            else:
                P.I("dve", "tensor_scalar", dict(out=hbf[:, kc, c0:c1], in0=dst, scalar1=A[:, kc:kc + 1], scalar2=vecs[:, ish, kc:kc + 1], op0=ALU.mult, op1=ALU.add), reads=dt + [A, vecs], writes=[hbf_toks[kc]])
        if half_hook is not None:
            half_hook(c0, c1)


def norm_scratch(P, want32, sw=512):
    S = Ctx()
    S.sq = P.sb("nm_sq", [128, 16, sw], BF16)
    S.rstd = P.sb("nm_rstd", [128, sw], F32)
    if want32:
        S.tmp32 = P.nc.alloc_sbuf_tensor("nm_tmp32", [128, 16, sw], F32)
        S.tmp32_toks = [P.tok(f"tmp32_{k}") for k in range(16)]
    else:
        S.tmp32 = None
        S.tmpk = [P.sb(f"nm_tmpk{i}", [128, 512], F32) for i in range(2)]
    return S


def linear_fm(P, C, W, K, N, src, evac, wring, halves=HALVES, n0=0):
    KC = (K + 127) // 128
    kp = min(K, 128)
    for nb in range(0, N, 512):
        nbw = min(512, N - nb)
        wt = wring.next()
        if K % 128 == 0:
            P.dma("pool", wt[:, :KC, :nbw], W[:, nb:nb + nbw].rearrange("(k p) n -> p k n", p=128), writes=[wt])
        else:
            P.dma("pool", wt[:kp, 0, :nbw], W[:, nb:nb + nbw], writes=[wt])
        for j in range(0, nbw, 128):
            m = min(128, nbw - j)
            for (c0, c1) in halves:
                ps = C.ps.next()
                for kc in range(KC):
                    sap, stoks = src(kc, c0, c1)
                    P.I("pe", "matmul", dict(out=ps[:m, :c1 - c0], lhsT=wt[:kp, kc, j:j + m], rhs=sap, start=(kc == 0), stop=(kc == KC - 1)),
                         reads=[wt] + stoks, writes=[ps])
                evac(n0 + (nb + j) // 128, m, c0, c1, ps)


def build_mod():
    P = Prog()
    c = P.dram_in("c", [1, 2048])
    w = P.dram_in("ada_w", [4, 2048, 1536])
    b = P.dram_in("ada_b", [4, 1536])
    o = P.dram_out("mod", [4, 1536])
    c_sb = P.sb("c_sb", [128, 16])
    sc = P.sb("sc", [128, 16])
    P.dma("sp", c_sb[:, :], c[0, :].rearrange("(k p) -> p k", p=128), writes=[c_sb], allow_slow_non_contiguous=True)
    P.I("act", "activation", dict(out=sc[:, :], in_=c_sb[:, :], func=AF.Silu), reads=[c_sb], writes=[sc])
    bsb = P.sb("bsb", [1, 4 * 1536])
    P.dma("sp", bsb[:, :], b.ap().rearrange("l n -> (l n)")[None, :], writes=[bsb])
    res = P.sb("res", [1, 4 * 1536])
    wt = [P.sb(f"wt{i}", [128, 4, 1536]) for i in range(3)]
    pss = [P.ps(f"ps{i}", [1, 512]) for i in range(6)]
    n = 0
    for l in range(4):
        for kg in range(4):
            t = wt[n % 3]
            n += 1
            P.dma("sp", t[:, :, :], w[l, kg * 512:(kg + 1) * 512, :].rearrange("(k p) n -> p k n", p=128), writes=[t])
            for nb in range(3):
                ps = pss[(l % 2) * 3 + nb]
                for k in range(4):
                    kc = kg * 4 + k
                    P.I("pe", "matmul", dict(out=ps[:, :], lhsT=sc[:, kc:kc + 1], rhs=t[:, k, nb * 512:(nb + 1) * 512], start=(kc == 0), stop=(kc == 15)),
                         reads=[sc, t], writes=[ps])
        for nb in range(3):
            ps = pss[(l % 2) * 3 + nb]
            off = l * 1536 + nb * 512
            P.I("dve", "tensor_tensor", dict(out=res[:, off:off + 512], in0=ps[:, :], in1=bsb[:, off:off + 512], op=ALU.add), reads=[ps, bsb], writes=[res])
    P.dma("sp", o.ap().rearrange("l n -> (l n)")[None, :], res[:, :], reads=[res])
    return P.build()


def moe_inputs(P, L):
    D = Ctx()
    D.rw = P.dram_in("moe_rw", [2048, 32])
    D.rb = P.dram_in("moe_rb", [1, 32])
    D.wgu = P.dram_in("moe_wgu", [32, 2048, 1536], BF16)
    D.bgu = P.dram_in("moe_bgu", [128, 32 * 12])
    D.wd = P.dram_in("moe_wd", [32, 768, 2048], BF16)
    D.bd = P.dram_in("moe_bd", [32, 2048])
    D.ident = P.dram_in("ident", [128, 128])
    return D


SL128 = [(i * 128, (i + 1) * 128) for i in range(8)]


def moe_tail(P, C, D, xh, xtoks, vecs, iv, wring, hbf, hbt, final_gain_idx=None, out_dram=None):
    A2 = make_AB(P, vecs, iv["gain2"], iv["sc2"], iv["sh2"])
    S = norm_scratch(P, True, 128)
    ident = P.sb("ident", [128, 128], F32)
    P.dma("sp", ident[:, :], D.ident[:, :], writes=[ident])
    rw = P.sb("rw", [128, 16, 32], F32)
    P.dma("sp", rw[:, :, :], D.rw.ap().rearrange("(k p) e -> p k e", p=128), writes=[rw], allow_slow_non_contiguous=True)
    rb = P.sb("rb", [128, 32], F32)
    P.dma("sp", rb[:, :], D.rb.ap().partition_broadcast(128), writes=[rb], allow_slow_non_contiguous=True)
    bgu = P.sb("bgu", [128, 32 * 12], F32)
    P.dma("sp", bgu[:, :], D.bgu[:, :], writes=[bgu])
    bd = P.sb("bd", [32, 2048], F32)
    P.dma("sp", bd[:, :], D.bd[:, :], writes=[bd])
    gT = P.sb("gT", [32, TOK], F32)
    sm = [P.sb(f"rt_sm{i}", [128, 64], F32) for i in range(6)]

    def router(c0, c1):
        for tt in range(c0 // 128, c1 // 128):
            o = tt * 128 - c0
            ps = C.ps.next()
            for kc in range(16):
                P.I("pe", "matmul", dict(out=ps[:, :32], lhsT=S.tmp32[:, kc, o:o + 128], rhs=rw[:, kc, :], start=(kc == 0), stop=(kc == 15)),
                     reads=[S.tmp32_toks[kc], rw], writes=[ps])
            lg, mx, ex, mk, sm_, gt = sm
            P.I("dve", "tensor_tensor", dict(out=lg[:, :32], in0=ps[:, :32], in1=rb[:, :], op=ALU.add), reads=[ps, rb], writes=[lg])
            P.I("dve", "max", dict(out=mx[:, :8], in_=lg[:, :32]), reads=[lg], writes=[mx])
            P.I("dve", "tensor_scalar", dict(out=mk[:, :32], in0=lg[:, :32], scalar1=mx[:, 3:4], scalar2=None, op0=ALU.is_ge), reads=[lg, mx], writes=[mk])
            P.I("dve", "tensor_scalar", dict(out=mx[:, 8:9], in0=mx[:, 0:1], scalar1=-1.0, scalar2=None, op0=ALU.mult), reads=[mx], writes=[mx])
            P.I("act", "activation", dict(out=ex[:, :32], in_=lg[:, :32], func=AF.Exp, bias=mx[:, 8:9], scale=1.0), reads=[lg, mx], writes=[ex])
            P.I("dve", "tensor_tensor", dict(out=ex[:, :32], in0=ex[:, :32], in1=mk[:, :32], op=ALU.mult), reads=[ex, mk], writes=[ex])
            P.I("dve", "reduce_sum", dict(out=sm_[:, 0:1], in_=ex[:, :32], axis=AX.X), reads=[ex], writes=[sm_])
            P.I("dve", "reciprocal", dict(out=sm_[:, 1:2], in_=sm_[:, 0:1]), reads=[sm_], writes=[sm_])
            P.I("dve", "tensor_scalar", dict(out=gt[:, :32], in0=ex[:, :32], scalar1=sm_[:, 1:2], scalar2=None, op0=ALU.mult), reads=[ex, sm_], writes=[gt])
            ps2 = C.ps.next()
            P.I("pe", "matmul", dict(out=ps2[:32, :128], lhsT=gt[:, :32], rhs=ident[:, :], start=True, stop=True), reads=[gt, ident], writes=[ps2])
            P.I("act", "copy", dict(out=gT[:, tt * 128:(tt + 1) * 128], in_=ps2[:32, :128]), reads=[ps2], writes=[gT])

    normmod(P, C, xh, xtoks, A2, vecs, iv["sh2"], hbf, hbt, S, half_hook=router, cols=SL128)

    Ge = [P.sb(f"Ge{i}", [128, TOK], F32) for i in range(1)]
    hT = [P.nc.alloc_sbuf_tensor(f"hT{i}", [128, 6, TOK], BF16) for i in range(1)]
    hTt = [[P.tok(f"hT{i}_{f}") for f in range(6)] for i in range(1)]
    sg = Ring([P.sb(f"sw_g{i}", [128, 512], F32) for i in range(1)])
    ss = Ring([P.sb(f"sw_s{i}", [128, 512], F32) for i in range(1)])
    su = Ring([P.sb(f"sw_u{i}", [128, 512], F32) for i in range(1)])
    g2i = iv["g2"]

    def src_h(kc, c0, c1):
        return hbf[:, kc, c0:c1], [hbt[kc]]

    for ex in range(32):
        G = Ge[0]
        hTe = hT[0]
        hTet = hTt[0]
        for (c0, c1) in HALVES:
            ps = C.ps.next()
            P.I("pe", "matmul", dict(out=ps[:, :], lhsT=ident[:32, ex:ex + 1].to_broadcast([32, 128]), rhs=gT[:, c0:c1], start=True, stop=True), reads=[ident, gT], writes=[ps])
            P.I("act", "copy", dict(out=G[:, c0:c1], in_=ps[:, :]), reads=[ps], writes=[G])
        for blk in range(3):
            wt = wring.next()
            P.dma("sp", wt[:, :, :], D.wgu[ex, :, blk * 512:(blk + 1) * 512].rearrange("(k p) n -> p k n", p=128), writes=[wt])
            for c2 in range(2):
                fc = blk * 2 + c2
                for (c0, c1) in HALVES:
                    psg = C.ps.next()
                    psu = C.ps.next()
                    for kc in range(16):
                        P.I("pe", "matmul", dict(out=psg[:, :], lhsT=wt[:, kc, c2 * 256:c2 * 256 + 256:2], rhs=hbf[:, kc, c0:c1], start=(kc == 0), stop=(kc == 15)),
                             reads=[wt, hbt[kc]], writes=[psg])
                    for kc in range(16):
                        P.I("pe", "matmul", dict(out=psu[:, :], lhsT=wt[:, kc, c2 * 256 + 1:c2 * 256 + 256:2], rhs=hbf[:, kc, c0:c1], start=(kc == 0), stop=(kc == 15)),
                             reads=[wt, hbt[kc]], writes=[psu])
                    bi = (ex * 6 + fc) * 2
                    tg, ts, tu = sg.next(), ss.next(), su.next()
                    P.I("dve", "tensor_scalar", dict(out=tg[:, :], in0=psg[:, :], scalar1=bgu[:, bi:bi + 1], scalar2=7.0, op0=ALU.add, op1=ALU.min), reads=[psg, bgu], writes=[tg])
                    P.I("act", "activation", dict(out=ts[:, :], in_=tg[:, :], func=AF.Sigmoid, scale=1.702), reads=[tg], writes=[ts])
                    P.I("dve", "tensor_scalar", dict(out=tu[:, :], in0=psu[:, :], scalar1=bgu[:, bi + 1:bi + 2], scalar2=7.0, op0=ALU.add, op1=ALU.min), reads=[psu, bgu], writes=[tu])
                    P.I("pool", "tensor_scalar", dict(out=tu[:, :], in0=tu[:, :], scalar1=-7.0, scalar2=1.0, op0=ALU.max, op1=ALU.add), reads=[tu], writes=[tu])
                    P.I("pool", "tensor_tensor", dict(out=tg[:, :], in0=tg[:, :], in1=ts[:, :], op=ALU.mult), reads=[tg, ts], writes=[tg])
                    P.I("dve", "tensor_tensor", dict(out=tu[:, :], in0=tu[:, :], in1=tg[:, :], op=ALU.mult), reads=[tg, tu], writes=[tu])
                    P.I("pool", "tensor_tensor", dict(out=hTe[:, fc, c0:c1], in0=tu[:, :], in1=G[:, c0:c1], op=ALU.mult), reads=[tu, G], writes=[hTet[fc]])
        for blk in range(4):
            wt = wring.next()
            P.dma("sp", wt[:, :6, :], D.wd[ex, :, blk * 512:(blk + 1) * 512].rearrange("(k p) n -> p k n", p=128), writes=[wt])
            for j in range(4):
                n = blk * 4 + j
                for (c0, c1) in HALVES:
                    ps = C.ps.next()
                    for f in range(6):
                        P.I("pe", "matmul", dict(out=ps[:, :], lhsT=wt[:, f, j * 128:(j + 1) * 128], rhs=hTe[:, f, c0:c1], start=(f == 0), stop=(f == 5)),
                             reads=[wt, hTet[f]], writes=[ps])
                    P.I("dve", "scalar_tensor_tensor", dict(out=xh[:, n, c0:c1], in0=ps[:, :], scalar=vecs[:, g2i, n:n + 1], in1=xh[:, n, c0:c1], op0=ALU.mult, op1=ALU.add),
                         reads=[ps, vecs, xtoks[n]], writes=[xtoks[n]])
    for n in range(16):
        for (c0, c1) in HALVES:
            ps = C.ps.next()
            P.I("pe", "matmul", dict(out=ps[:, :], lhsT=bd[:, n * 128:(n + 1) * 128], rhs=gT[:, c0:c1], start=True, stop=True), reads=[bd, gT], writes=[ps])
            P.I("dve", "scalar_tensor_tensor", dict(out=xh[:, n, c0:c1], in0=ps[:, :], scalar=vecs[:, g2i, n:n + 1], in1=xh[:, n, c0:c1], op0=ALU.mult, op1=ALU.add),
                 reads=[ps, vecs, xtoks[n]], writes=[xtoks[n]])
    if final_gain_idx is not None:
        for (c0, c1) in SL128:
            w = c1 - c0
            for kc in range(16):
                P.I("act", "activation", dict(out=S.sq[:, kc, :w], in_=xh[:, kc, c0:c1], func=AF.Square), reads=[xtoks[kc]], writes=[S.sq])
            ps = C.ps.next()
            for kc in range(16):
                P.I("pe", "matmul", dict(out=ps[:, :w], lhsT=C.ones_bf[:, :], rhs=S.sq[:, kc, :w], start=(kc == 0), stop=(kc == 15)), reads=[C.ones_bf, S.sq], writes=[ps])
            rstd_from_ps(P, ps, w, 1.0 / 2048, 1e-6, S.rstd)
            for kc in range(16):
                P.I("dve", "scalar_tensor_tensor", dict(out=xh[:, kc, c0:c1], in0=xh[:, kc, c0:c1], scalar=vecs[:, final_gain_idx, kc:kc + 1], in1=S.rstd[:, :w], op0=ALU.mult, op1=ALU.mult),
                     reads=[xtoks[kc], vecs, S.rstd], writes=[xtoks[kc]])
    for kc in range(16):
        P.dma("sp", out_dram[kc * 128:(kc + 1) * 128, :], xh[:, kc, :], reads=[xtoks[kc]])


def build_t1m():
    P = Prog()
    C = setup_common(P)
    x = P.dram_in("xT", [2048, TOK])
    vd = P.dram_in("vecs", [128, 3, 16])
    w = P.dram_in("w_in", [2048, 6152])
    bgd = P.dram_in("b_gates", [1, 8])
    trid = P.dram_in("triu", [128, 128])
    qkv = P.dram_out("qkvT", [4096, TOK])
    og = P.dram_out("ogT", [2048, TOK])
    ab = P.dram_out("ab", [TOK, 12])
    xh, xt = load_x(P, x)
    vecs = P.sb("vecs", [128, 3, 16])
    P.dma("sp", vecs[:, :, :], vd[:, :, :], writes=[vecs])
    A = make_AB(P, vecs, 0, 1, 2)
    S = norm_scratch(P, False)
    hbf = P.nc.alloc_sbuf_tensor("hbf", [128, 16, TOK], BF16)
    hbt = [P.tok(f"hbf{k}") for k in range(16)]
    normmod(P, C, xh, xt, A, vecs, 2, hbf, hbt, S)
    wring = Ring([P.sb(f"wt{i}", [128, 16, 512], BF16) for i in range(2)])
    stg = Ring([P.sb(f"stg{i}", [128, 512], F32) for i in range(4)])

    def src(kc, c0, c1):
        return hbf[:, kc, c0:c1], [hbt[kc]]

    def evac(n, m, c0, c1, ps):
        t = stg.next()
        if n < 8:
            P.I("act", "mul", dict(out=t[:, :], in_=ps[:, :], mul=1.0 / 16.0), reads=[ps], writes=[t])
        elif n < 32:
            P.I("dve", "tensor_copy", dict(out=t[:, :], in_=ps[:, :]), reads=[ps], writes=[t])
        else:
            P.I("act", "activation", dict(out=t[:, :], in_=ps[:, :], func=AF.Sigmoid), reads=[ps], writes=[t])
        if n < 32:
            P.dma("sp", qkv[n * 128:(n + 1) * 128, c0:c1], t[:, :], reads=[t])
        else:
            P.dma("sp", og[(n - 32) * 128:(n - 31) * 128, c0:c1], t[:, :], reads=[t])

    linear_fm(P, C, w[:, 0:6144], 2048, 6144, src, evac, wring)
    wg = P.sb("wg", [128, 16, 8], BF16)
    P.dma("pool", wg[:, :, :], w[:, 6144:6152].rearrange("(k p) n -> p k n", p=128), writes=[wg], allow_slow_non_contiguous=True)
    bg = P.sb("bg", [128, 8], F32)
    P.dma("sp", bg[:, :], bgd.ap().partition_broadcast(128), writes=[bg], allow_slow_non_contiguous=True)
    tri = P.sb("tri", [128, 128], F32)
    P.dma("sp", tri[:, :], trid[:, :], writes=[tri])
    for tt in range(8):
        ps = C.ps.next()
        for kc in range(16):
            P.I("pe", "matmul", dict(out=ps[:, :8], lhsT=hbf[:, kc, tt * 128:(tt + 1) * 128], rhs=wg[:, kc, :], start=(kc == 0), stop=(kc == 15)), reads=[hbt[kc], wg], writes=[ps])
        g = P.sb(f"gt{tt}", [128, 40], F32)
        P.I("dve", "tensor_tensor", dict(out=g[:, 0:8], in0=ps[:, :8], in1=bg[:, :], op=ALU.add), reads=[ps, bg], writes=[g])
        P.I("act", "activation", dict(out=g[:, 0:8], in_=g[:, 0:8], func=AF.Tanh, scale=1.0 / 15.0), reads=[g], writes=[g])
        P.I("act", "activation", dict(out=g[:, 8:12], in_=g[:, 4:8], func=AF.Exp, scale=-15.0), reads=[g], writes=[g])
        P.I("dve", "tensor_scalar", dict(out=g[:, 8:12], in0=g[:, 8:12], scalar1=1.0, scalar2=None, op0=ALU.add), reads=[g], writes=[g])
        P.I("act", "activation", dict(out=g[:, 8:12], in_=g[:, 8:12], func=AF.Ln), reads=[g], writes=[g])
        ps2 = C.ps.next()
        P.I("pe", "matmul", dict(out=ps2[:, 0:4], lhsT=tri[:, :], rhs=g[:, 8:12], start=True, stop=True), reads=[tri, g], writes=[ps2])
        ps3 = C.ps.next()
        P.I("pe", "matmul", dict(out=ps3[:, 0:4], lhsT=C.ones_f[:, :], rhs=g[:, 8:12], start=True, stop=True), reads=[C.ones_f, g], writes=[ps3])
        P.I("act", "activation", dict(out=g[:, 16:20], in_=ps2[:, 0:4], func=AF.Exp, scale=-1.0), reads=[ps2], writes=[g])
        P.I("dve", "scalar_tensor_tensor", dict(out=g[:, 12:16], in0=g[:, 0:4], scalar=15.0, in1=ps2[:, 0:4], op0=ALU.mult, op1=ALU.add), reads=[ps2, g], writes=[g])
        P.I("act", "activation", dict(out=g[:, 20:24], in_=g[:, 12:16], func=AF.Exp), reads=[g], writes=[g])
        P.I("act", "activation", dict(out=g[:, 24:28], in_=ps3[:, 0:4], func=AF.Exp, scale=-1.0), reads=[ps3], writes=[g])
        P.dma("sp", ab[tt * 128:(tt + 1) * 128, :], g[:, 16:28], reads=[g])
    return P.build()


def build_mixm():
    P = Prog()
    C = setup_common(P)
    S_ = 8192
    qd = P.dram_in("qT", [256, S_])
    kd = P.dram_in("kT", [256, S_])
    ktd = P.dram_in("ktok", [S_, 256])
    vtd = P.dram_in("vtok", [S_, 256])
    abd = P.dram_in("abg", [128, 192])
    trid = P.dram_in("triu", [128, 128])
    out = P.dram_out("hh", [S_, 256])
    NP = 8
    pw = S_ // NP
    qr = [P.sb(f"qb{i}", [128, 2, pw], F32) for i in range(2)]
    kr = [P.sb(f"kb{i}", [128, 2, pw], F32) for i in range(2)]
    ktr = [P.sb(f"ktb{i}", [128, 8, 256], F32) for i in range(2)]
    vr = [P.sb(f"vb{i}", [128, 8, 256], F32) for i in range(2)]
    abg = P.sb("abg", [128, 192], F32)
    P.dma("sp", abg[:, :], abd[:, :], writes=[abg])
    tri = P.sb("tri", [128, 128], F32)
    P.dma("sp", tri[:, :], trid[:, :], writes=[tri])
    C32 = P.sb("C32", [128, 2, 256], F32)
    n32 = P.sb("n32", [128, 2], F32)
    for t_ in (C32, n32):
        P.I("dve", "memset", dict(ap=t_[:], constant=0.0), writes=[t_])
    onec = P.sb("onec", [128, 1], F32)
    P.I("dve", "memset", dict(ap=onec[:, :], constant=1.0), writes=[onec])
    STr = Ring([P.sb(f"ST{i}", [128, 128], F32) for i in range(2)])
    kpr = Ring([P.sb(f"kp{i}", [128, 256], F32) for i in range(2)])
    smr = Ring([P.sb(f"sm{i}", [128, 4], F32) for i in range(2)])
    hor = Ring([P.sb(f"ho{i}", [128, 256], F32) for i in range(2)])
    tmpc = P.sb("tmpc", [128, 2, 256], F32)
    for n in range(64):
        pi = n // 8
        ci = n % 8
        qb, kb, ktb, vb = qr[pi % 2], kr[pi % 2], ktr[pi % 2], vr[pi % 2]
        if ci == 0:
            P.dma("sp", qb[:, :, :], qd[:, pi * pw:(pi + 1) * pw].rearrange("(k p) t -> p k t", p=128), writes=[qb])
            P.dma("sp", kb[:, :, :], kd[:, pi * pw:(pi + 1) * pw].rearrange("(k p) t -> p k t", p=128), writes=[kb])
            P.dma("sp", ktb[:, :, :], ktd[pi * pw:(pi + 1) * pw, :].rearrange("(n j) d -> j n d", j=128), writes=[ktb])
            P.dma("sp", vb[:, :, :], vtd[pi * pw:(pi + 1) * pw, :].rearrange("(n j) d -> j n d", j=128), writes=[vb])
        a_ap = abg[:, n * 3 + 0:n * 3 + 1]
        b_ap = abg[:, n * 3 + 1:n * 3 + 2]
        G_ap = abg[:, n * 3 + 2:n * 3 + 3]
        sl = slice(ci * 128, (ci + 1) * 128)
        osl = slice(n * 128, (n + 1) * 128)
        ps = C.ps.next()
        for kc in range(2):
            P.I("pe", "matmul", dict(out=ps[:, :128], lhsT=kb[:, kc, sl], rhs=qb[:, kc, sl], start=(kc == 0), stop=(kc == 1)), reads=[kb, qb], writes=[ps])
        ST = STr.next()
        P.I("dve", "scalar_tensor_tensor", dict(out=ST[:, :], in0=ps[:, :128], scalar=b_ap, in1=tri[:, :], op0=ALU.mult, op1=ALU.mult), reads=[ps, abg, tri], writes=[ST])
        pn = C.ps.next()
        P.I("pe", "matmul", dict(out=pn[:, :256], lhsT=ST[:, :], rhs=vb[:, ci, :], start=True, stop=False), reads=[ST, vb], writes=[pn])
        for kc in range(2):
            P.I("pe", "matmul", dict(out=pn[:, :256], lhsT=qb[:, kc, sl], rhs=C32[:, kc, :], start=False, stop=(kc == 1)), reads=[qb, C32], writes=[pn])
        pd = C.ps.next()
        P.I("pe", "matmul", dict(out=pd[:, :1], lhsT=ST[:, :], rhs=onec[:, :], start=True, stop=False), reads=[ST, onec], writes=[pd])
        for kc in range(2):
            P.I("pe", "matmul", dict(out=pd[:, :1], lhsT=qb[:, kc, sl], rhs=n32[:, kc:kc + 1], start=False, stop=(kc == 1)), reads=[qb, n32], writes=[pd])
        sm = smr.next()
        P.I("act", "activation", dict(out=sm[:, 3:4], in_=pd[:, :1], func=AF.Abs, scale=a_ap), reads=[pd, abg], writes=[sm])
        P.I("dve", "tensor_scalar", dict(out=sm[:, 0:1], in0=sm[:, 3:4], scalar1=1.0, scalar2=None, op0=ALU.max), reads=[sm], writes=[sm])
        P.I("dve", "reciprocal", dict(out=sm[:, 1:2], in_=sm[:, 0:1]), reads=[sm], writes=[sm])
        P.I("dve", "tensor_tensor", dict(out=sm[:, 2:3], in0=sm[:, 1:2], in1=a_ap, op=ALU.mult), reads=[sm, abg], writes=[sm])
        ho = hor.next()
        P.I("act", "mul", dict(out=ho[:, :], in_=pn[:, :256], mul=sm[:, 2:3]), reads=[pn, sm], writes=[ho])
        P.dma("sp", out[osl, :], ho[:, :], reads=[ho])
        kp = kpr.next()
        P.I("dve", "tensor_scalar", dict(out=kp[:, :], in0=ktb[:, ci, :], scalar1=b_ap, scalar2=None, op0=ALU.mult), reads=[ktb, abg], writes=[kp])
        for kc in range(2):
            pc = C.ps.next()
            P.I("pe", "matmul", dict(out=pc[:, :256], lhsT=kp[:, kc * 128:(kc + 1) * 128], rhs=vb[:, ci, :], start=True, stop=True), reads=[kp, vb], writes=[pc])
            P.I("pe", "matmul", dict(out=pc[:, 256:257], lhsT=kp[:, kc * 128:(kc + 1) * 128], rhs=onec[:, :], start=True, stop=True), reads=[kp, onec], writes=[pc])
            P.I("dve", "tensor_tensor", dict(out=tmpc[:, kc, :], in0=C32[:, kc, :], in1=pc[:, :256], op=ALU.add), reads=[pc, C32], writes=[tmpc])
            P.I("dve", "tensor_scalar", dict(out=C32[:, kc, :], in0=tmpc[:, kc, :], scalar1=G_ap, scalar2=None, op0=ALU.mult), reads=[tmpc, abg], writes=[C32])
            P.I("dve", "tensor_tensor", dict(out=n32[:, kc:kc + 1], in0=n32[:, kc:kc + 1], in1=pc[:, 256:257], op=ALU.add), reads=[pc, n32], writes=[n32])
            P.I("dve", "tensor_scalar", dict(out=n32[:, kc:kc + 1], in0=n32[:, kc:kc + 1], scalar1=G_ap, scalar2=None, op0=ALU.mult), reads=[n32, abg], writes=[n32])
    return P.build()


def t2_common_inputs(P, nv):
    x = P.dram_in("xT", [2048, TOK])
    vd = P.dram_in("vecs", [128, nv, 16])
    out = P.dram_out("xout", [2048, TOK])
    xh, xt = load_x(P, x)
    vecs = P.sb("vecs", [128, nv, 16])
    P.dma("sp", vecs[:, :, :], vd[:, :, :], writes=[vecs])
    return xh, xt, vecs, out


def outproj_residual(P, C, wo, zbf, zt, xh, xt, vecs, ig1, wring):
    def src(kc, c0, c1):
        return zbf[:, kc, c0:c1], [zt[kc]]

    def evac(n, m, c0, c1, ps):
        P.I("dve", "scalar_tensor_tensor", dict(out=xh[:, n, c0:c1], in0=ps[:, :], scalar=vecs[:, ig1, n:n + 1], in1=xh[:, n, c0:c1], op0=ALU.mult, op1=ALU.add),
             reads=[ps, vecs, xt[n]], writes=[xt[n]])

    linear_fm(P, C, wo[:, :], 2048, 2048, src, evac, wring)


def build_t2m(final):
    P = Prog()
    C = setup_common(P)
    xh, xt, vecs, out = t2_common_inputs(P, 7)
    hhd = P.dram_in("hhT", [2048, TOK])
    ogd = P.dram_in("ogT", [2048, TOK])
    wo = P.dram_in("w_out", [2048, 2048])
    D = moe_inputs(P, 0)
    wring = Ring([P.sb(f"wt{i}", [128, 16, 512], BF16) for i in range(2)])
    zbf = P.nc.alloc_sbuf_tensor("zbf", [128, 16, TOK], BF16)
    zt = [P.tok(f"z{k}") for k in range(16)]
    hr = Ring([P.sb(f"hh{i}", [128, TOK], F32) for i in range(2)])
    orr = Ring([P.sb(f"og{i}", [128, TOK], F32) for i in range(2)])
    rstd = P.sb("rstd_h", [128, TOK], F32)
    for h in range(4):
        pss = [C.ps.next(), C.ps.next()]
        for k4 in range(4):
            kc = h * 4 + k4
            t = hr.next()
            P.dma("sp", t[:, :], hhd[kc * 128:(kc + 1) * 128, :], writes=[t])
            P.I("act", "activation", dict(out=zbf[:, kc, :], in_=t[:, :], func=AF.Square), reads=[t], writes=[zt[kc]])
            for hi, (c0, c1) in enumerate(HALVES):
                P.I("pe", "matmul", dict(out=pss[hi][:, :], lhsT=C.ones_bf[:, :], rhs=zbf[:, kc, c0:c1], start=(k4 == 0), stop=(k4 == 3)), reads=[C.ones_bf, zt[kc]], writes=[pss[hi]])
        for hi, (c0, c1) in enumerate(HALVES):
            ps = pss[hi]
            P.I("dve", "tensor_scalar", dict(out=rstd[:, c0:c1], in0=ps[:, :], scalar1=1.0 / 512, scalar2=1e-6, op0=ALU.mult, op1=ALU.add), reads=[ps], writes=[rstd])
        P.I("act", "activation", dict(out=rstd[:, :], in_=rstd[:, :], func=AF.Sqrt), reads=[rstd], writes=[rstd])
        P.I("dve", "reciprocal", dict(out=rstd[:, :], in_=rstd[:, :]), reads=[rstd], writes=[rstd])
        for k4 in range(4):
            kc = h * 4 + k4
            t = hr.next()
            o = orr.next()
            P.dma("sp", t[:, :], hhd[kc * 128:(kc + 1) * 128, :], writes=[t])
            P.dma("sp", o[:, :], ogd[kc * 128:(kc + 1) * 128, :], writes=[o])
            P.I("dve", "scalar_tensor_tensor", dict(out=t[:, :], in0=t[:, :], scalar=vecs[:, 0, kc:kc + 1], in1=rstd[:, :], op0=ALU.mult, op1=ALU.mult), reads=[t, vecs, rstd], writes=[t])
            P.I("pool", "tensor_tensor", dict(out=zbf[:, kc, :], in0=t[:, :], in1=o[:, :], op=ALU.mult), reads=[t, o], writes=[zt[kc]])
    outproj_residual(P, C, wo, zbf, zt, xh, xt, vecs, 1, wring)
    iv = dict(gain2=2, sc2=3, sh2=4, g2=5)
    moe_tail(P, C, D, xh, xt, vecs, iv, wring, zbf, zt, final_gain_idx=(6 if final else None), out_dram=out)
    return P.build()


_PROGS = {}
DEBUG = {}


def _prog(name, builder, *a):
    key = (name,) + a
    if key not in _PROGS:
        _PROGS[key] = builder(*a)
    return _PROGS[key]


def _run(nc, in_maps):
    res = run_bass_kernel_spmd(nc, in_maps, core_ids=list(range(NCORES)))
    return res.results


def _c(a):
    return np.ascontiguousarray(a, dtype=np.float32)


def vlay(vs):
    return _c(np.stack([np.asarray(v, np.float32).reshape(16, 128).T for v in vs], axis=1))


def _consts():
    return dict(triu=_c(np.triu(np.ones((128, 128), np.float32))), ident=_c(np.eye(128, dtype=np.float32)))


def run_mod(inp):
    nc = _prog("mod", build_mod)
    w = inp["ada_w"]
    b = inp["ada_b"]
    maps = [{"c": _c(inp["c"]), "ada_w": _c(w[:, :, i * 1536:(i + 1) * 1536]), "ada_b": _c(b[:, i * 1536:(i + 1) * 1536])} for i in range(NCORES)]
    r = _run(nc, maps)
    mod = np.concatenate([x["mod"] for x in r], axis=1)
    return mod.reshape(4, 6, 2048)


def build_wcast():
    P = Prog()
    wg = P.dram_in("wg", [16, 2048, 1536])
    wd = P.dram_in("wd", [16, 768, 2048])
    og = P.dram_out("wg_bf", [16, 2048, 1536], BF16)
    od = P.dram_out("wd_bf", [16, 768, 2048], BF16)
    tg = [P.sb(f"tg{i}", [128, 16, 1536], BF16) for i in range(2)]
    td = [P.sb(f"td{i}", [128, 6, 2048], BF16) for i in range(2)]
    for i in range(16):
        t = tg[i % 2]
        for g in range(4):
            P.dma("pool", t[:, g * 4:(g + 1) * 4, :], wg[i, g * 512:(g + 1) * 512, :].rearrange("(k p) n -> p k n", p=128), writes=[t])
        for g in range(4):
            P.dma("sp", og[i, g * 512:(g + 1) * 512, :].rearrange("(k p) n -> p k n", p=128), t[:, g * 4:(g + 1) * 4, :], reads=[t])
        u = td[i % 2]
        for g in range(2):
            P.dma("pool", u[:, g * 3:(g + 1) * 3, :], wd[i, g * 384:(g + 1) * 384, :].rearrange("(k p) n -> p k n", p=128), writes=[u])
        for g in range(2):
            P.dma("sp", od[i, g * 384:(g + 1) * 384, :].rearrange("(k p) n -> p k n", p=128), u[:, g * 3:(g + 1) * 3, :], reads=[u])
    return P.build()


_WBF = {}


def run_wcast(inp):
    wg = inp["moe_w_gate_up"]
    wd = inp["moe_w_down"]
    maps = []
    for c in range(NCORES):
        maps.append(dict(wg=_c(wg[:, 4 * c:4 * c + 4].reshape(16, 2048, 1536)), wd=_c(wd[:, 4 * c:4 * c + 4].reshape(16, 768, 2048))))
    r = _run(_prog("wcast", build_wcast), maps)
    for layer in range(4):
        _WBF[("g", layer)] = np.ascontiguousarray(np.concatenate([x["wg_bf"].reshape(4, 4, 2048, 1536)[layer] for x in r], axis=0))
        _WBF[("d", layer)] = np.ascontiguousarray(np.concatenate([x["wd_bf"].reshape(4, 4, 768, 2048)[layer] for x in r], axis=0))


def moe_maps(inp, layer):
    bgu = inp["moe_b_gate_up"][layer]
    bgu_l = _c(bgu.reshape(32, 6, 128, 2).transpose(2, 0, 1, 3).reshape(128, 32 * 12))
    return dict(moe_rw=_c(inp["moe_router_w"][layer]), moe_rb=_c(inp["moe_router_b"][layer][None, :]), moe_wgu=_WBF[("g", layer)],
                moe_bgu=bgu_l, moe_wd=_WBF[("d", layer)], moe_bd=_c(inp["moe_b_down"][layer]), ident=_consts()["ident"])


def layer_mlstm(inp, xs, mod, layer, j, final):
    sh1, sc1, g1, sh2, sc2, g2 = [mod[layer, i] for i in range(6)]
    cst = _consts()
    nc1 = _prog("t1m", build_t1m)
    v1 = vlay([inp["norm_gain"][layer, 0], sc1, sh1])
    w_in = _c(inp["mlstm_w_in"][j])
    bg = _c(inp["mlstm_b_gates"][j][None, :])
    r1 = _run(nc1, [dict(xT=xs[c], vecs=v1, w_in=w_in, b_gates=bg, triu=cst["triu"]) for c in range(NCORES)])
    qkvT = np.concatenate([r["qkvT"] for r in r1], axis=1)
    ab = np.concatenate([r["ab"] for r in r1], axis=0)
    if "on" in DEBUG:
        DEBUG["qkvT"] = qkvT; DEBUG["ab"] = ab; DEBUG["ogT"] = np.concatenate([r["ogT"] for r in r1], axis=1)
    maps = []
    for c in range(NCORES):
        h, half = c // 2, c % 2
        vrows = qkvT[2048 + h * 512 + half * 256: 2048 + h * 512 + half * 256 + 256]
        abh = np.stack([ab[:, h], ab[:, 4 + h], ab[:, 8 + h]], axis=1)
        abg = _c(abh.reshape(64, 128, 3).transpose(1, 0, 2).reshape(128, 192))
        maps.append(dict(qT=_c(qkvT[h * 256:(h + 1) * 256]), kT=_c(qkvT[1024 + h * 256:1024 + (h + 1) * 256]),
                         ktok=_c(qkvT[1024 + h * 256:1024 + (h + 1) * 256].T), vtok=_c(vrows.T), abg=abg, triu=cst["triu"]))
    r2 = _run(_prog("mixm", build_mixm), maps)
    hhT = np.concatenate([r["hh"].T for r in r2], axis=0)
    if "on" in DEBUG:
        DEBUG["hhT"] = hhT
        if "stop_after_mix" in DEBUG:
            return None
    v2 = vlay([inp["mlstm_norm_gain"][j], g1, inp["norm_gain"][layer, 1], sc2, sh2, g2, inp["final_gain"]])
    mm = moe_maps(inp, layer)
    wo = _c(inp["mlstm_w_out"][j])
    maps = [dict(xT=xs[c], vecs=v2, hhT=_c(hhT[:, c * TOK:(c + 1) * TOK]), ogT=r1[c]["ogT"], w_out=wo, **mm) for c in range(NCORES)]
    r3 = _run(_prog("t2m", build_t2m, final), maps)
    return [r["xout"] for r in r3]


import math
LAM_INIT2 = 0.8 - 0.6 * math.exp(-0.3 * 2)


def build_t1d():
    P = Prog()
    C = setup_common(P)
    x = P.dram_in("xT", [2048, TOK])
    vd = P.dram_in("vecs", [128, 3, 16])
    w = P.dram_in("w_qkv", [2048, 6144])
    posd = P.dram_in("pos", [1, TOK], I32)
    invfd = P.dram_in("invf", [128, 1])
    rotd = P.dram_in("rotT", [128, 128])
    qk = P.dram_out("qkT", [4096, TOK])
    vo = P.dram_out("vT", [2048, TOK])
    xh, xt = load_x(P, x)
    vecs = P.sb("vecs", [128, 3, 16])
    P.dma("sp", vecs[:, :, :], vd[:, :, :], writes=[vecs])
    A = make_AB(P, vecs, 0, 1, 2)
    S = norm_scratch(P, False)
    hbf = P.nc.alloc_sbuf_tensor("hbf", [128, 16, TOK], BF16)
    hbt = [P.tok(f"hbf{k}") for k in range(16)]
    normmod(P, C, xh, xt, A, vecs, 2, hbf, hbt, S)
    posi = P.sb("posi", [128, TOK], I32)
    P.dma("sp", posi[:, :], posd.ap().partition_broadcast(128), writes=[posi], allow_slow_non_contiguous=True)
    invf = P.sb("invf", [128, 1], F32)
    P.dma("sp", invf[:, :], invfd[:, :], writes=[invf])
    rot = P.sb("rot", [128, 128], F32)
    P.dma("sp", rot[:, :], rotd[:, :], writes=[rot])
    ang = P.sb("ang", [128, TOK], F32)
    cos = P.sb("cos", [128, TOK], F32)
    sin = P.sb("sin", [128, TOK], F32)
    P.I("dve", "tensor_copy", dict(out=ang[:, :], in_=posi[:, :]), reads=[posi], writes=[ang])
    P.I("dve", "tensor_scalar", dict(out=ang[:, :], in0=ang[:, :], scalar1=invf[:, 0:1], scalar2=None, op0=ALU.mult), reads=[ang, invf], writes=[ang])
    pit = P.sb("pit", [128, 1], F32)
    P.I("dve", "memset", dict(ap=pit[:, :], constant=math.pi), writes=[pit])
    ki = P.sb("ki", [128, TOK], I32)
    kf = P.sb("kf", [128, TOK], F32)
    for dst, off in ((sin, 0.0), (cos, 0.25)):
        P.I("dve", "tensor_scalar", dict(out=dst[:, :], in0=ang[:, :], scalar1=off, scalar2=None, op0=ALU.add), reads=[ang], writes=[dst])
        P.I("dve", "tensor_copy", dict(out=ki[:, :], in_=dst[:, :]), reads=[dst], writes=[ki])
        P.I("dve", "tensor_copy", dict(out=kf[:, :], in_=ki[:, :]), reads=[ki], writes=[kf])
        P.I("dve", "tensor_tensor", dict(out=dst[:, :], in0=dst[:, :], in1=kf[:, :], op=ALU.subtract), reads=[dst, kf], writes=[dst])
        P.I("dve", "tensor_scalar", dict(out=kf[:, :], in0=dst[:, :], scalar1=0.0, scalar2=None, op0=ALU.is_lt), reads=[dst], writes=[kf])
        P.I("dve", "tensor_tensor", dict(out=dst[:, :], in0=dst[:, :], in1=kf[:, :], op=ALU.add), reads=[dst, kf], writes=[dst])
        P.I("act", "activation", dict(out=dst[:, :], in_=dst[:, :], func=AF.Sin, bias=pit[:, 0:1], scale=-2.0 * math.pi), reads=[dst, pit], writes=[dst])
    wring = Ring([P.sb(f"wt{i}", [128, 16, 512], BF16) for i in range(2)])
    stg = Ring([P.sb(f"stg{i}", [128, 512], F32) for i in range(4)])
    xbr = Ring([P.sb(f"xb{i}", [128, 512], F32) for i in range(2)])
    t2r = Ring([P.sb(f"t2{i}", [128, 512], F32) for i in range(2)])

    def src(kc, c0, c1):
        return hbf[:, kc, c0:c1], [hbt[kc]]

    def evac(n, m, c0, c1, ps):
        t = stg.next()
        if n < 32:
            xb = xbr.next()
            if n < 16:
                P.I("act", "mul", dict(out=xb[:, :], in_=ps[:, :], mul=128.0 ** -0.5), reads=[ps], writes=[xb])
            else:
                P.I("act", "copy", dict(out=xb[:, :], in_=ps[:, :]), reads=[ps], writes=[xb])
            ps2 = C.ps.next()
            P.I("pe", "matmul", dict(out=ps2[:, :], lhsT=rot[:, :], rhs=xb[:, :], start=True, stop=True), reads=[rot, xb], writes=[ps2])
            t2 = t2r.next()
            P.I("dve", "tensor_tensor", dict(out=t2[:, :], in0=ps2[:, :], in1=sin[:, c0:c1], op=ALU.mult), reads=[ps2, sin], writes=[t2])
            P.I("dve", "tensor_tensor", dict(out=t[:, :], in0=xb[:, :], in1=cos[:, c0:c1], op=ALU.mult), reads=[xb, cos], writes=[t])
            P.I("dve", "tensor_tensor", dict(out=t[:, :], in0=t[:, :], in1=t2[:, :], op=ALU.add), reads=[t, t2], writes=[t])
            P.dma("sp", qk[n * 128:(n + 1) * 128, c0:c1], t[:, :], reads=[t])
        else:
            P.I("act", "copy", dict(out=t[:, :], in_=ps[:, :]), reads=[ps], writes=[t])
            P.dma("sp", vo[(n - 32) * 128:(n - 31) * 128, c0:c1], t[:, :], reads=[t])

    linear_fm(P, C, w[:, :], 2048, 6144, src, evac, wring)
    return P.build()


def build_mixd():
    P = Prog()
    C = setup_common(P, npsum=4)
    S_ = 8192
    qd = P.dram_in("qT", [256, S_])
    kd = P.dram_in("kT", [256, S_])
    vtd = P.dram_in("vtok", [S_, 256])
    lamd = P.dram_in("lam", [1, 512])
    sgd = P.dram_in("sgain", [1, 256])
    idd = P.dram_in("ident", [128, 128])
    mkd = P.dram_in("maskb", [128, 128])
    out = P.dram_out("attn", [S_, 256])
    scale = 128.0 ** -0.5
    qb = P.nc.alloc_sbuf_tensor("qb", [128, 2, S_], BF16)
    kb = P.nc.alloc_sbuf_tensor("kb", [128, 2, S_], BF16)
    vb = P.nc.alloc_sbuf_tensor("vb", [128, 64, 256], BF16)
    NP = 8
    pw = S_ // NP
    qt = [P.tok(f"q{i}") for i in range(NP)]
    kt = [P.tok(f"k{i}") for i in range(NP)]
    vt = [P.tok(f"v{i}") for i in range(NP)]
    for i in range(NP):
        P.dma("pool", qb[:, :, i * pw:(i + 1) * pw], qd[:, i * pw:(i + 1) * pw].rearrange("(k p) t -> p k t", p=128), writes=[qt[i]])
        P.dma("pool", kb[:, :, i * pw:(i + 1) * pw], kd[:, i * pw:(i + 1) * pw].rearrange("(k p) t -> p k t", p=128), writes=[kt[i]])
        P.dma("pool", vb[:, i * 8:(i + 1) * 8, :], vtd[i * pw:(i + 1) * pw, :].rearrange("(n j) d -> j n d", j=128), writes=[vt[i]])
    identb = P.sb("identb", [128, 128], BF16)
    P.dma("pool", identb[:, :], idd[:, :], writes=[identb])
    maskb = P.sb("maskb", [128, 128], F32)
    P.dma("sp", maskb[:, :], mkd[:, :], writes=[maskb])
    sg = P.sb("sg", [128, 256], F32)
    P.dma("sp", sg[:, :], sgd.ap().partition_broadcast(128), writes=[sg], allow_slow_non_contiguous=True)
    P.I("dve", "tensor_scalar", dict(out=sg[:, :], in0=sg[:, :], scalar1=1.0 - LAM_INIT2, scalar2=None, op0=ALU.mult), reads=[sg], writes=[sg])
    lp = P.sb("lp", [1, 4, 128], F32)
    P.dma("sp", lp[:, :, :], lamd.ap().rearrange("o (a b) -> o a b", a=4), writes=[lp])
    pr = P.sb("pr", [1, 2, 128], F32)
    P.I("dve", "tensor_tensor", dict(out=pr[:, :, :], in0=lp[:, 0::2, :], in1=lp[:, 1::2, :], op=ALU.mult), reads=[lp], writes=[pr])
    l2 = P.sb("l2", [1, 4], F32)
    P.I("dve", "reduce_sum", dict(out=l2[:, 0:2], in_=pr[:, :, :], axis=AX.X), reads=[pr], writes=[l2])
    P.I("act", "activation", dict(out=l2[:, 0:2], in_=l2[:, 0:2], func=AF.Exp), reads=[l2], writes=[l2])
    P.I("dve", "tensor_tensor", dict(out=l2[:, 2:3], in0=l2[:, 0:1], in1=l2[:, 1:2], op=ALU.subtract), reads=[l2], writes=[l2])
    P.I("dve", "tensor_scalar", dict(out=l2[:, 2:3], in0=l2[:, 2:3], scalar1=-1.0, scalar2=-LAM_INIT2, op0=ALU.mult, op1=ALU.add), reads=[l2], writes=[l2])
    psl = C.ps.next()
    P.I("pe", "matmul", dict(out=psl[:, 0:1], lhsT=C.ones_f[0:1, :], rhs=l2[0:1, 2:3], start=True, stop=True), reads=[C.ones_f, l2], writes=[psl])
    nlam = P.sb("nlam", [128, 1], F32)
    P.I("act", "copy", dict(out=nlam[:, :], in_=psl[:, 0:1]), reads=[psl], writes=[nlam])
    Sc = P.sb("Sc", [128, S_], F32)
    Pb = P.sb("Pb", [128, S_], BF16)
    pTr = Ring([P.ps(f"pT{i}", [128, 512], BF16) for i in range(2)])
    PTr = Ring([P.sb(f"PTs{i}", [128, 512], BF16) for i in range(3)])
    po = [P.ps(f"po{i}", [128, 512], F32) for i in range(2)]
    st = P.sb("st", [128, 16], F32)
    o1 = P.sb("o1", [128, 256], F32)
    o2 = P.sb("o2", [128, 256], F32)
    junk = P.sb("junk", [128, 256], F32)
    hor = Ring([P.sb(f"ho{i}", [128, 256], F32) for i in range(2)])
    ev = 0
    for qi in range(64):
        nk = (qi + 1) * 128
        qsl = slice(qi * 128, (qi + 1) * 128)
        qp = qi // 8
        for comp in range(2):
            for ck in range(0, nk, 512):
                w_ = min(512, nk - ck)
                ps = C.ps.next()
                rds = [qt[qp]] + [kt[i] for i in range(ck // pw, (ck + w_ - 1) // pw + 1)]
                P.I("pe", "matmul", dict(out=ps[:, :w_], lhsT=qb[:, comp, qsl], rhs=kb[:, comp, ck:ck + w_], start=True, stop=True), reads=rds, writes=[ps])
                last = (ck + w_ == nk)
                wf = w_ - 128 if last else w_
                if wf > 0:
                    eng = "act" if ev % 2 == 0 else "dve"
                    ev += 1
                    if eng == "act":
                        P.I("act", "copy", dict(out=Sc[:, ck:ck + wf], in_=ps[:, :wf]), reads=[ps], writes=[Sc])
                    else:
                        P.I("dve", "tensor_copy", dict(out=Sc[:, ck:ck + wf], in_=ps[:, :wf]), reads=[ps], writes=[Sc])
                if last:
                    P.I("dve", "tensor_tensor", dict(out=Sc[:, nk - 128:nk], in0=ps[:, w_ - 128:w_], in1=maskb[:, :], op=ALU.add), reads=[ps, maskb], writes=[Sc])
            P.I("dve", "reduce_max", dict(out=st[:, 0:1], in_=Sc[:, :nk], axis=AX.X), reads=[Sc], writes=[st])
            P.I("dve", "tensor_scalar", dict(out=st[:, 1:2], in0=st[:, 0:1], scalar1=-1.0, scalar2=None, op0=ALU.mult), reads=[st], writes=[st])
            P.I("act", "activation", dict(out=Pb[:, :nk], in_=Sc[:, :nk], func=AF.Exp, bias=st[:, 1:2], scale=1.0, accum_out=st[:, 2 + comp:3 + comp]), reads=[Sc, st], writes=[Pb, st])
            acc = po[comp]
            for g0 in range(0, qi + 1, 4):
                gn = min(4, qi + 1 - g0)
                pT = pTr.next()
                for b_ in range(gn):
                    kb_ = g0 + b_
                    P.I("pe", "transpose", dict(out=pT[:, b_ * 128:(b_ + 1) * 128], in_=Pb[:, kb_ * 128:(kb_ + 1) * 128], identity=identb[:, :]), reads=[Pb, identb], writes=[pT])
                PT = PTr.next()
                eng = "act" if ev % 2 == 0 else "dve"
                ev += 1
                if eng == "act":
                    P.I("act", "copy", dict(out=PT[:, :gn * 128], in_=pT[:, :gn * 128]), reads=[pT], writes=[PT])
                else:
                    P.I("dve", "tensor_copy", dict(out=PT[:, :gn * 128], in_=pT[:, :gn * 128]), reads=[pT], writes=[PT])
                for b_ in range(gn):
                    kb_ = g0 + b_
                    P.I("pe", "matmul", dict(out=acc[:, :256], lhsT=PT[:, b_ * 128:(b_ + 1) * 128], rhs=vb[:, kb_, :], start=(kb_ == 0), stop=(kb_ == qi)), reads=[PT, vt[kb_ // 8]], writes=[acc])
        P.I("dve", "reciprocal", dict(out=st[:, 4:6], in_=st[:, 2:4]), reads=[st], writes=[st])
        P.I("dve", "tensor_tensor", dict(out=st[:, 6:7], in0=st[:, 5:6], in1=nlam[:, :], op=ALU.mult), reads=[st, nlam], writes=[st])
        P.I("act", "mul", dict(out=o1[:, :], in_=po[0][:, :256], mul=st[:, 4:5]), reads=[po[0], st], writes=[o1])
        P.I("dve", "scalar_tensor_tensor", dict(out=o2[:, :], in0=po[1][:, :256], scalar=st[:, 6:7], in1=o1[:, :], op0=ALU.mult, op1=ALU.add), reads=[po[1], st, o1], writes=[o2])
        P.I("act", "activation", dict(out=junk[:, :], in_=o2[:, :], func=AF.Square, accum_out=st[:, 8:9]), reads=[o2], writes=[junk, st])
        P.I("dve", "tensor_scalar", dict(out=st[:, 9:10], in0=st[:, 8:9], scalar1=1.0 / 256, scalar2=1e-5, op0=ALU.mult, op1=ALU.add), reads=[st], writes=[st])
        P.I("act", "activation", dict(out=st[:, 9:10], in_=st[:, 9:10], func=AF.Sqrt), reads=[st], writes=[st])
        P.I("dve", "reciprocal", dict(out=st[:, 10:11], in_=st[:, 9:10]), reads=[st], writes=[st])
        ho = hor.next()
        P.I("dve", "scalar_tensor_tensor", dict(out=ho[:, :], in0=o2[:, :], scalar=st[:, 10:11], in1=sg[:, :], op0=ALU.mult, op1=ALU.mult), reads=[o2, st, sg], writes=[ho])
        P.dma("sp", out[qsl, :], ho[:, :], reads=[ho])
    return P.build()


def build_t2_simple(final, wname):
    P = Prog()
    C = setup_common(P)
    xh, xt, vecs, out = t2_common_inputs(P, 6)
    zd = P.dram_in("zT", [2048, TOK])
    wo = P.dram_in(wname, [2048, 2048])
    D = moe_inputs(P, 0)
    wring = Ring([P.sb(f"wt{i}", [128, 16, 512], BF16) for i in range(2)])
    zbf = P.nc.alloc_sbuf_tensor("zbf", [128, 16, TOK], BF16)
    zt = [P.tok(f"z{k}") for k in range(16)]
    for k in range(16):
        P.dma("pool", zbf[:, k, :], zd[k * 128:(k + 1) * 128, :], writes=[zt[k]])
    outproj_residual(P, C, wo, zbf, zt, xh, xt, vecs, 0, wring)
    iv = dict(gain2=1, sc2=2, sh2=3, g2=4)
    moe_tail(P, C, D, xh, xt, vecs, iv, wring, zbf, zt, final_gain_idx=(5 if final else None), out_dram=out)
    return P.build()


def layer_diff(inp, xs, mod, layer, j, final):
    sh1, sc1, g1, sh2, sc2, g2 = [mod[layer, i] for i in range(6)]
    v1 = vlay([inp["norm_gain"][layer, 0], sc1, sh1])
    pos = np.asarray(inp["positions"], np.int32)
    invf = np.zeros((128, 1), np.float32)
    fr = (500000.0 ** (-np.arange(0, 32, 2, dtype=np.float32) / 32)).astype(np.float32)
    invf[:16, 0] = fr / np.float32(2 * np.pi)
    invf[16:32, 0] = fr / np.float32(2 * np.pi)
    rotT = np.zeros((128, 128), np.float32)
    for m in range(16):
        rotT[m + 16, m] = -1.0
        rotT[m, m + 16] = 1.0
    w = _c(inp["diff_w_qkv"][j])
    r1 = _run(_prog("t1d", build_t1d), [dict(xT=xs[c], vecs=v1, w_qkv=w, pos=np.ascontiguousarray(pos[:, c * TOK:(c + 1) * TOK]), invf=invf, rotT=rotT) for c in range(NCORES)])
    qkT = np.concatenate([r["qkT"] for r in r1], axis=1)
    vT = np.concatenate([r["vT"] for r in r1], axis=1)
    if "on" in DEBUG:
        DEBUG["qkT"] = qkT; DEBUG["vT"] = vT
    maskb = _c(np.where(np.tril(np.ones((128, 128))) > 0, 0.0, -30000.0))
    cst = _consts()
    lam = _c(inp["diff_lambda"][j].reshape(1, 512))
    sg = _c(inp["diff_subln_gain"][j][None, :])
    maps = [dict(qT=_c(qkT[h * 256:(h + 1) * 256]), kT=_c(qkT[2048 + h * 256:2048 + (h + 1) * 256]), vtok=_c(vT[h * 256:(h + 1) * 256].T),
                 lam=lam, sgain=sg, ident=cst["ident"], maskb=maskb) for h in range(NCORES)]
    r2 = _run(_prog("mixd", build_mixd), maps)
    zT = np.concatenate([r["attn"].T for r in r2], axis=0)
    if "on" in DEBUG:
        DEBUG["zT"] = zT
        if "stop_after_mix" in DEBUG:
            return None
    v2 = vlay([g1, inp["norm_gain"][layer, 1], sc2, sh2, g2, inp["final_gain"]])
    mm = moe_maps(inp, layer)
    wo = _c(inp["diff_w_o"][j])
    maps = [dict(xT=xs[c], vecs=v2, zT=_c(zT[:, c * TOK:(c + 1) * TOK]), w_o=wo, **mm) for c in range(NCORES)]
    r3 = _run(_prog("t2s", build_t2_simple, final, "w_o"), maps)
    return [r["xout"] for r in r3]


NEG_E05 = -math.exp(-0.5)


def build_t1r():
    P = Prog()
    C = setup_common(P)
    x = P.dram_in("xT", [2048, TOK])
    xpd = P.dram_in("xprev", [128, 16])
    nfd = P.dram_in("notfirst", [128, 1])
    vd = P.dram_in("vecs", [128, 13, 16])
    wrkv = P.dram_in("w_rkv", [3, 2048, 2048])
    w1 = P.dram_in("w1", [2048, 96]); w2 = P.dram_in("w2", [96, 2048])
    a1 = P.dram_in("a1", [2048, 96]); a2 = P.dram_in("a2", [96, 2048])
    g1 = P.dram_in("g1", [2048, 256]); g2 = P.dram_in("g2", [256, 2048])
    blkd = P.dram_in("blk64", [128, 128])
    outs = {nm: P.dram_out(nm, [2048, TOK]) for nm in ("r", "k2", "v", "lw", "kk", "kka", "gate")}
    a_scr = P.dram("a_scr", [2048, TOK])
    xh, xt = load_x(P, x)
    vecs = P.sb("vecs", [128, 13, 16])
    P.dma("sp", vecs[:, :, :], vd[:, :, :], writes=[vecs])
    A = make_AB(P, vecs, 0, 1, 2)
    blk = P.sb("blk", [128, 128], BF16)
    P.dma("pool", blk[:, :], blkd[:, :], writes=[blk])
    xp = P.sb("xp", [128, 16], F32)
    P.dma("sp", xp[:, :], xpd[:, :], writes=[xp])
    nf = P.sb("nf", [128, 1], F32)
    P.dma("sp", nf[:, :], nfd[:, :], writes=[nf])
    xq = P.sb("xq", [128, 16], F32)
    P.I("dve", "tensor_tensor", dict(out=xq[:, :], in0=xp[:, :], in1=xp[:, :], op=ALU.mult), reads=[xp], writes=[xq])
    xs_ = P.sb("xs_", [128, 2], F32)
    P.I("dve", "reduce_sum", dict(out=xs_[:, 0:1], in_=xq[:, :], axis=AX.X), reads=[xq], writes=[xs_])
    psx = C.ps.next()
    P.I("pe", "matmul", dict(out=psx[:, 0:1], lhsT=C.ones_f[:, :], rhs=xs_[:, 0:1], start=True, stop=True), reads=[C.ones_f, xs_], writes=[psx])
    P.I("dve", "tensor_scalar", dict(out=xs_[:, 1:2], in0=psx[:, 0:1], scalar1=1.0 / 2048, scalar2=1e-6, op0=ALU.mult, op1=ALU.add), reads=[psx], writes=[xs_])
    P.I("act", "activation", dict(out=xs_[:, 1:2], in_=xs_[:, 1:2], func=AF.Sqrt), reads=[xs_], writes=[xs_])
    P.I("dve", "reciprocal", dict(out=xs_[:, 1:2], in_=xs_[:, 1:2]), reads=[xs_], writes=[xs_])
    hp = P.sb("hp", [128, 16], F32)
    P.I("dve", "tensor_scalar", dict(out=hp[:, :], in0=xp[:, :], scalar1=xs_[:, 1:2], scalar2=None, op0=ALU.mult), reads=[xp, xs_], writes=[hp])
    P.I("dve", "tensor_tensor", dict(out=hp[:, :], in0=hp[:, :], in1=A[:, :], op=ALU.mult), reads=[hp, A], writes=[hp])
    P.I("dve", "tensor_tensor", dict(out=hp[:, :], in0=hp[:, :], in1=vecs[:, 2, :], op=ALU.add), reads=[hp, vecs], writes=[hp])
    P.I("dve", "tensor_scalar", dict(out=hp[:, :], in0=hp[:, :], scalar1=nf[:, 0:1], scalar2=None, op0=ALU.mult), reads=[hp, nf], writes=[hp])
    sq = P.sb("nm_sq", [128, 16, 512], BF16)
    rstd = P.sb("nm_rstd", [128, 512], F32)
    for (c0, c1) in HALVES:
        w_ = c1 - c0
        for kc in range(16):
            P.I("act", "activation", dict(out=sq[:, kc, :w_], in_=xh[:, kc, c0:c1], func=AF.Square), reads=[xt[kc]], writes=[sq])
        ps = C.ps.next()
        for kc in range(16):
            P.I("pe", "matmul", dict(out=ps[:, :w_], lhsT=C.ones_bf[:, :], rhs=sq[:, kc, :w_], start=(kc == 0), stop=(kc == 15)), reads=[C.ones_bf, sq], writes=[ps])
        rstd_from_ps(P, ps, w_, 1.0 / 2048, 1e-6, rstd)
        for kc in range(16):
            P.I("dve", "tensor_tensor", dict(out=xh[:, kc, c0:c1], in0=xh[:, kc, c0:c1], in1=rstd[:, :w_], op=ALU.mult), reads=[xt[kc], rstd], writes=[xt[kc]])
            P.I("dve", "tensor_scalar", dict(out=xh[:, kc, c0:c1], in0=xh[:, kc, c0:c1], scalar1=A[:, kc:kc + 1], scalar2=vecs[:, 2, kc:kc + 1], op0=ALU.mult, op1=ALU.add), reads=[xt[kc], A, vecs], writes=[xt[kc]])
    xx = P.nc.alloc_sbuf_tensor("xx", [128, 16, TOK], BF16)
    xxt = [P.tok(f"xx{k}") for k in range(16)]
    for kc in range(16):
        P.I("pool", "tensor_tensor", dict(out=xx[:, kc, 1:TOK], in0=xh[:, kc, 0:TOK - 1], in1=xh[:, kc, 1:TOK], op=ALU.subtract), reads=[xt[kc]], writes=[xxt[kc]])
        P.I("dve", "tensor_tensor", dict(out=xx[:, kc, 0:1], in0=hp[:, kc:kc + 1], in1=xh[:, kc, 0:1], op=ALU.subtract), reads=[xt[kc], hp], writes=[xxt[kc]])
    mx = P.nc.alloc_sbuf_tensor("mixed", [128, 16, TOK], BF16)
    mxt = [P.tok(f"mx{k}") for k in range(16)]
    wring = Ring([P.sb(f"wt{i}", [128, 16, 512], BF16) for i in range(2)])
    stg = Ring([P.sb(f"stg{i}", [128, 512], F32) for i in range(4)])
    lora = P.nc.alloc_sbuf_tensor("lora", [128, 2, TOK], BF16)
    lot = [P.tok("lo0"), P.tok("lo1")]

    def mix(j):
        for kc in range(16):
            eng = "dve" if kc % 2 == 0 else "pool"
            P.I("dve", "scalar_tensor_tensor", dict(out=mx[:, kc, :], in0=xx[:, kc, :], scalar=vecs[:, 3 + j, kc:kc + 1], in1=xh[:, kc, :], op0=ALU.mult, op1=ALU.add), reads=[xxt[kc], vecs, xt[kc]], writes=[mxt[kc]])

    def src_m(kc, c0, c1):
        return mx[:, kc, c0:c1], [mxt[kc]]

    def lora_src(kp):
        def f(kc, c0, c1):
            return lora[:kp, kc, c0:c1], [lot[kc]]
        return f

    def ev_lora(func):
        def f(n, m, c0, c1, ps):
            if func is None:
                P.I("act", "copy", dict(out=lora[:m, n, c0:c1], in_=ps[:m, :]), reads=[ps], writes=[lot[n]])
            else:
                P.I("act", "activation", dict(out=lora[:m, n, c0:c1], in_=ps[:m, :], func=func), reads=[ps], writes=[lot[n]])
        return f

    def ev_out(dst, post):
        def f(n, m, c0, c1, ps):
            t = stg.next()
            post(n, ps, t)
            P.dma("sp", dst[n * 128:(n + 1) * 128, c0:c1], t[:, :], reads=[t])
        return f

    mix(4)
    linear_fm(P, C, a1[:, :], 2048, 96, src_m, ev_lora(None), wring)
    linear_fm(P, C, a2[:, :], 96, 2048, lora_src(96), ev_out(a_scr, lambda n, ps, t: P.I("act", "activation", dict(out=t[:, :], in_=ps[:, :], func=AF.Sigmoid, bias=vecs[:, 10, n:n + 1], scale=1.0), reads=[ps, vecs], writes=[t])), wring)
    mix(3)
    linear_fm(P, C, w1[:, :], 2048, 96, src_m, ev_lora(AF.Tanh), wring)

    def post_w(n, ps, t):
        P.I("act", "activation", dict(out=t[:, :], in_=ps[:, :], func=AF.Sigmoid, bias=vecs[:, 9, n:n + 1], scale=1.0), reads=[ps, vecs], writes=[t])
        P.I("dve", "tensor_scalar", dict(out=t[:, :], in0=t[:, :], scalar1=NEG_E05, scalar2=None, op0=ALU.mult), reads=[t], writes=[t])
    linear_fm(P, C, w2[:, :], 96, 2048, lora_src(96), ev_out(outs["lw"], post_w), wring)
    mix(5)
    linear_fm(P, C, g1[:, :], 2048, 256, src_m, ev_lora(AF.Sigmoid), wring)
    linear_fm(P, C, g2[:, :], 256, 2048, lora_src(128), ev_out(outs["gate"], lambda n, ps, t: P.I("act", "copy", dict(out=t[:, :], in_=ps[:, :]), reads=[ps], writes=[t])), wring)
    mix(2)
    linear_fm(P, C, wrkv[2], 2048, 2048, src_m, ev_out(outs["v"], lambda n, ps, t: P.I("act", "copy", dict(out=t[:, :], in_=ps[:, :]), reads=[ps], writes=[t])), wring)
    mix(0)
    linear_fm(P, C, wrkv[0], 2048, 2048, src_m, ev_out(outs["r"], lambda n, ps, t: P.I("act", "copy", dict(out=t[:, :], in_=ps[:, :]), reads=[ps], writes=[t])), wring)
    mix(1)
    at = Ring([P.sb(f"at{i}", [128, 512], F32) for i in range(2)])
    kq = Ring([P.sb(f"kq{i}", [128, 512], BF16) for i in range(2)])
    kt_ = Ring([P.sb(f"ktmp{i}", [128, 512], F32) for i in range(2)])

    def ev_k(n, m, c0, c1, ps):
        a_t = at.next()
        P.dma("sp", a_t[:, :], a_scr[n * 128:(n + 1) * 128, c0:c1], writes=[a_t])
        kp = kt_.next()
        P.I("dve", "tensor_scalar", dict(out=kp[:, :], in0=ps[:, :], scalar1=vecs[:, 11, n:n + 1], scalar2=None, op0=ALU.mult), reads=[ps, vecs], writes=[kp])
        q_ = kq.next()
        P.I("act", "activation", dict(out=q_[:, :], in_=kp[:, :], func=AF.Square), reads=[kp], writes=[q_])
        ps2 = C.ps.next()
        P.I("pe", "matmul", dict(out=ps2[:, :], lhsT=blk[:, :], rhs=q_[:, :], start=True, stop=True), reads=[blk, q_], writes=[ps2])
        rn = stg.next()
        P.I("dve", "tensor_scalar", dict(out=rn[:, :], in0=ps2[:, :], scalar1=1e-24, scalar2=None, op0=ALU.add), reads=[ps2], writes=[rn])
        P.I("act", "activation", dict(out=rn[:, :], in_=rn[:, :], func=AF.Sqrt), reads=[rn], writes=[rn])
        P.I("dve", "reciprocal", dict(out=rn[:, :], in_=rn[:, :]), reads=[rn], writes=[rn])
        tkk = stg.next()
        P.I("dve", "tensor_tensor", dict(out=tkk[:, :], in0=kp[:, :], in1=rn[:, :], op=ALU.mult), reads=[kp, rn], writes=[tkk])
        P.dma("sp", outs["kk"][n * 128:(n + 1) * 128, c0:c1], tkk[:, :], reads=[tkk])
        tka = stg.next()
        P.I("pool", "tensor_tensor", dict(out=tka[:, :], in0=tkk[:, :], in1=a_t[:, :], op=ALU.mult), reads=[tkk, a_t], writes=[tka])
        P.dma("sp", outs["kka"][n * 128:(n + 1) * 128, c0:c1], tka[:, :], reads=[tka])
        P.I("dve", "tensor_scalar", dict(out=a_t[:, :], in0=a_t[:, :], scalar1=-1.0, scalar2=vecs[:, 12, n:n + 1], op0=ALU.add, op1=ALU.mult), reads=[a_t, vecs], writes=[a_t])
        tk2 = stg.next()
        P.I("dve", "scalar_tensor_tensor", dict(out=tk2[:, :], in0=a_t[:, :], scalar=1.0, in1=ps[:, :], op0=ALU.add, op1=ALU.mult), reads=[a_t, ps], writes=[tk2])
        P.dma("sp", outs["k2"][n * 128:(n + 1) * 128, c0:c1], tk2[:, :], reads=[tk2])

    linear_fm(P, C, wrkv[1], 2048, 2048, src_m, ev_k, wring)
    return P.build()


def build_mixr(nchunks=128, seg=False):
    P = Prog()
    C = setup_common(P)
    L = 64
    NB = nchunks if seg else DEBUG.get("mixr_nb", 4)
    S_ = nchunks * L if seg else 8192
    BW = NB * L
    fmn = ("rF", "k2F", "kkF", "kkaF", "lwF")
    tmn = ("vT", "k2T", "kkaT", "lwT")
    fmd = {n: P.dram_in(n, [64, 4, S_]) for n in fmn}
    tmd = {n: P.dram_in(n, [S_, 256]) for n in tmn}
    triud = P.dram_in("triu64", [64, 64])
    sud = P.dram_in("su4", [64, 4, 64])
    sld = P.dram_in("sl4", [64, 4, 64])
    tr4d = P.dram_in("triu4", [64, 4, 64])
    out = P.dram_out("y", [S_, 256])
    triu = P.sb("triu", [64, 64], F32); P.dma("sp", triu[:, :], triud[:, :], writes=[triu])
    su4 = P.sb("su4", [64, 4, 64], F32); P.dma("sp", su4[:, :, :], sud[:, :, :], writes=[su4])
    sl4 = P.sb("sl4", [64, 4, 64], F32); P.dma("sp", sl4[:, :, :], sld[:, :, :], writes=[sl4])
    tr4 = P.sb("tr4", [64, 4, 64], F32); P.dma("sp", tr4[:, :, :], tr4d[:, :, :], writes=[tr4])
    nbuf = 1 if seg else 2
    fmb = {n: [P.sb(f"{n}{i}", [64, 4, BW], F32) for i in range(nbuf)] for n in fmn}
    tmb = {n: [P.sb(f"{n}{i}", [64, NB, 256], F32) for i in range(nbuf)] for n in tmn}
    M0 = P.sb("M0", [64, 4, 64], F32)
    if seg:
        m_in = P.dram_in("m_in", [64, 4, 64])
        m_out = P.dram_out("m_out", [64, 4, 64])
        P.dma("sp", M0[:, :, :], m_in[:, :, :], writes=[M0])
    else:
        P.I("dve", "memset", dict(ap=M0[:, :, :], constant=0.0), writes=[M0])

    def t3(name, n=2):
        return Ring([P.sb(f"{name}{i}", [64, 4, 64], F32) for i in range(n)])
    eGr, eGnr, eGxr, eGtr = t3("eG"), t3("eGn"), t3("eGx"), t3("eGt")
    rTr, aTr, bTr, kTr, btr, ktr = t3("rT"), t3("aT"), t3("bT"), t3("kT"), t3("b_t"), t3("k_t")
    Aakr, Arbr, Arkr = t3("Aak"), t3("Arb"), t3("Ark")
    Nr = [t3(f"N{i}") for i in range(6)]
    Ar = [t3(f"A{i}") for i in range(5)]
    Zr = t3("Z", 3)
    Yr = t3("Y")
    tM = P.sb("tM", [64, 4, 64], F32)

    def v3(ps):
        return ps[:64, :256].rearrange("p (h t) -> p h t", h=4)

    engs = ["dve", "dve"]
    ei = 0
    for c in range(nchunks):
        bi = c // NB
        ci = c % NB
        bsel = (bi % 2) if (DEBUG.get("mixr_db", 1) and not seg) else 0
        if ci == 0:
            for n in fmn:
                sb_ = 0 if DEBUG.get("mixr_src0") else bi
                P.dma(DEBUG.get("mixr_q", "pool"), fmb[n][bsel][:, :, :], fmd[n][:, :, sb_ * BW:(sb_ + 1) * BW], writes=[fmb[n][bsel]])
            for n in tmn:
                P.dma(DEBUG.get("mixr_q", "pool"), tmb[n][bsel][:, :, :], tmd[n][sb_ * BW:(sb_ + 1) * BW, :].rearrange("(n t) d -> t n d", t=L), writes=[tmb[n][bsel]])
        F = {n: fmb[n][bsel] for n in fmn}
        T = {n: tmb[n][bsel] for n in tmn}
        cs = slice(ci * L, (ci + 1) * L)
        psG = C.ps.next()
        for hd in range(4):
            P.I("pe", "matmul", dict(out=psG[:64, hd * 64:(hd + 1) * 64], lhsT=T["lwT"][:, ci, hd * 64:(hd + 1) * 64], rhs=triu[:, :], start=True, stop=True), reads=[T["lwT"], triu], writes=[psG])
        psGt = C.ps.next()
        P.I("pe", "matmul", dict(out=psGt[:64, :256], lhsT=triu[:, :], rhs=T["lwT"][:, ci, :], start=True, stop=True), reads=[T["lwT"], triu], writes=[psGt])
        eG, eGn, eGx, eGt = eGr.next(), eGnr.next(), eGxr.next(), eGtr.next()
        P.I("act", "activation", dict(out=eG[:, :, :], in_=v3(psG), func=AF.Exp), reads=[psG], writes=[eG])
        P.I("act", "activation", dict(out=eGn[:, :, :], in_=v3(psG), func=AF.Exp, scale=-1.0), reads=[psG], writes=[eGn])
        P.I("dve", "tensor_tensor", dict(out=eGx[:, :, :], in0=v3(psG), in1=F["lwF"][:, :, cs], op=ALU.subtract), reads=[psG, F["lwF"]], writes=[eGx])
        P.I("act", "activation", dict(out=eGx[:, :, :], in_=eGx[:, :, :], func=AF.Exp), reads=[eGx], writes=[eGx])
        P.I("act", "activation", dict(out=eGt[:, :, :], in_=v3(psGt), func=AF.Exp, scale=-1.0), reads=[psGt], writes=[eGt])
        rT, aT, bT, kT, b_t, k_t = rTr.next(), aTr.next(), bTr.next(), kTr.next(), btr.next(), ktr.next()
        P.I("dve", "tensor_tensor", dict(out=rT[:, :, :], in0=F["rF"][:, :, cs], in1=eG[:, :, :], op=ALU.mult), reads=[F["rF"], eG], writes=[rT])
        P.I("dve", "scalar_tensor_tensor", dict(out=aT[:, :, :], in0=F["kkF"][:, :, cs], scalar=-1.0, in1=eGx[:, :, :], op0=ALU.mult, op1=ALU.mult), reads=[F["kkF"], eGx], writes=[aT])
        P.I("dve", "tensor_tensor", dict(out=bT[:, :, :], in0=F["kkaF"][:, :, cs], in1=eGn[:, :, :], op=ALU.mult), reads=[F["kkaF"], eGn], writes=[bT])
        P.I("dve", "tensor_tensor", dict(out=kT[:, :, :], in0=F["k2F"][:, :, cs], in1=eGn[:, :, :], op=ALU.mult), reads=[F["k2F"], eGn], writes=[kT])
        P.I("dve", "tensor_tensor", dict(out=b_t[:, :, :], in0=T["kkaT"][:, ci, :].rearrange("p (h k) -> p h k", h=4), in1=eGt[:, :, :], op=ALU.mult), reads=[T["kkaT"], eGt], writes=[b_t])
        P.I("dve", "tensor_tensor", dict(out=k_t[:, :, :], in0=T["k2T"][:, ci, :].rearrange("p (h k) -> p h k", h=4), in1=eGt[:, :, :], op=ALU.mult), reads=[T["k2T"], eGt], writes=[k_t])

        def mm4(lhs, rhs, dst, mask):
            ps = C.ps.next()
            for hd in range(4):
                P.I("pe", "matmul", dict(out=ps[:64, hd * 64:(hd + 1) * 64], lhsT=lhs[:, hd, :], rhs=rhs[:, hd, :], start=True, stop=True), reads=[lhs, rhs], writes=[ps])
            nonlocal ei
            eng = engs[ei % 2]
            ei += 1
            if mask is not None:
                if eng == "pool":
                    eng = "dve"
                P.I(eng, "tensor_tensor", dict(out=dst[:, :, :], in0=v3(ps), in1=mask[:, :, :], op=ALU.mult), reads=[ps, mask], writes=[dst])
            else:
                if ei % 2 == 0:
                    P.I("act", "copy", dict(out=dst[:, :, :], in_=v3(ps)), reads=[ps], writes=[dst])
                else:
                    P.I("dve", "tensor_copy", dict(out=dst[:, :, :], in_=v3(ps)), reads=[ps], writes=[dst])

        N = [r_.next() for r_ in Nr]
        A = [r_.next() for r_ in Ar]
        Aak, Arb, Ark = Aakr.next(), Arbr.next(), Arkr.next()
        mm4(bT, aT, N[0], su4)
        mm4(aT, bT, A[0], sl4)
        mm4(kT, aT, Aak, su4)
        mm4(bT, rT, Arb, tr4)
        mm4(kT, rT, Ark, tr4)
        for i in range(5):
            if i < 4:
                mm4(N[i], A[i], A[i + 1], None)
            mm4(A[i], N[i], N[i + 1], None)
        vT = T["vT"]
        psX = C.ps.next()
        for hd in range(4):
            hs = slice(hd * 64, (hd + 1) * 64)
            P.I("pe", "matmul", dict(out=psX[:64, hs], lhsT=aT[:, hd, :], rhs=M0[:, hd, :], start=True, stop=False), reads=[aT, M0], writes=[psX])
            P.I("pe", "matmul", dict(out=psX[:64, hs], lhsT=Aak[:, hd, :], rhs=vT[:, ci, hs], start=False, stop=True), reads=[Aak, vT], writes=[psX])
        Z = Zr.next()
        P.I("act", "copy", dict(out=Z[:, :, :], in_=v3(psX)), reads=[psX], writes=[Z])
        for i in range(6):
            psZ = C.ps.next()
            for hd in range(4):
                P.I("pe", "matmul", dict(out=psZ[:64, hd * 64:(hd + 1) * 64], lhsT=N[i][:, hd, :], rhs=Z[:, hd, :], start=True, stop=True), reads=[N[i], Z], writes=[psZ])
            Z2 = Zr.next()
            P.I("dve", "tensor_tensor", dict(out=Z2[:, :, :], in0=v3(psZ), in1=Z[:, :, :], op=ALU.add), reads=[psZ, Z], writes=[Z2])
            Z = Z2
        U = Z
        psY = C.ps.next()
        for hd in range(4):
            hs = slice(hd * 64, (hd + 1) * 64)
            P.I("pe", "matmul", dict(out=psY[:64, hs], lhsT=rT[:, hd, :], rhs=M0[:, hd, :], start=True, stop=False), reads=[rT, M0], writes=[psY])
            P.I("pe", "matmul", dict(out=psY[:64, hs], lhsT=Arb[:, hd, :], rhs=U[:, hd, :], start=False, stop=False), reads=[Arb, U], writes=[psY])
            P.I("pe", "matmul", dict(out=psY[:64, hs], lhsT=Ark[:, hd, :], rhs=vT[:, ci, hs], start=False, stop=True), reads=[Ark, vT], writes=[psY])
        Y = Yr.next()
        P.I("act", "copy", dict(out=Y[:, :, :], in_=v3(psY)), reads=[psY], writes=[Y])
        P.dma("sp", out[c * L:(c + 1) * L, :].rearrange("t (h v) -> t h v", h=4), Y[:, :, :], reads=[Y])
        psM = C.ps.next()
        for hd in range(4):
            hs = slice(hd * 64, (hd + 1) * 64)
            P.I("pe", "matmul", dict(out=psM[:64, hs], lhsT=b_t[:, hd, :], rhs=U[:, hd, :], start=True, stop=False), reads=[b_t, U], writes=[psM])
            P.I("pe", "matmul", dict(out=psM[:64, hs], lhsT=k_t[:, hd, :], rhs=vT[:, ci, hs], start=False, stop=True), reads=[k_t, vT], writes=[psM])
        P.I("dve", "tensor_tensor", dict(out=tM[:, :, :], in0=v3(psM), in1=M0[:, :, :], op=ALU.add), reads=[psM, M0], writes=[tM])
        for hd in range(4):
            P.I("dve", "tensor_scalar", dict(out=M0[:, hd, :], in0=tM[:, hd, :], scalar1=eG[:, hd, 63:64], scalar2=None, op0=ALU.mult), reads=[tM, eG], writes=[M0])
    if seg:
        P.dma("sp", m_out[:, :, :], M0[:, :, :], reads=[M0])
    return P.build()


def build_t2r(final):
    P = Prog()
    C = setup_common(P)
    xh, xt, vecs, out = t2_common_inputs(P, 9)
    ind = {n: P.dram_in(n, [2048, TOK]) for n in ("yT", "r", "k2", "v", "gate")}
    wo = P.dram_in("w_o", [2048, 2048])
    blkd = P.dram_in("blk64", [128, 128])
    D = moe_inputs(P, 0)
    wring = Ring([P.sb(f"wt{i}", [128, 16, 512], BF16) for i in range(2)])
    zbf = P.nc.alloc_sbuf_tensor("zbf", [128, 16, TOK], BF16)
    zt = [P.tok(f"z{k}") for k in range(16)]
    blk = P.sb("blk", [128, 128], F32)
    P.dma("sp", blk[:, :], blkd[:, :], writes=[blk])
    rings = {n: Ring([P.sb(f"{n}{i}", [128, 512], F32) for i in range(1)]) for n in ("yT", "r", "k2", "v", "gate")}
    dr = Ring([P.sb(f"d{i}", [128, 512], F32) for i in range(1)])
    d2r = Ring([P.sb(f"d2{i}", [128, 512], F32) for i in range(1)])
    for kc in range(16):
        for (c0, c1) in HALVES:
            t = {}
            for n in ("yT", "r", "k2", "v", "gate"):
                t[n] = rings[n].next()
                P.dma("sp", t[n][:, :], ind[n][kc * 128:(kc + 1) * 128, c0:c1], writes=[t[n]])
            y = t["yT"]
            psm = C.ps.next()
            P.I("pe", "matmul", dict(out=psm[:, :], lhsT=blk[:, :], rhs=y[:, :], start=True, stop=True), reads=[blk, y], writes=[psm])
            d = dr.next()
            P.I("dve", "scalar_tensor_tensor", dict(out=d[:, :], in0=psm[:, :], scalar=-1.0 / 64, in1=y[:, :], op0=ALU.mult, op1=ALU.add), reads=[psm, y], writes=[d])
            d2 = d2r.next()
            P.I("act", "activation", dict(out=d2[:, :], in_=d[:, :], func=AF.Square), reads=[d], writes=[d2])
            psv = C.ps.next()
            P.I("pe", "matmul", dict(out=psv[:, :], lhsT=blk[:, :], rhs=d2[:, :], start=True, stop=True), reads=[blk, d2], writes=[psv])
            P.I("dve", "tensor_scalar", dict(out=d2[:, :], in0=psv[:, :], scalar1=1.0 / 64, scalar2=64e-5, op0=ALU.mult, op1=ALU.add), reads=[psv], writes=[d2])
            P.I("act", "activation", dict(out=d2[:, :], in_=d2[:, :], func=AF.Sqrt), reads=[d2], writes=[d2])
            P.I("dve", "reciprocal", dict(out=d2[:, :], in_=d2[:, :]), reads=[d2], writes=[d2])
            P.I("pool", "tensor_tensor", dict(out=d[:, :], in0=d[:, :], in1=d2[:, :], op=ALU.mult), reads=[d, d2], writes=[d])
            P.I("dve", "tensor_scalar", dict(out=d[:, :], in0=d[:, :], scalar1=vecs[:, 0, kc:kc + 1], scalar2=vecs[:, 1, kc:kc + 1], op0=ALU.mult, op1=ALU.add), reads=[d, vecs], writes=[d])
            rk = t["r"]
            P.I("pool", "tensor_tensor", dict(out=rk[:, :], in0=rk[:, :], in1=t["k2"][:, :], op=ALU.mult), reads=[rk, t["k2"]], writes=[rk])
            P.I("dve", "tensor_scalar", dict(out=rk[:, :], in0=rk[:, :], scalar1=vecs[:, 2, kc:kc + 1], scalar2=None, op0=ALU.mult), reads=[rk, vecs], writes=[rk])
            psb = C.ps.next()
            P.I("pe", "matmul", dict(out=psb[:, :], lhsT=blk[:, :], rhs=rk[:, :], start=True, stop=True), reads=[blk, rk], writes=[psb])
            P.I("dve", "tensor_tensor", dict(out=t["v"][:, :], in0=psb[:, :], in1=t["v"][:, :], op=ALU.mult), reads=[psb, t["v"]], writes=[t["v"]])
            P.I("pool", "tensor_tensor", dict(out=d[:, :], in0=d[:, :], in1=t["v"][:, :], op=ALU.add), reads=[d, t["v"]], writes=[d])
            P.I("dve", "tensor_tensor", dict(out=zbf[:, kc, c0:c1], in0=d[:, :], in1=t["gate"][:, :], op=ALU.mult), reads=[d, t["gate"]], writes=[zt[kc]])
    outproj_residual(P, C, wo, zbf, zt, xh, xt, vecs, 3, wring)
    iv = dict(gain2=4, sc2=5, sh2=6, g2=7)
    moe_tail(P, C, D, xh, xt, vecs, iv, wring, zbf, zt, final_gain_idx=(8 if final else None), out_dram=out)
    return P.build()


def layer_rwkv(inp, xs, mod, layer, j, final):
    sh1, sc1, g1, sh2, sc2, g2 = [mod[layer, i] for i in range(6)]
    mu = inp["rwkv_mu"][j]
    v1 = vlay([inp["norm_gain"][layer, 0], sc1, sh1] + [mu[i] for i in range(6)] + [inp["rwkv_w0"][j], inp["rwkv_a0"][j], inp["rwkv_k_k"][j], inp["rwkv_k_a"][j]])
    blk = _c(np.kron(np.eye(2), np.ones((64, 64))))
    wts = dict(w_rkv=_c(inp["rwkv_w_rkv"][j]), w1=_c(inp["rwkv_w1"][j]), w2=_c(inp["rwkv_w2"][j]), a1=_c(inp["rwkv_a1"][j]), a2=_c(inp["rwkv_a2"][j]),
               g1=_c(inp["rwkv_g1"][j]), g2=_c(inp["rwkv_g2"][j]), blk64=blk)
    maps = []
    for c in range(NCORES):
        if c == 0:
            xp = np.zeros((128, 16), np.float32); nf = np.zeros((128, 1), np.float32)
        else:
            xp = _c(xs[c - 1][:, TOK - 1].reshape(16, 128).T); nf = np.ones((128, 1), np.float32)
        maps.append(dict(xT=xs[c], xprev=xp, notfirst=nf, vecs=v1, **wts))
    r1 = _run(_prog("t1r", build_t1r), maps)
    full = {n: np.concatenate([r[n] for r in r1], axis=1) for n in ("r", "k2", "v", "lw", "kk", "kka")}
    if "on" in DEBUG:
        DEBUG["t1r"] = full; DEBUG["gate"] = np.concatenate([r["gate"] for r in r1], axis=1)
    triu64 = _c(np.triu(np.ones((64, 64))))
    su = np.triu(np.ones((64, 64)), 1)
    cst = dict(triu64=triu64, su4=_c(np.repeat(su[:, None, :], 4, axis=1)), sl4=_c(np.repeat(su.T[:, None, :], 4, axis=1)), triu4=_c(np.repeat(triu64[:, None, :], 4, axis=1)))
    NSEG = 8
    SEGT = 8192 // NSEG
    states = [np.zeros((64, 4, 64), np.float32) for _ in range(NCORES)]
    ysegs = [[] for _ in range(NCORES)]
    ncm = _prog("mixr", build_mixr, SEGT // 64, True)
    for sg_ in range(NSEG):
        ts_ = slice(sg_ * SEGT, (sg_ + 1) * SEGT)
        maps = []
        for c in range(NCORES):
            rows = slice(c * 256, (c + 1) * 256)
            m = dict(cst)
            for fn, src in (("rF", "r"), ("k2F", "k2"), ("kkF", "kk"), ("kkaF", "kka"), ("lwF", "lw")):
                m[fn] = _c(full[src][rows, ts_].reshape(4, 64, SEGT).transpose(1, 0, 2))
            for tn, src in (("vT", "v"), ("k2T", "k2"), ("kkaT", "kka"), ("lwT", "lw")):
                m[tn] = _c(full[src][rows, ts_].T)
            m["m_in"] = states[c]
            maps.append(m)
        rs = _run(ncm, maps)
        for c in range(NCORES):
            states[c] = _c(rs[c]["m_out"])
            ysegs[c].append(rs[c]["y"])
    r2 = [dict(y=np.concatenate(ysegs[c], axis=0)) for c in range(NCORES)]
    yT = np.concatenate([r["y"].T for r in r2], axis=0)
    if "on" in DEBUG:
        DEBUG["yT"] = yT
        if "stop_after_mix" in DEBUG:
            return None
    v2 = vlay([inp["rwkv_ln_gain"][j], inp["rwkv_ln_bias"][j], inp["rwkv_r_k"][j].reshape(-1), g1, inp["norm_gain"][layer, 1], sc2, sh2, g2, inp["final_gain"]])
    mm = moe_maps(inp, layer)
    wo = _c(inp["rwkv_w_o"][j])
    maps = [dict(xT=xs[c], vecs=v2, yT=_c(yT[:, c * TOK:(c + 1) * TOK]), r=r1[c]["r"], k2=r1[c]["k2"], v=r1[c]["v"], gate=r1[c]["gate"], w_o=wo, blk64=blk, **mm) for c in range(NCORES)]
    r3 = _run(_prog("t2r", build_t2r, final), maps)
    return [r["xout"] for r in r3]


def kernel(**inputs):
    inp = {k: np.asarray(v) for k, v in inputs.items()}
    mod = run_mod(inp)
    run_wcast(inp)
    x = inp["x"][0]
    xs = [_c(x[c * TOK:(c + 1) * TOK].T) for c in range(NCORES)]
    seen = [0, 0, 0]
    for layer in range(4):
        kind = layer % 3
        j = seen[kind]
        seen[kind] += 1
        final = (layer == 3)
        if kind == 0:
            xs = layer_mlstm(inp, xs, mod, layer, j, final)
        elif kind == 1:
            xs = layer_rwkv(inp, xs, mod, layer, j, final)
        else:
            xs = layer_diff(inp, xs, mod, layer, j, final)
    out = np.concatenate([a.T for a in xs], axis=0)[None]
    return np.ascontiguousarray(out, dtype=np.float32)
```
